# Optimizing a Trainium2 kernel written in Bass

```python
import math
import jax
import jax.numpy as jnp
from jax import lax
import numpy as np

D_MODEL = 1024
BATCH = 8
SEQ = 8192
DEPTH = 2

GRID_W = 64
CTX_LEN = 256
EPS = 1e-6

MLSTM_HEADS = 8
MLSTM_DQK = 64
MLSTM_DV = 64
MLSTM_CONV = 3
MLSTM_CHUNK = 64
DIFF_HEADS = 4
DIFF_DQK = 64
DIFF_DV = 128
ROPE_BASE = 10000.0
Q_BLOCK = 128
CONV_WIDTH = 31
D_FF = 2816
N_EXPERTS = 8
TOP_K = 2
D_FF_EXPERT = 2816
MOE_BLOCK = 128

MQK = MLSTM_HEADS * MLSTM_DQK
MV = MLSTM_HEADS * MLSTM_DV
MG = 4 * MLSTM_HEADS
DQK = DIFF_HEADS * 2 * DIFF_DQK
DVW = DIFF_HEADS * DIFF_DV
OFF_MQ = 0
OFF_MK = OFF_MQ + MQK
OFF_MV = OFF_MK + MQK
OFF_MO = OFF_MV + MV
OFF_MG = OFF_MO + MV
OFF_DQ = OFF_MG + MG
OFF_DK = OFF_DQ + DQK
OFF_DV = OFF_DK + DQK
IN0_WIDTH = OFF_DV + DVW
MIX0_WIDTH = MV + DVW

kernel_name = 'hybrid_mlstm_diffattn_conformer_moe_dit'

F32 = jnp.float32


def rmsnorm(x, g):
    xf = x.astype(F32)
    y = xf * lax.rsqrt(jnp.mean(xf * xf, axis=-1, keepdims=True) + EPS)
    return (y * g.astype(F32)).astype(x.dtype)


def layernorm(x, g, b):
    xf = x.astype(F32)
    mu = jnp.mean(xf, axis=-1, keepdims=True)
    var = jnp.mean(jnp.square(xf - mu), axis=-1, keepdims=True)
    return ((xf - mu) * lax.rsqrt(var + EPS) * g.astype(F32) + b.astype(F32)).astype(x.dtype)


def adaln(cond, w, b):
    return jax.nn.silu(cond) @ w + b


def modulate(x, g, shift, scale):
    return rmsnorm(x, g) * (1 + scale) + shift


def depthwise_conv(x, w):
    k = w.shape[0]
    return lax.conv_general_dilated(
        x, w[:, None, :].astype(x.dtype), window_strides=(1,),
        padding=[((k - 1) // 2, k // 2)], dimension_numbers=('NWC', 'WIO', 'NWC'),
        feature_group_count=x.shape[-1])


def to_heads(a, n_heads):
    b, t, _ = a.shape
    return a.reshape(b, t, n_heads, -1).transpose(0, 2, 1, 3)


def axial_rope_tables(t_len):
    rows = t_len // GRID_W
    row = jnp.repeat(jnp.arange(rows, dtype=F32), GRID_W)
    col = jnp.tile(jnp.arange(GRID_W, dtype=F32), rows)
    axis_dim = DIFF_DQK // 2
    inv = ROPE_BASE ** (-jnp.arange(0, axis_dim, 2, dtype=F32) / axis_dim)
    ang_r = row[:, None] * inv
    ang_c = col[:, None] * inv
    return jnp.cos(ang_r), jnp.sin(ang_r), jnp.cos(ang_c), jnp.sin(ang_c)


def rotate(x, cos, sin):
    x1, x2 = jnp.split(x, 2, axis=-1)
    return jnp.concatenate([x1 * cos - x2 * sin, x2 * cos + x1 * sin], axis=-1)


def apply_axial_rope(x, tables):
    cr, sr, cc, sc = tables
    xr, xc = jnp.split(x.astype(F32), 2, axis=-1)
    return jnp.concatenate([rotate(xr, cr, sr), rotate(xc, cc, sc)], axis=-1).astype(x.dtype)


def mlstm_gates(g, gate_b):
    g = (g + gate_b).astype(F32)
    b, t, _ = g.shape
    g = g.reshape(b, t, 4, MLSTM_HEADS).transpose(2, 0, 3, 1)
    return g[0], jax.nn.log_sigmoid(g[1]), g[2], jax.nn.log_sigmoid(g[3])


def mlstm_state_update(C, n, m, k, v, ig, b):
    b_last = b[..., -1]
    log_s = b_last[..., None] - b + ig
    m_new = jnp.maximum(b_last + m, jnp.max(log_s, axis=-1))
    ws = jnp.exp(log_s - m_new[..., None])
    decay = jnp.exp(b_last + m - m_new)
    C_new = decay[..., None, None] * C + jnp.einsum('bhs,bhsd,bhse->bhde', ws, k, v)
    n_new = decay[..., None] * n + jnp.einsum('bhs,bhsd->bhd', ws, k)
    return C_new, n_new, m_new


def mlstm_chunk(carry, inp):
    C, n, m = carry
    q, k, v, ig, lf = inp
    L = q.shape[2]
    b = jnp.cumsum(lf, axis=-1)
    order = jnp.tril(jnp.ones((L, L), dtype=bool))
    log_d = jnp.where(order, b[..., :, None] - b[..., None, :] + ig[..., None, :], -jnp.inf)
    log_inter = b + m[..., None]
    m_t = jnp.maximum(log_inter, jnp.max(log_d, axis=-1))
    s = jnp.einsum('bhtd,bhsd->bhts', q, k) * jnp.exp(log_d - m_t[..., None])
    w_inter = jnp.exp(log_inter - m_t)
    num = w_inter[..., None] * jnp.einsum('bhtd,bhde->bhte', q, C) + jnp.einsum('bhts,bhse->bhte', s, v)
    den = w_inter * jnp.einsum('bhtd,bhd->bht', q, n) + jnp.sum(s, axis=-1)
    h = num / jnp.maximum(jnp.abs(den), jnp.exp(-m_t))[..., None]
    return mlstm_state_update(C, n, m, k, v, ig, b), h


def mlstm_scan(q, k, v, ig, lf, state):
    b, h, t, _ = q.shape
    nc = t // MLSTM_CHUNK

    def chunks(a):
        return jnp.moveaxis(a.reshape(a.shape[:2] + (nc, MLSTM_CHUNK) + a.shape[3:]), 2, 0)

    _, out = lax.scan(mlstm_chunk, state, (chunks(q), chunks(k), chunks(v), chunks(ig), chunks(lf)))
    return jnp.moveaxis(out, 0, 2).reshape(b, h, t, -1)


def mlstm_context_state(k, v, ig, lf):
    b, h, _, dk = k.shape
    C0 = jnp.zeros((b, h, dk, v.shape[-1]), F32)
    n0 = jnp.zeros((b, h, dk), F32)
    m0 = jnp.zeros((b, h), F32)
    return mlstm_state_update(C0, n0, m0, k, v, ig, jnp.cumsum(lf, axis=-1))


def mlstm_mixer(q, k, v, o, ig_f, lf_f, ig_b, lf_b, k_c, v_c, igc_f, lfc_f, igc_b, lfc_b, norm_g):
    H = MLSTM_HEADS
    B_, T_, _ = q.shape
    q = to_heads(q, H).astype(F32) * (MLSTM_DQK ** -0.5)
    k = to_heads(k, H).astype(F32)
    v = to_heads(v, H).astype(F32)
    k_c = to_heads(k_c, H).astype(F32)
    v_c = to_heads(v_c, H).astype(F32)
    st_f = mlstm_context_state(k_c, v_c, igc_f, lfc_f)
    st_b = mlstm_context_state(jnp.flip(k_c, 2), jnp.flip(v_c, 2), jnp.flip(igc_b, -1), jnp.flip(lfc_b, -1))
    h_f = mlstm_scan(q, k, v, ig_f, lf_f, st_f)
    h_b = jnp.flip(mlstm_scan(jnp.flip(q, 2), jnp.flip(k, 2), jnp.flip(v, 2),
                              jnp.flip(ig_b, -1), jnp.flip(lf_b, -1), st_b), 2)
    h = (h_f + h_b).transpose(0, 2, 1, 3)
    h = rmsnorm(h, norm_g.reshape(H, MLSTM_DV)).reshape(B_, T_, MV)
    return (h * jax.nn.sigmoid(o.astype(F32))).astype(o.dtype)


def diff_attention_blocks(q1, q2, k1_all, k2_all, v_all, lam):
    B_, H, T_, d = q1.shape
    nb = T_ // Q_BLOCK
    scale = DIFF_DQK ** -0.5

    def blocks(a):
        return a.reshape(B_, H, nb, Q_BLOCK, d).transpose(2, 0, 1, 3, 4)

    def one_block(qs):
        a1, a2 = qs
        s1 = jnp.einsum('bhqd,bhkd->bhqk', a1, k1_all).astype(F32) * scale
        s2 = jnp.einsum('bhqd,bhkd->bhqk', a2, k2_all).astype(F32) * scale
        p = jax.nn.softmax(s1, axis=-1) - lam * jax.nn.softmax(s2, axis=-1)
        return jnp.einsum('bhqk,bhkv->bhqv', p.astype(v_all.dtype), v_all)

    out = lax.map(one_block, (blocks(q1), blocks(q2)))
    return out.transpose(1, 2, 0, 3, 4).reshape(B_, H, T_, -1)


def diff_mixer(q, k, v, k_c, v_c, lq1, lk1, lq2, lk2, norm_g, layer_idx):
    H = DIFF_HEADS
    B_, T_, _ = q.shape

    def split_qk(a):
        a = a.reshape(a.shape[0], a.shape[1], H, 2, DIFF_DQK)
        return a[..., 0, :].transpose(0, 2, 1, 3), a[..., 1, :].transpose(0, 2, 1, 3)

    tables = axial_rope_tables(T_)
    q1, q2 = [apply_axial_rope(a, tables) for a in split_qk(q)]
    k1, k2 = [apply_axial_rope(a, tables) for a in split_qk(k)]
    k1c, k2c = split_qk(k_c)
    k1_all = jnp.concatenate([k1c, k1], axis=2)
    k2_all = jnp.concatenate([k2c, k2], axis=2)
    v_all = jnp.concatenate([to_heads(v_c, H), to_heads(v, H)], axis=2)
    lam_init = 0.8 - 0.6 * math.exp(-0.3 * layer_idx)
    lam = (jnp.exp(jnp.sum(lq1.astype(F32) * lk1.astype(F32)))
           - jnp.exp(jnp.sum(lq2.astype(F32) * lk2.astype(F32))) + lam_init)
    o = diff_attention_blocks(q1, q2, k1_all, k2_all, v_all, lam).transpose(0, 2, 1, 3)
    o = rmsnorm(o, norm_g.reshape(H, DIFF_DV)) * (1.0 - lam_init)
    return o.reshape(B_, T_, DVW)


def swiglu(u, w1, w3, w2):
    return (jax.nn.silu(u @ w1) * (u @ w3)) @ w2


def moe_swiglu(u, router_w, w1, w3, w2):
    B_, T_, D = u.shape
    tokens = u.reshape(-1, D)
    N = tokens.shape[0]
    logits = (tokens @ router_w).astype(F32)
    top_vals, top_idx = lax.top_k(logits, TOP_K)
    gates = jax.nn.softmax(top_vals, axis=-1)
    A = N * TOP_K
    flat_e = top_idx.reshape(-1)
    flat_tok = jnp.repeat(jnp.arange(N, dtype=jnp.int32), TOP_K)
    flat_g = gates.reshape(-1)
    order = jnp.argsort(flat_e)
    s_e, s_tok, s_g = flat_e[order], flat_tok[order], flat_g[order]
    counts = jnp.bincount(flat_e, length=N_EXPERTS)
    starts = jnp.cumsum(counts) - counts
    padded = ((counts + MOE_BLOCK - 1) // MOE_BLOCK) * MOE_BLOCK
    pends = jnp.cumsum(padded)
    pstarts = pends - padded
    dest = pstarts[s_e] + (jnp.arange(A) - starts[s_e])
    n_blk = (A + MOE_BLOCK - 1) // MOE_BLOCK + N_EXPERTS
    R = n_blk * MOE_BLOCK
    slot_tok = jnp.zeros((R,), jnp.int32).at[dest].set(s_tok)
    slot_g = jnp.zeros((R,), F32).at[dest].set(s_g)
    block_e = jnp.clip(jnp.searchsorted(pends, jnp.arange(n_blk) * MOE_BLOCK, side='right'), 0, N_EXPERTS - 1)

    def run_block(args):
        tok_idx, e = args
        xb = tokens[tok_idx]
        return (jax.nn.silu(xb @ w1[e]) * (xb @ w3[e])) @ w2[e]

    y = lax.map(run_block, (slot_tok.reshape(n_blk, MOE_BLOCK), block_e)).reshape(R, D)
    y = y * slot_g[:, None].astype(y.dtype)
    out = jnp.zeros((N, D), y.dtype).at[slot_tok].add(y)
    return out.reshape(B_, T_, D)


def even_layer(x, c, ctx, c_ctx, layer_idx, mod_w, mod_b, mix_pre_g, mix_post_g, w_in, mlstm_gate_b,
               mlstm_conv_w, mlstm_norm_g, lambda_q1, lambda_k1, lambda_q2, lambda_k2, diff_norm_g,
               w_out, ffn_pre_g, ffn_post_g, ffn_w1, ffn_w3, ffn_w2):
    sh_m, sc_m, gt_m, sh_f, sc_f, gt_f = jnp.split(adaln(c, mod_w, mod_b)[:, None, :], 6, axis=-1)
    sh_c, sc_c = jnp.split(adaln(c_ctx, mod_w, mod_b), 6, axis=-1)[:2]
    u = modulate(x, mix_pre_g, sh_m, sc_m)
    p = u @ w_in
    q_m = jax.nn.silu(depthwise_conv(p[..., OFF_MQ:OFF_MK], mlstm_conv_w[:, :MQK]))
    k_m = jax.nn.silu(depthwise_conv(p[..., OFF_MK:OFF_MV], mlstm_conv_w[:, MQK:]))
    v_m = p[..., OFF_MV:OFF_MO]
    o_m = p[..., OFF_MO:OFF_MG]
    ig_f, lf_f, ig_b, lf_b = mlstm_gates(p[..., OFF_MG:OFF_DQ], mlstm_gate_b)
    uc = modulate(ctx, mix_pre_g, sh_c, sc_c)
    kv_mc = uc @ w_in[:, OFF_MK:OFF_MO]
    k_mc = jax.nn.silu(depthwise_conv(kv_mc[..., :MQK], mlstm_conv_w[:, MQK:]))
    v_mc = kv_mc[..., MQK:]
    igc_f, lfc_f, igc_b, lfc_b = mlstm_gates(uc @ w_in[:, OFF_MG:OFF_DQ], mlstm_gate_b)
    kv_dc = uc @ w_in[:, OFF_DK:]
    h_m = mlstm_mixer(q_m, k_m, v_m, o_m, ig_f, lf_f, ig_b, lf_b,
                      k_mc, v_mc, igc_f, lfc_f, igc_b, lfc_b, mlstm_norm_g)
    h_d = diff_mixer(p[..., OFF_DQ:OFF_DK], p[..., OFF_DK:OFF_DV], p[..., OFF_DV:],
                     kv_dc[..., :DQK], kv_dc[..., DQK:],
                     lambda_q1, lambda_k1, lambda_q2, lambda_k2, diff_norm_g, layer_idx)
    mix = jnp.concatenate([h_m, h_d], axis=-1) @ w_out
    x = x + gt_m * rmsnorm(mix, mix_post_g)
    y = swiglu(modulate(x, ffn_pre_g, sh_f, sc_f), ffn_w1, ffn_w3, ffn_w2)
    return x + gt_f * rmsnorm(y, ffn_post_g)


def odd_layer(x, c, mod_w, mod_b, mix_pre_g, mix_post_g, pw1_w, pw1_b, dw_w, dw_b, ln_g, ln_b,
              pw2_w, pw2_b, ffn_pre_g, ffn_post_g, router_w, moe_w1, moe_w3, moe_w2):
    sh_m, sc_m, gt_m, sh_f, sc_f, gt_f = jnp.split(adaln(c, mod_w, mod_b)[:, None, :], 6, axis=-1)
    u = modulate(x, mix_pre_g, sh_m, sc_m)
    a, g = jnp.split(u @ pw1_w + pw1_b, 2, axis=-1)
    h = a * jax.nn.sigmoid(g)
    h = depthwise_conv(h, dw_w) + dw_b
    h = jax.nn.silu(layernorm(h, ln_g, ln_b))
    y = h @ pw2_w + pw2_b
    x = x + gt_m * rmsnorm(y, mix_post_g)
    y = moe_swiglu(modulate(x, ffn_pre_g, sh_f, sc_f), router_w, moe_w1, moe_w3, moe_w2)
    return x + gt_f * rmsnorm(y, ffn_post_g)


def setup_inputs(seed: int = 0) -> dict:
    key = jax.random.key(seed)
    keys = list(jax.random.split(key, 48))
    D = D_MODEL

    def nrm(shape, scale):
        return jax.random.normal(keys.pop(), shape, F32) * scale

    def gain(n):
        return 1.0 + nrm((n,), 0.05)

    inp = {}
    inp['x'] = nrm((BATCH, SEQ, D), 1.0)
    inp['c'] = nrm((BATCH, D), 1.0)
    inp['ctx'] = nrm((BATCH, CTX_LEN, D), 1.0)
    inp['c_ctx'] = nrm((D,), 1.0)
    inp['l0_mod_w'] = nrm((D, 6 * D), 0.5 * D ** -0.5)
    inp['l0_mod_b'] = nrm((6 * D,), 0.02)
    inp['l0_mix_pre_g'] = gain(D)
    inp['l0_mix_post_g'] = gain(D)
    inp['l0_w_in'] = nrm((D, IN0_WIDTH), D ** -0.5)
    gate_offsets = jnp.concatenate([
        jnp.full((MLSTM_HEADS,), -1.0, F32), jnp.linspace(3.0, 6.0, MLSTM_HEADS, dtype=F32),
        jnp.full((MLSTM_HEADS,), -1.0, F32), jnp.linspace(3.0, 6.0, MLSTM_HEADS, dtype=F32)])
    inp['l0_mlstm_gate_b'] = gate_offsets + nrm((MG,), 0.1)
    inp['l0_mlstm_conv_w'] = nrm((MLSTM_CONV, 2 * MQK), MLSTM_CONV ** -0.5)
    inp['l0_mlstm_norm_g'] = gain(MV)
    inp['l0_lambda_q1'] = nrm((DIFF_DQK,), 0.1)
    inp['l0_lambda_k1'] = nrm((DIFF_DQK,), 0.1)
    inp['l0_lambda_q2'] = nrm((DIFF_DQK,), 0.1)
    inp['l0_lambda_k2'] = nrm((DIFF_DQK,), 0.1)
    inp['l0_diff_norm_g'] = gain(DVW)
    inp['l0_w_out'] = nrm((MIX0_WIDTH, D), MIX0_WIDTH ** -0.5)
    inp['l0_ffn_pre_g'] = gain(D)
    inp['l0_ffn_post_g'] = gain(D)
    inp['l0_ffn_w1'] = nrm((D, D_FF), D ** -0.5)
    inp['l0_ffn_w3'] = nrm((D, D_FF), D ** -0.5)
    inp['l0_ffn_w2'] = nrm((D_FF, D), D_FF ** -0.5)
    inp['l1_mod_w'] = nrm((D, 6 * D), 0.5 * D ** -0.5)
    inp['l1_mod_b'] = nrm((6 * D,), 0.02)
    inp['l1_mix_pre_g'] = gain(D)
    inp['l1_mix_post_g'] = gain(D)
    inp['l1_conv_pw1_w'] = nrm((D, 2 * D), D ** -0.5)
    inp['l1_conv_pw1_b'] = nrm((2 * D,), 0.02)
    inp['l1_conv_dw_w'] = nrm((CONV_WIDTH, D), CONV_WIDTH ** -0.5)
    inp['l1_conv_dw_b'] = nrm((D,), 0.02)
    inp['l1_conv_ln_g'] = gain(D)
    inp['l1_conv_ln_b'] = nrm((D,), 0.02)
    inp['l1_conv_pw2_w'] = nrm((D, D), D ** -0.5)
    inp['l1_conv_pw2_b'] = nrm((D,), 0.02)
    inp['l1_ffn_pre_g'] = gain(D)
    inp['l1_ffn_post_g'] = gain(D)
    inp['l1_router_w'] = nrm((D, N_EXPERTS), D ** -0.5)
    inp['l1_moe_w1'] = nrm((N_EXPERTS, D, D_FF_EXPERT), D ** -0.5)
    inp['l1_moe_w3'] = nrm((N_EXPERTS, D, D_FF_EXPERT), D ** -0.5)
    inp['l1_moe_w2'] = nrm((N_EXPERTS, D_FF_EXPERT, D), D_FF_EXPERT ** -0.5)
    return inp


def reference(x, c, ctx, c_ctx,
              l0_mod_w, l0_mod_b, l0_mix_pre_g, l0_mix_post_g, l0_w_in, l0_mlstm_gate_b,
              l0_mlstm_conv_w, l0_mlstm_norm_g, l0_lambda_q1, l0_lambda_k1, l0_lambda_q2,
              l0_lambda_k2, l0_diff_norm_g, l0_w_out, l0_ffn_pre_g, l0_ffn_post_g,
              l0_ffn_w1, l0_ffn_w3, l0_ffn_w2,
              l1_mod_w, l1_mod_b, l1_mix_pre_g, l1_mix_post_g, l1_conv_pw1_w, l1_conv_pw1_b,
              l1_conv_dw_w, l1_conv_dw_b, l1_conv_ln_g, l1_conv_ln_b, l1_conv_pw2_w,
              l1_conv_pw2_b, l1_ffn_pre_g, l1_ffn_post_g, l1_router_w, l1_moe_w1, l1_moe_w3,
              l1_moe_w2):
    layer_params = (
        (l0_mod_w, l0_mod_b, l0_mix_pre_g, l0_mix_post_g, l0_w_in, l0_mlstm_gate_b,
         l0_mlstm_conv_w, l0_mlstm_norm_g, l0_lambda_q1, l0_lambda_k1, l0_lambda_q2,
         l0_lambda_k2, l0_diff_norm_g, l0_w_out, l0_ffn_pre_g, l0_ffn_post_g,
         l0_ffn_w1, l0_ffn_w3, l0_ffn_w2),
        (l1_mod_w, l1_mod_b, l1_mix_pre_g, l1_mix_post_g, l1_conv_pw1_w, l1_conv_pw1_b,
         l1_conv_dw_w, l1_conv_dw_b, l1_conv_ln_g, l1_conv_ln_b, l1_conv_pw2_w,
         l1_conv_pw2_b, l1_ffn_pre_g, l1_ffn_post_g, l1_router_w, l1_moe_w1, l1_moe_w3,
         l1_moe_w2),
    )
    for layer in range(DEPTH):
        if layer % 2 == 0:
            x = even_layer(x, c, ctx, c_ctx, layer, *layer_params[layer])
        else:
            x = odd_layer(x, c, *layer_params[layer])
    return x
```

```python
import numpy as np
from contextlib import ExitStack
import concourse.bass as bass
import concourse.mybir as mybir
from concourse.bass_utils import run_bass_kernel_spmd

F32 = mybir.dt.float32
BF16 = mybir.dt.bfloat16
AF = mybir.ActivationFunctionType
ALU = mybir.AluOpType
AX = mybir.AxisListType

D = 1024
DFF = 2816
NEXP = 8
CTX = 256
EPS = 1e-6
IN0 = 3616
NCH_FF = DFF // 128


class Buf:
    __slots__ = ("name", "w", "r")

    def __init__(self, name):
        self.name = name
        self.w = None
        self.r = []


class Tile:
    def __init__(self, t, name):
        self.t = t
        self.b = Buf(name)

    def __getitem__(self, k):
        return self.t[k]


class _PEProxy:
    def __init__(self, eng):
        self.eng = eng
        self.stop = True

    def matmul(self, *a, **kw):
        self.stop = kw.get("stop", True) is not False
        return self.eng.matmul(*a, **kw)

    def transpose(self, *a, **kw):
        return self.eng.transpose(*a, **kw)


class Sched:
    EPOCH = 20000
    NPOOL = 24
    STRICT = True

    def __init__(self, nc, es):
        self.nc = nc
        self.es = es
        self.engs = {"pe": nc.tensor, "act": nc.scalar, "dve": nc.vector, "pool": nc.gpsimd, "sp": nc.sync}
        self.cnt = {e: 0 for e in self.engs}
        self.pend = {e: [] for e in self.engs}
        self.sems = {}
        self.seen = {e: {} for e in self.engs}
        self.dma_pool = {}
        self.dma_idx = {q: 0 for q in self.engs}
        self.n_inst = {e: 0 for e in self.engs}
        self.n_wait = 0

    def _eng_sem(self, e, epoch):
        k = (e, epoch)
        if k not in self.sems:
            self.sems[k] = self.es.enter_context(self.nc.semaphore("s_%s_%d" % (e, epoch)))
        return self.sems[k]

    def _sem_of(self, key):
        if key[0] == "dma":
            return self.dma_pool[key[1]][key[2]][0]
        return self._eng_sem(key[0], key[1])

    def _wait(self, e, dep):
        key, val = dep
        if self.seen[e].get(key, 0) >= val:
            return
        self.engs[e].wait_ge(self._sem_of(key), val)
        self.seen[e][key] = val
        self.n_wait += 1

    def _need(self, e, d):
        if d[0][0] != e:
            return True
        if not self.STRICT:
            return False
        c = self.cnt[e]
        epoch, idx = divmod(c, self.EPOCH)
        return not (d[0][1] == epoch and d[1] > idx)

    def _deps(self, e, reads, writes):
        for b in reads:
            if b.w is not None:
                self._wait(e, b.w)
        for b in writes:
            if b.w is not None and self._need(e, b.w):
                self._wait(e, b.w)
            for d in b.r:
                if self._need(e, d):
                    self._wait(e, d)

    @staticmethod
    def _record(stamp, reads, writes):
        for b in reads:
            b.r.append(stamp)
        for b in writes:
            b.w = stamp
            b.r = []

    def op(self, e, fn, reads=(), writes=(), sig=None):
        reads = [x.b if isinstance(x, Tile) else x for x in reads]
        writes = [x.b if isinstance(x, Tile) else x for x in writes]
        self._deps(e, reads, writes)
        if e == "pe":
            px = _PEProxy(self.engs[e])
            inst = fn(px)
            if sig is None:
                sig = px.stop
        else:
            inst = fn(self.engs[e])
            if sig is None:
                sig = True
        self.n_inst[e] += 1
        c = self.cnt[e]
        epoch, idx = divmod(c, self.EPOCH)
        stamp = ((e, epoch), idx + 1)
        self._record(stamp, reads, writes)
        if sig:
            inst.then_inc(self._eng_sem(e, epoch), 1)
            self.cnt[e] = c + 1
        return inst

    def dma(self, q, out, in_, reads=(), writes=(), **kw):
        reads = [x.b if isinstance(x, Tile) else x for x in reads]
        writes = [x.b if isinstance(x, Tile) else x for x in writes]
        pool = self.dma_pool.setdefault(q, [])
        i = self.dma_idx[q]
        slot = i % self.NPOOL
        if slot >= len(pool):
            pool.append([self.es.enter_context(self.nc.semaphore("d_%s_%d" % (q, slot))), 0])
        sem, uses = pool[slot]
        if uses > 0:
            self._wait(q, (("dma", q, slot), 16 * uses))
        self._deps(q, reads, writes)
        inst = self.engs[q].dma_start(out=out, in_=in_, **kw)
        inst.then_inc(sem, 16)
        pool[slot][1] = uses + 1
        self.dma_idx[q] = i + 1
        self.n_inst[q] += 1
        self._record((("dma", q, slot), 16 * (uses + 1)), reads, writes)
        return inst

    def idma(self, out, out_off, in_, in_off, reads=(), writes=()):
        q = "pool"
        reads = [x.b if isinstance(x, Tile) else x for x in reads]
        writes = [x.b if isinstance(x, Tile) else x for x in writes]
        pool = self.dma_pool.setdefault(q, [])
        i = self.dma_idx[q]
        slot = i % self.NPOOL
        if slot >= len(pool):
            pool.append([self.es.enter_context(self.nc.semaphore("d_%s_%d" % (q, slot))), 0])
        sem, uses = pool[slot]
        if uses > 0:
            self._wait(q, (("dma", q, slot), 16 * uses))
        self._deps(q, reads, writes)
        inst = self.engs[q].indirect_dma_start(out=out, out_offset=out_off, in_=in_, in_offset=in_off)
        inst.then_inc(sem, 16)
        pool[slot][1] = uses + 1
        self.dma_idx[q] = i + 1
        self.n_inst[q] += 1
        self._record((("dma", q, slot), 16 * (uses + 1)), reads, writes)
        return inst

    def barrier(self, queues=("sp",)):
        stamps = []
        for e in ("pe", "act", "dve", "pool"):
            c = self.cnt[e]
            if c > 0:
                epoch, idx = divmod(c - 1, self.EPOCH)
                stamps.append(((e, epoch), idx + 1))
        for q in queues:
            for slot, (sem, uses) in enumerate(self.dma_pool.get(q, [])):
                if uses > 0:
                    stamps.append((("dma", q, slot), 16 * uses))
        for e in ("pe", "act", "dve", "pool", "sp"):
            for st in stamps:
                if st[0][0] == e:
                    continue
                self._wait(e, st)

    def finish(self, bufs, e="sp"):
        for b in bufs:
            b = b.b if isinstance(b, Tile) else b
            if b.w is not None:
                self._wait(e, b.w)


class K:
    def __init__(self, T, layers=(0, 1), debug=False):
        self.T = T
        self.layers = layers
        self.debug = debug
        self.nc = bass.Bass("TRN2", target_bir_lowering=False)
        self.inp = {}
        self.dbg_out = []

    def din(self, name, shape):
        t = self.nc.dram_tensor(name, list(shape), F32, kind="ExternalInput").ap()
        self.inp[name] = t
        return t

    def dscr(self, name, shape, dt, dbg=False):
        kind = "ExternalOutput" if (dbg and self.debug) else "Internal"
        t = self.nc.dram_tensor(name, list(shape), dt, kind=kind).ap()
        if dbg and self.debug:
            self.dbg_out.append(name)
        return t

    def _uniq(self, name):
        self._nid = getattr(self, "_nid", 0) + 1
        return "%s_%d" % (name, self._nid)

    def sb(self, es, name, shape, dt):
        name = self._uniq(name)
        return Tile(es.enter_context(self.nc.sbuf_tensor(name, list(shape), dt)), name)

    def ps(self, es, name, shape, dt):
        name = self._uniq(name)
        full = es.enter_context(self.nc.psum_tensor(name, [128, 512], F32))
        ap = full[:]
        n = int(np.prod(shape[1:]))
        if dt == BF16:
            ap = ap.bitcast(BF16)
            assert n <= 1024
        else:
            assert n <= 512
        ap = ap[0:shape[0], 0:n]
        if len(shape) == 3:
            ap = ap.rearrange("p (a b) -> p a b", b=shape[2])
        return Tile(ap, name)

    def scope(self):
        k = self

        class _Scope(ExitStack):
            def __exit__(self, *a):
                if a[0] is None:
                    k.S.barrier()
                return super().__exit__(*a)
        return _Scope()

    def convert(self, src, name, rows, cols, rb=128):
        dst = self.nc.dram_tensor(name, [rows, cols], BF16, kind="Internal").ap()
        bufs = []
        for r0 in range(0, rows, rb):
            b = Buf("%s_%d" % (name, r0))
            self.S.dma("pool", dst[r0:r0 + rb, :], src[r0:r0 + rb, :], writes=[b])
            bufs.append(b)
        return dst, bufs

    def rstd_cols(self, ss, n, scale):
        S = self.S
        S.op("act", lambda e: e.activation(out=ss[:, 0:n], in_=ss[:, 0:n], func=AF.Ln, scale=scale, bias=self.epsc[:, 0:1]),
             reads=[ss, self.epsc], writes=[ss])
        S.op("act", lambda e: e.activation(out=ss[:, 0:n], in_=ss[:, 0:n], func=AF.Exp, scale=-0.5), reads=[ss], writes=[ss])

    def norm_mod_T(self, xt, gmod, shift, uT_dst, col0, ss, tmp, u, pT, junk):
        S = self.S
        self._nm = getattr(self, "_nm", 0) + 1
        pick = lambda b: b[self._nm % len(b)] if isinstance(b, list) else b
        ss, tmp, u, pT, junk = pick(ss), pick(tmp), pick(u), pick(pT), pick(junk)
        S.op("act", lambda e: e.activation(out=junk[:], in_=xt[:], func=AF.Square, accum_out=ss[:, 0:1]),
             reads=[xt], writes=[junk, ss])
        self.rstd_cols(ss, 1, 1.0 / D)
        S.op("dve", lambda e: e.scalar_tensor_tensor(out=tmp[:], in0=xt[:], scalar=ss[:, 0:1], in1=gmod[:], op0=ALU.mult, op1=ALU.mult),
             reads=[xt, ss, gmod], writes=[tmp])
        S.op("dve", lambda e: e.tensor_tensor(out=u[:], in0=tmp[:], in1=shift[:], op=ALU.add), reads=[tmp, shift], writes=[u])
        for c in range(8):
            S.op("pe", lambda e: e.transpose(out=pT[:, c, :], in_=u[:, c * 128:(c + 1) * 128], identity=self.identb[:]),
                 reads=[u, self.identb], writes=[pT], sig=(c == 7))
        S.op("act", lambda e: e.copy(out=uT_dst[:, :, col0:col0 + 128], in_=pT[:]), reads=[pT], writes=[uT_dst])
        return u

    def post_norm_residual(self, y, xt, ggt, ss, junk, xo):
        S = self.S
        S.op("act", lambda e: e.activation(out=junk[:], in_=y[:], func=AF.Square, accum_out=ss[:, 0:1]), reads=[y], writes=[junk, ss])
        self.rstd_cols(ss, 1, 1.0 / D)
        S.op("dve", lambda e: e.scalar_tensor_tensor(out=y[:], in0=y[:], scalar=ss[:, 0:1], in1=ggt[:], op0=ALU.mult, op1=ALU.mult),
             reads=[y, ss, ggt], writes=[y])
        S.op("dve", lambda e: e.tensor_tensor(out=xo[:], in0=y[:], in1=xt[:], op=ALU.add), reads=[y, xt], writes=[xo])

    def adaln(self, es, cvec, mod_w, mod_b, gains, want):
        S, nc = self.S, self.nc
        self._adn = getattr(self, "_adn", 0) + 1
        sfx = "_%d" % self._adn
        out = {}
        for (name, idx, kind, gain) in want:
            out[name] = self.sb(es, "bc_" + name + sfx, [128, D], F32)
        with self.scope() as les:
            cT = self.sb(les, "ad_cT" + sfx, [128, 8], F32)
            sc = self.sb(les, "ad_sc" + sfx, [128, 8], F32)
            row = self.sb(les, "ad_row" + sfx, [1, 6 * D], F32)
            brow = self.sb(les, "ad_brow" + sfx, [1, 6 * D], F32)
            wbuf = [self.sb(les, "ad_w%d" % i + sfx, [128, 8, 512], F32) for i in range(2)]
            gt = self.sb(les, "ad_g" + sfx, [128, D], F32)
            pr = self.ps(les, "ad_pr" + sfx, [1, 512], F32)
            pb = self.ps(les, "ad_pb" + sfx, [128, 512], F32)
            S.dma("sp", cT[:], cvec.rearrange("(c p) -> p c", p=128), writes=[cT], allow_slow_non_contiguous=True)
            S.dma("sp", brow[:], mod_b.unsqueeze(0), writes=[brow])
            S.op("act", lambda e: e.activation(out=sc[:], in_=cT[:], func=AF.Silu), reads=[cT], writes=[sc])
            need = sorted(set(i for (_, i, _, _) in want))
            for gi in range(12):
                if gi // 2 not in need:
                    continue
                wb = wbuf[gi % 2]
                S.dma("sp", wb[:], mod_w.rearrange("(c p) n -> p c n", p=128)[:, :, gi * 512:(gi + 1) * 512], writes=[wb])
                for c in range(8):
                    S.op("pe", lambda e: e.matmul(pr[:], lhsT=sc[:, c:c + 1], rhs=wb[:, c, :], start=(c == 0), stop=(c == 7)),
                         reads=[sc, wb], writes=[pr])
                S.op("dve", lambda e: e.tensor_tensor(out=row[:, gi * 512:(gi + 1) * 512], in0=pr[:], in1=brow[:, gi * 512:(gi + 1) * 512], op=ALU.add),
                     reads=[pr, brow], writes=[row])
            for (name, idx, kind, gain) in want:
                dst = out[name]
                if gain is not None:
                    S.dma("sp", gt[:], gain.partition_broadcast(128), writes=[gt])
                for h in range(2):
                    S.op("pe", lambda e: e.matmul(pb[:], lhsT=self.ones1[:], rhs=row[:, idx * D + h * 512: idx * D + (h + 1) * 512], start=True, stop=True),
                         reads=[self.ones1, row], writes=[pb])
                    sl = slice(h * 512, (h + 1) * 512)
                    if kind == "gmod":
                        S.op("dve", lambda e: e.scalar_tensor_tensor(out=dst[:, sl], in0=pb[:], scalar=1.0, in1=gt[:, sl], op0=ALU.add, op1=ALU.mult),
                             reads=[pb, gt], writes=[dst])
                    elif kind == "shift":
                        S.op("dve", lambda e: e.tensor_copy(out=dst[:, sl], in_=pb[:]), reads=[pb], writes=[dst])
                    else:
                        S.op("dve", lambda e: e.tensor_tensor(out=dst[:, sl], in0=pb[:], in1=gt[:, sl], op=ALU.mult), reads=[pb, gt], writes=[dst])
        return out

    def ffn_phase(self, pfx, xin, xin_bufs, xout, xout_bufs, bc, w1, w1b, w3, w3b, w2, w2b, nexp, router=None):
        S, nc, T = self.S, self.nc, self.T
        TS = min(2048, T)
        NT = TS // 128
        FG = 4
        grp = [(g0, min(FG, NCH_FF - g0)) for g0 in range(0, NCH_FF, FG)]
        with self.scope() as es:
            xt = [self.sb(es, pfx + "xt%d" % i, [128, D], F32) for i in range(2)]
            tmp = self.sb(es, pfx + "tmp", [128, D], F32)
            junk = self.sb(es, pfx + "junk", [128, D], BF16)
            u = [self.sb(es, pfx + "u%d" % i, [128, D], BF16) for i in range(2)]
            ss = self.sb(es, pfx + "ss", [128, 4], F32)
            ssn = [self.sb(es, pfx + "ssn%d" % i, [128, 4], F32) for i in range(2)]
            uT = self.sb(es, pfx + "uT", [128, 8, TS], BF16)
            yacc = self.sb(es, pfx + "yacc", [128, NT, D], F32)
            w1g = [self.sb(es, pfx + "w1g%d" % i, [128, 8, FG * 128], BF16) for i in range(2)]
            w3g = [self.sb(es, pfx + "w3g%d" % i, [128, 8, FG * 128], BF16) for i in range(2)]
            w2g = [self.sb(es, pfx + "w2g%d" % i, [128, FG, D], BF16) for i in range(2)]
            hT = [self.sb(es, pfx + "hT%d" % i, [128, FG, 512], BF16) for i in range(2)]
            sl_ = [self.sb(es, pfx + "sl%d" % i, [128, 512], F32) for i in range(2)]
            xos = [self.sb(es, pfx + "xo%d" % i, [128, D], F32) for i in range(2)]
            pT = self.ps(es, pfx + "pT", [128, 8, 128], BF16)
            p1 = [self.ps(es, pfx + "p1_%d" % i, [128, 512], F32) for i in range(2)]
            p3 = [self.ps(es, pfx + "p3_%d" % i, [128, 512], F32) for i in range(2)]
            py = [self.ps(es, pfx + "py_%d" % i, [128, 512], F32) for i in range(2)]
            if router is not None:
                rw = self.sb(es, pfx + "rw", [128, 8, NEXP], BF16)
                S.dma("pool", rw[:], router.rearrange("(c p) n -> p c n", p=128), writes=[rw])
                gates = self.sb(es, pfx + "gates", [128, NT, NEXP], F32)
                lg = self.sb(es, pfx + "lg", [128, NEXP], F32)
                l2 = self.sb(es, pfx + "l2", [128, NEXP], F32)
                mk = self.sb(es, pfx + "mk", [128, NEXP], F32)
                m1 = self.sb(es, pfx + "m1", [128, 4], F32)
                pl = self.ps(es, pfx + "pl", [128, NEXP], F32)
            gi = 0
            cnt_s1 = 0
            cnt_y = 0
            kh = 0
            for st in range(T // TS):
                for tt in range(NT):
                    tile_i = st * NT + tt
                    x_ = xt[tt % 2]
                    S.dma("sp", x_[:], xin[tile_i * 128:(tile_i + 1) * 128, :], reads=[xin_bufs[tile_i]], writes=[x_])
                    self.norm_mod_T(x_, bc["gmod_f"], bc["shift_f"], uT, tt * 128, ssn, tmp, u, pT, junk)
                    if router is not None:
                        for c in range(8):
                            S.op("pe", lambda e: e.matmul(pl[:], lhsT=uT[:, c, tt * 128:(tt + 1) * 128], rhs=rw[:, c, :], start=(c == 0), stop=(c == 7)),
                                 reads=[uT, rw], writes=[pl])
                        S.op("dve", lambda e: e.tensor_copy(out=lg[:], in_=pl[:]), reads=[pl], writes=[lg])
                        S.op("dve", lambda e: e.reduce_max(out=m1[:, 0:1], in_=lg[:], axis=AX.X), reads=[lg], writes=[m1])
                        S.op("dve", lambda e: e.tensor_scalar(out=mk[:], in0=lg[:], scalar1=m1[:, 0:1], scalar2=-1e30, op0=ALU.is_ge, op1=ALU.mult),
                             reads=[lg, m1], writes=[mk])
                        S.op("dve", lambda e: e.tensor_tensor(out=l2[:], in0=lg[:], in1=mk[:], op=ALU.add), reads=[lg, mk], writes=[l2])
                        S.op("dve", lambda e: e.reduce_max(out=m1[:, 1:2], in_=l2[:], axis=AX.X), reads=[l2], writes=[m1])
                        S.op("dve", lambda e: e.tensor_scalar(out=mk[:], in0=lg[:], scalar1=m1[:, 1:2], scalar2=None, op0=ALU.is_ge),
                             reads=[lg, m1], writes=[mk])
                        S.op("dve", lambda e: e.tensor_scalar(out=m1[:, 2:3], in0=m1[:, 0:1], scalar1=-1.0, scalar2=None, op0=ALU.mult),
                             reads=[m1], writes=[m1])
                        S.op("act", lambda e: e.activation(out=l2[:], in_=lg[:], func=AF.Exp, bias=m1[:, 2:3]), reads=[lg, m1], writes=[l2])
                        S.op("dve", lambda e: e.tensor_tensor(out=l2[:], in0=l2[:], in1=mk[:], op=ALU.mult), reads=[l2, mk], writes=[l2])
                        S.op("dve", lambda e: e.reduce_sum(out=m1[:, 3:4], in_=l2[:], axis=AX.X), reads=[l2], writes=[m1])
                        S.op("dve", lambda e: e.reciprocal(out=m1[:, 3:4], in_=m1[:, 3:4]), reads=[m1], writes=[m1])
                        S.op("dve", lambda e: e.tensor_scalar(out=gates[:, tt, :], in0=l2[:], scalar1=m1[:, 3:4], scalar2=None, op0=ALU.mult),
                             reads=[l2, m1], writes=[gates])
                for ex in range(nexp):
                    for g, (g0, nv) in enumerate(grp):
                        f0 = g0 * 128
                        wa, wb_, wc = w1g[gi % 2], w3g[gi % 2], w2g[gi % 2]
                        gi += 1
                        rb = [w1b[(ex * D) // 128 + c] for c in range(8)]
                        S.dma("sp", wa[:, :, 0:nv * 128], w1[ex * D:(ex + 1) * D, f0:f0 + nv * 128].rearrange("(c p) f -> p c f", p=128), reads=rb, writes=[wa])
                        rb = [w3b[(ex * D) // 128 + c] for c in range(8)]
                        S.dma("sp", wb_[:, :, 0:nv * 128], w3[ex * D:(ex + 1) * D, f0:f0 + nv * 128].rearrange("(c p) f -> p c f", p=128), reads=rb, writes=[wb_])
                        r0 = ex * DFF + f0
                        rb = [w2b[(r0 // 128) + c] for c in range(nv)]
                        S.dma("sp", wc[:, 0:nv, :], w2[r0:r0 + nv * 128, :].rearrange("(c p) d -> p c d", p=128), reads=rb, writes=[wc])
                        for sub in range(TS // 512):
                            h_ = hT[kh % 2]
                            kh += 1
                            for j in range(nv):
                                a1, a3, s_ = p1[cnt_s1 % 2], p3[cnt_s1 % 2], sl_[cnt_s1 % 2]
                                cnt_s1 += 1
                                for c in range(8):
                                    S.op("pe", lambda e: e.matmul(a1[:], lhsT=wa[:, c, j * 128:(j + 1) * 128], rhs=uT[:, c, sub * 512:(sub + 1) * 512],
                                                                  start=(c == 0), stop=(c == 7)), reads=[wa, uT], writes=[a1])
                                for c in range(8):
                                    S.op("pe", lambda e: e.matmul(a3[:], lhsT=wb_[:, c, j * 128:(j + 1) * 128], rhs=uT[:, c, sub * 512:(sub + 1) * 512],
                                                                  start=(c == 0), stop=(c == 7)), reads=[wb_, uT], writes=[a3])
                                S.op("act", lambda e: e.activation(out=s_[:], in_=a1[:], func=AF.Silu), reads=[a1], writes=[s_])
                                S.op("dve", lambda e: e.tensor_tensor(out=h_[:, j, :], in0=s_[:], in1=a3[:], op=ALU.mult), reads=[s_, a3], writes=[h_])
                            for t4 in range(4):
                                tt = sub * 4 + t4
                                for dg in range(2):
                                    y_ = py[cnt_y % 2]
                                    cnt_y += 1
                                    for j in range(nv):
                                        S.op("pe", lambda e: e.matmul(y_[:], lhsT=h_[:, j, t4 * 128:(t4 + 1) * 128], rhs=wc[:, j, dg * 512:(dg + 1) * 512],
                                                                      start=(j == 0), stop=(j == nv - 1)), reads=[h_, wc], writes=[y_])
                                    ya = yacc[:, tt, dg * 512:(dg + 1) * 512]
                                    first = (ex == 0 and g == 0)
                                    if router is None:
                                        if first:
                                            S.op("dve", lambda e: e.tensor_copy(out=ya, in_=y_[:]), reads=[y_], writes=[yacc])
                                        else:
                                            S.op("dve", lambda e: e.tensor_tensor(out=ya, in0=y_[:], in1=ya, op=ALU.add), reads=[y_, yacc], writes=[yacc])
                                    else:
                                        gs = gates[:, tt, ex:ex + 1]
                                        if first:
                                            S.op("dve", lambda e: e.tensor_scalar(out=ya, in0=y_[:], scalar1=gs, scalar2=None, op0=ALU.mult),
                                                 reads=[y_, gates], writes=[yacc])
                                        else:
                                            S.op("dve", lambda e: e.scalar_tensor_tensor(out=ya, in0=y_[:], scalar=gs, in1=ya, op0=ALU.mult, op1=ALU.add),
                                                 reads=[y_, gates, yacc], writes=[yacc])
                if self.debug and st == 0:
                    dg_ = self.dscr(pfx + "dbg_yacc", [128, NT, D], F32, dbg=True)
                    S.dma("sp", dg_[:, :, :], yacc[:], reads=[yacc], writes=[Buf("dbgy")])
                    du_ = self.dscr(pfx + "dbg_uT", [128, 8, TS], BF16, dbg=True)
                    S.dma("sp", du_[:, :, :], uT[:], reads=[uT], writes=[Buf("dbgu")])
                    if router is not None:
                        dgt_ = self.dscr(pfx + "dbg_gates", [128, NT, NEXP], F32, dbg=True)
                        S.dma("sp", dgt_[:, :, :], gates[:], reads=[gates], writes=[Buf("dbgg")])
                for tt in range(NT):
                    tile_i = st * NT + tt
                    x_ = xt[tt % 2]
                    xo = xos[tt % 2]
                    ss = ssn[tt % 2]
                    S.dma("sp", x_[:], xin[tile_i * 128:(tile_i + 1) * 128, :], reads=[xin_bufs[tile_i]], writes=[x_])
                    S.op("act", lambda e: e.activation(out=junk[:], in_=yacc[:, tt, :], func=AF.Square, accum_out=ss[:, 0:1]),
                         reads=[yacc], writes=[junk, ss])
                    self.rstd_cols(ss, 1, 1.0 / D)
                    S.op("dve", lambda e: e.scalar_tensor_tensor(out=tmp[:], in0=yacc[:, tt, :], scalar=ss[:, 0:1], in1=bc["ggt_f"][:], op0=ALU.mult, op1=ALU.mult),
                         reads=[yacc, ss, bc["ggt_f"]], writes=[tmp])
                    S.op("dve", lambda e: e.tensor_tensor(out=xo[:], in0=tmp[:], in1=x_[:], op=ALU.add), reads=[tmp, x_], writes=[xo])
                    S.dma("sp", xout[tile_i * 128:(tile_i + 1) * 128, :], xo[:], reads=[xo], writes=[xout_bufs[tile_i]])


    def moe_phase(self, xin, xin_bufs, xout, xout_bufs, bc, W):
        S, nc, T = self.S, self.nc, self.T
        I = self.inp
        I32 = mybir.dt.int32
        NTL = T // 128
        NSB = (2 * T) // 512 + NEXP
        G, FW, NJ = 6, 512, 4
        nval = [4, 4, 4, 4, 4, 2]
        NE = NEXP * NTL
        assert NE <= 512
        U2 = self.dscr("m_U2", [T, D], BF16)
        XS = self.dscr("m_XS", [NSB * 512, D], BF16)
        YS = self.dscr("m_YS", [NSB * 512, D], F32)
        u2_b = [Buf("u2_%d" % i) for i in range(NTL)]
        xs_b = [Buf("xs_%d" % i) for i in range(2 * NTL)]
        ys_b = [Buf("ys_%d" % i) for i in range(NSB)]
        with self.scope() as es:
            gatesE = self.sb(es, "m_gE", [128, NEXP, NTL], F32)
            sl_i = self.sb(es, "m_sli", [128, 2, NTL], I32)
            g12 = self.sb(es, "m_g12", [128, 2, NTL], F32)
            idxw = self.sb(es, "m_idxw", [128, NSB, G], I32)
            with self.scope() as p1:
                rw = self.sb(p1, "m_rw", [128, 8, NEXP], BF16)
                S.dma("pool", rw[:], I["l1_router_w"].rearrange("(c p) n -> p c n", p=128), writes=[rw])
                xt = [self.sb(p1, "m_xt%d" % i, [128, D], F32) for i in range(2)]
                tmp = [self.sb(p1, "m_tmp%d" % i, [128, D], F32) for i in range(2)]
                junk = self.sb(p1, "m_junk", [128, D], BF16)
                u = [self.sb(p1, "m_u%d" % i, [128, D], BF16) for i in range(2)]
                ss = [self.sb(p1, "m_ss%d" % i, [128, 4], F32) for i in range(2)]
                uT = [self.sb(p1, "m_uT%d" % i, [128, 8, 128], BF16) for i in range(2)]
                lg = self.sb(p1, "m_lg", [128, NEXP], F32)
                l2 = self.sb(p1, "m_l2", [128, NEXP], F32)
                mk = self.sb(p1, "m_mk", [128, NEXP], F32)
                m1 = self.sb(p1, "m_m1", [128, 4], F32)
                pT = [self.ps(p1, "m_pT%d" % i, [128, 8, 128], BF16) for i in range(2)]
                pl = self.ps(p1, "m_pl", [128, NEXP], F32)
                def front(tt):
                    x_, uT_ = xt[tt % 2], uT[tt % 2]
                    S.dma("sp", x_[:], xin[tt * 128:(tt + 1) * 128, :], reads=[xin_bufs[tt]], writes=[x_])
                    u_ = self.norm_mod_T(x_, bc["gmod_f"], bc["shift_f"], uT_, 0, ss, tmp, u, pT, junk)
                    S.dma("sp", U2[tt * 128:(tt + 1) * 128, :], u_[:], reads=[u_], writes=[u2_b[tt]])

                front(0)
                for tt in range(NTL):
                    uT_ = uT[tt % 2]
                    if tt + 1 < NTL:
                        front(tt + 1)
                    for c in range(8):
                        S.op("pe", lambda e: e.matmul(pl[:], lhsT=uT_[:, c, :], rhs=rw[:, c, :], start=(c == 0), stop=(c == 7)), reads=[uT_, rw], writes=[pl])
                    S.op("dve", lambda e: e.tensor_copy(out=lg[:], in_=pl[:]), reads=[pl], writes=[lg])
                    S.op("dve", lambda e: e.reduce_max(out=m1[:, 0:1], in_=lg[:], axis=AX.X), reads=[lg], writes=[m1])
                    S.op("dve", lambda e: e.tensor_scalar(out=mk[:], in0=lg[:], scalar1=m1[:, 0:1], scalar2=-1e30, op0=ALU.is_ge, op1=ALU.mult), reads=[lg, m1], writes=[mk])
                    S.op("dve", lambda e: e.tensor_tensor(out=l2[:], in0=lg[:], in1=mk[:], op=ALU.add), reads=[lg, mk], writes=[l2])
                    S.op("dve", lambda e: e.reduce_max(out=m1[:, 1:2], in_=l2[:], axis=AX.X), reads=[l2], writes=[m1])
                    S.op("dve", lambda e: e.tensor_scalar(out=mk[:], in0=lg[:], scalar1=m1[:, 1:2], scalar2=None, op0=ALU.is_ge), reads=[lg, m1], writes=[mk])
                    S.op("dve", lambda e: e.tensor_scalar(out=m1[:, 2:3], in0=m1[:, 0:1], scalar1=-1.0, scalar2=None, op0=ALU.mult), reads=[m1], writes=[m1])
                    S.op("act", lambda e: e.activation(out=l2[:], in_=lg[:], func=AF.Exp, bias=m1[:, 2:3]), reads=[lg, m1], writes=[l2])
                    S.op("dve", lambda e: e.tensor_tensor(out=l2[:], in0=l2[:], in1=mk[:], op=ALU.mult), reads=[l2, mk], writes=[l2])
                    S.op("dve", lambda e: e.reduce_sum(out=m1[:, 3:4], in_=l2[:], axis=AX.X), reads=[l2], writes=[m1])
                    S.op("dve", lambda e: e.reciprocal(out=m1[:, 3:4], in_=m1[:, 3:4]), reads=[m1], writes=[m1])
                    S.op("dve", lambda e: e.tensor_scalar(out=gatesE[:, :, tt], in0=l2[:], scalar1=m1[:, 3:4], scalar2=None, op0=ALU.mult), reads=[l2, m1], writes=[gatesE])
            with self.scope() as p2:
                f3 = lambda n: self.sb(p2, n, [128, NEXP, NTL], F32)
                sel, Wt, Ct, Ic, S3, cs, mm, pr = f3("m_sel"), f3("m_Wt"), f3("m_Ct"), f3("m_Ic"), f3("m_S3"), f3("m_cs"), f3("m_mm"), f3("m_pr")
                selb = self.sb(p2, "m_selb", [128, NE], BF16)
                tri = self.sb(p2, "m_tri", [128, 128], BF16)
                onek = self.sb(p2, "m_onek", [128, 128], BF16)
                one1 = self.sb(p2, "m_one1", [128, 1], F32)
                cnt = self.sb(p2, "m_cnt", [128, NEXP], F32)
                pad = self.sb(p2, "m_pad", [128, NEXP], F32)
                pend = self.sb(p2, "m_pend", [128, NEXP], F32)
                pst = self.sb(p2, "m_pst", [128, NEXP], F32)
                sbpos = self.sb(p2, "m_sbpos", [128, NSB], F32)
                cmp_ = self.sb(p2, "m_cmp", [128, NSB, NEXP], F32)
                esb = self.sb(p2, "m_esb", [128, NSB], F32)
                wbase = self.sb(p2, "m_wbase", [128, G], F32)
                idxf = self.sb(p2, "m_idxf", [128, NSB, G], F32)
                slf = self.sb(p2, "m_slf", [128, 2, NTL], F32)
                pw = self.ps(p2, "m_pw", [128, NE], F32)
                pc = self.ps(p2, "m_pc", [128, NE], F32)
                fl = lambda t: t[:].rearrange("p e n -> p (e n)")
                S.dma("pool", tri[:], I["k_tri"][:, :], writes=[tri])
                S.dma("sp", sbpos[:], I["k_sbpos"][0:NSB].partition_broadcast(128), writes=[sbpos])
                S.dma("sp", wbase[:], I["k_wbase"][:, :], writes=[wbase])
                S.op("dve", lambda e: e.memset(onek[:], 1.0), writes=[onek])
                S.op("dve", lambda e: e.memset(one1[:], 1.0), writes=[one1])
                S.op("dve", lambda e: e.tensor_scalar(out=fl(sel), in0=fl(gatesE), scalar1=0.0, scalar2=None, op0=ALU.is_gt), reads=[gatesE], writes=[sel])
                S.op("dve", lambda e: e.tensor_copy(out=selb[:], in_=fl(sel)), reads=[sel], writes=[selb])
                S.op("pe", lambda e: e.matmul(pw[:], lhsT=tri[:], rhs=selb[:], start=True, stop=True), reads=[tri, selb], writes=[pw])
                S.op("pe", lambda e: e.matmul(pc[:], lhsT=onek[:], rhs=selb[:], start=True, stop=True), reads=[onek, selb], writes=[pc])
                S.op("act", lambda e: e.copy(out=fl(Wt), in_=pw[:]), reads=[pw], writes=[Wt])
                S.op("act", lambda e: e.copy(out=fl(Ct), in_=pc[:]), reads=[pc], writes=[Ct])
                for ex in range(NEXP):
                    S.op("dve", lambda e: e.tensor_tensor_scan(out=Ic[:, ex, :], data0=one1[:, 0:1].to_broadcast([128, NTL]), data1=Ct[:, ex, :], initial=0.0, op0=ALU.mult, op1=ALU.add),
                         reads=[one1, Ct], writes=[Ic])
                S.op("dve", lambda e: e.tensor_copy(out=cnt[:], in_=Ic[:, :, NTL - 1]), reads=[Ic], writes=[cnt])
                S.op("dve", lambda e: e.tensor_scalar(out=pad[:], in0=cnt[:], scalar1=1.0 / 512, scalar2=511.0 / 512 - 0.5 + 1.0 / 1024, op0=ALU.mult, op1=ALU.add), reads=[cnt], writes=[pad])
                S.op("dve", lambda e: e.tensor_scalar(out=pad[:], in0=pad[:], scalar1=8388608.0, scalar2=None, op0=ALU.add), reads=[pad], writes=[pad])
                S.op("dve", lambda e: e.tensor_scalar(out=pad[:], in0=pad[:], scalar1=-8388608.0, scalar2=512.0, op0=ALU.add, op1=ALU.mult), reads=[pad], writes=[pad])
                S.op("dve", lambda e: e.tensor_tensor_scan(out=pend[:], data0=one1[:, 0:1].to_broadcast([128, NEXP]), data1=pad[:], initial=0.0, op0=ALU.mult, op1=ALU.add),
                     reads=[one1, pad], writes=[pend])
                S.op("dve", lambda e: e.tensor_tensor(out=pst[:], in0=pend[:], in1=pad[:], op=ALU.subtract), reads=[pend, pad], writes=[pst])
                S.op("dve", lambda e: e.tensor_tensor(out=fl(S3), in0=fl(Ic), in1=fl(Ct), op=ALU.subtract), reads=[Ic, Ct], writes=[S3])
                S.op("dve", lambda e: e.tensor_tensor(out=fl(S3), in0=fl(S3), in1=fl(Wt), op=ALU.add), reads=[S3, Wt], writes=[S3])
                S.op("dve", lambda e: e.tensor_tensor(out=S3[:], in0=S3[:], in1=pst[:].unsqueeze(2).to_broadcast([128, NEXP, NTL]), op=ALU.add), reads=[S3, pst], writes=[S3])
                S.op("dve", lambda e: e.tensor_copy(out=cs[:, 0, :], in_=sel[:, 0, :]), reads=[sel], writes=[cs])
                for ex in range(1, NEXP):
                    S.op("dve", lambda e: e.tensor_tensor(out=cs[:, ex, :], in0=cs[:, ex - 1, :], in1=sel[:, ex, :], op=ALU.add), reads=[cs, sel], writes=[cs])
                for k in range(2):
                    S.op("dve", lambda e: e.tensor_scalar(out=fl(mm), in0=fl(cs), scalar1=float(k + 1), scalar2=None, op0=ALU.is_equal), reads=[cs], writes=[mm])
                    S.op("dve", lambda e: e.tensor_tensor(out=fl(mm), in0=fl(mm), in1=fl(sel), op=ALU.mult), reads=[mm, sel], writes=[mm])
                    S.op("dve", lambda e: e.tensor_tensor(out=fl(pr), in0=fl(mm), in1=fl(S3), op=ALU.mult), reads=[mm, S3], writes=[pr])
                    S.op("dve", lambda e: e.tensor_reduce(out=slf[:, k, :], in_=pr[:].rearrange("p e n -> p n e"), axis=AX.X, op=ALU.add), reads=[pr], writes=[slf])
                    S.op("dve", lambda e: e.tensor_tensor(out=fl(pr), in0=fl(mm), in1=fl(gatesE), op=ALU.mult), reads=[mm, gatesE], writes=[pr])
                    S.op("dve", lambda e: e.tensor_reduce(out=g12[:, k, :], in_=pr[:].rearrange("p e n -> p n e"), axis=AX.X, op=ALU.add), reads=[pr], writes=[g12])
                S.op("dve", lambda e: e.tensor_copy(out=sl_i[:], in_=slf[:]), reads=[slf], writes=[sl_i])
                S.op("dve", lambda e: e.tensor_tensor(out=cmp_[:], in0=pend[:].unsqueeze(1).to_broadcast([128, NSB, NEXP]), in1=sbpos[:].unsqueeze(2).to_broadcast([128, NSB, NEXP]), op=ALU.is_le),
                     reads=[pend, sbpos], writes=[cmp_])
                S.op("dve", lambda e: e.tensor_reduce(out=esb[:], in_=cmp_[:], axis=AX.X, op=ALU.add), reads=[cmp_], writes=[esb])
                S.op("dve", lambda e: e.tensor_scalar(out=esb[:], in0=esb[:], scalar1=float(NEXP - 1), scalar2=float(G * 128), op0=ALU.min, op1=ALU.mult), reads=[esb], writes=[esb])
                S.op("dve", lambda e: e.tensor_tensor(out=idxf[:], in0=esb[:].unsqueeze(2).to_broadcast([128, NSB, G]), in1=wbase[:].unsqueeze(1).to_broadcast([128, NSB, G]), op=ALU.add),
                     reads=[esb, wbase], writes=[idxf])
                S.op("dve", lambda e: e.tensor_copy(out=idxw[:], in_=idxf[:]), reads=[idxf], writes=[idxw])
            with self.scope() as p3:
                ut = [self.sb(p3, "m_ut%d" % i, [128, D], BF16) for i in range(3)]
                for tt in range(NTL):
                    u_ = ut[tt % 3]
                    S.dma("sp", u_[:], U2[tt * 128:(tt + 1) * 128, :], reads=[u2_b[tt]], writes=[u_])
                    for k in range(2):
                        S.idma(XS[:, :], bass.IndirectOffsetOnAxis(ap=sl_i[:, k, tt:tt + 1], axis=0), u_[:], None, reads=[u_, sl_i], writes=[xs_b[tt * 2 + k]])
            with self.scope() as p4:
                xs = [self.sb(p4, "m_xs%d" % i, [128, 4, D], BF16) for i in range(2)]
                uT = [self.sb(p4, "m_uT4_%d" % i, [128, 8, 512], BF16) for i in range(2)]
                w1g = [self.sb(p4, "m_w1g%d" % i, [128, 8, FW], BF16) for i in range(2)]
                w3g = [self.sb(p4, "m_w3g%d" % i, [128, 8, FW], BF16) for i in range(2)]
                w2g = [self.sb(p4, "m_w2g%d" % i, [128, NJ, D], BF16) for i in range(2)]
                hT = [self.sb(p4, "m_hT%d" % i, [128, NJ, 512], BF16) for i in range(2)]
                sl_ = [self.sb(p4, "m_sl%d" % i, [128, 512], F32) for i in range(2)]
                yacc = [self.sb(p4, "m_yacc%d" % i, [128, 4, D], F32) for i in range(2)]
                pTs = [self.ps(p4, "m_pT4_%d" % i, [128, 8, 128], BF16) for i in range(2)]
                pp1 = [self.ps(p4, "m_p1_%d" % i, [128, 512], F32) for i in range(2)]
                pp3 = [self.ps(p4, "m_p3_%d" % i, [128, 512], F32) for i in range(2)]
                py = [self.ps(p4, "m_py_%d" % i, [128, 512], F32) for i in range(2)]
                gi = 0
                k1 = 0
                ky = 0
                kt_ = 0
                for sb in range(NSB):
                    xs_, uT_, ya = xs[sb % 2], uT[sb % 2], yacc[sb % 2]
                    S.dma("sp", xs_[:], XS[sb * 512:(sb + 1) * 512, :].rearrange("(a p) d -> p a d", p=128), reads=xs_b, writes=[xs_])
                    for t4 in range(4):
                        pT = pTs[kt_ % 2]
                        kt_ += 1
                        for c in range(8):
                            S.op("pe", lambda e: e.transpose(out=pT[:, c, :], in_=xs_[:, t4, c * 128:(c + 1) * 128], identity=self.identb[:]), reads=[xs_, self.identb], writes=[pT], sig=(c == 7))
                        S.op("act", lambda e: e.copy(out=uT_[:, :, t4 * 128:(t4 + 1) * 128], in_=pT[:]), reads=[pT], writes=[uT_])
                    for g in range(G):
                        wa, wb_, wc = w1g[gi % 2], w3g[gi % 2], w2g[gi % 2]
                        gi += 1
                        off = bass.IndirectOffsetOnAxis(ap=idxw[:, sb, g:g + 1], axis=0)
                        S.idma(wa[:].rearrange("p c f -> p (c f)"), None, W["mg1"][:, :], off, reads=[idxw] + [W["mg1_b"][ex * G + g] for ex in range(NEXP)], writes=[wa])
                        off = bass.IndirectOffsetOnAxis(ap=idxw[:, sb, g:g + 1], axis=0)
                        S.idma(wb_[:].rearrange("p c f -> p (c f)"), None, W["mg3"][:, :], off, reads=[idxw] + [W["mg3_b"][ex * G + g] for ex in range(NEXP)], writes=[wb_])
                        off = bass.IndirectOffsetOnAxis(ap=idxw[:, sb, g:g + 1], axis=0)
                        S.idma(wc[:].rearrange("p j d -> p (j d)"), None, W["mg2"][:, :], off, reads=[idxw] + [W["mg2_b"][ex * G + g] for ex in range(NEXP)], writes=[wc])
                        h_ = hT[g % 2]
                        nv = nval[g]
                        for j in range(nv):
                            a1, a3, s_ = pp1[k1 % 2], pp3[k1 % 2], sl_[k1 % 2]
                            k1 += 1
                            for c in range(8):
                                S.op("pe", lambda e: e.matmul(a1[:], lhsT=wa[:, c, j * 128:(j + 1) * 128], rhs=uT_[:, c, :], start=(c == 0), stop=(c == 7)), reads=[wa, uT_], writes=[a1])
                            for c in range(8):
                                S.op("pe", lambda e: e.matmul(a3[:], lhsT=wb_[:, c, j * 128:(j + 1) * 128], rhs=uT_[:, c, :], start=(c == 0), stop=(c == 7)), reads=[wb_, uT_], writes=[a3])
                            S.op("act", lambda e: e.activation(out=s_[:], in_=a1[:], func=AF.Silu), reads=[a1], writes=[s_])
                            S.op("dve", lambda e: e.tensor_tensor(out=h_[:, j, :], in0=s_[:], in1=a3[:], op=ALU.mult), reads=[s_, a3], writes=[h_])
                        for t4 in range(4):
                            for dg in range(2):
                                y_ = py[ky % 2]
                                ky += 1
                                for j in range(nv):
                                    S.op("pe", lambda e: e.matmul(y_[:], lhsT=h_[:, j, t4 * 128:(t4 + 1) * 128], rhs=wc[:, j, dg * 512:(dg + 1) * 512], start=(j == 0), stop=(j == nv - 1)), reads=[h_, wc], writes=[y_])
                                yv = ya[:, t4, dg * 512:(dg + 1) * 512]
                                if g == 0:
                                    S.op("act", lambda e: e.copy(out=yv, in_=y_[:]), reads=[y_], writes=[ya])
                                else:
                                    S.op("dve", lambda e: e.tensor_tensor(out=yv, in0=y_[:], in1=yv, op=ALU.add), reads=[y_, ya], writes=[ya])
                    S.dma("sp", YS[sb * 512:(sb + 1) * 512, :].rearrange("(a p) d -> p a d", p=128), ya[:], reads=[ya], writes=[ys_b[sb]])
            with self.scope() as p5:
                ya_ = [self.sb(p5, "m_ya%d" % i, [128, D], F32) for i in range(4)]
                yb_ = [self.sb(p5, "m_yb%d" % i, [128, D], F32) for i in range(4)]
                xt = [self.sb(p5, "m_x5_%d" % i, [128, D], F32) for i in range(4)]
                junk5 = [self.sb(p5, "m_junk5_%d" % i, [128, D], BF16) for i in range(2)]
                ss5 = [self.sb(p5, "m_ss5_%d" % i, [128, 4], F32) for i in range(2)]
                xo = [self.sb(p5, "m_xo%d" % i, [128, D], F32) for i in range(4)]
                for tt in range(NTL):
                    a_, b_, x_, xo_ = ya_[tt % 4], yb_[tt % 4], xt[tt % 4], xo[tt % 4]
                    S.dma("sp", x_[:], xin[tt * 128:(tt + 1) * 128, :], reads=[xin_bufs[tt]], writes=[x_])
                    S.idma(a_[:], None, YS[:, :], bass.IndirectOffsetOnAxis(ap=sl_i[:, 0, tt:tt + 1], axis=0), reads=ys_b + [sl_i.b], writes=[a_])
                    S.idma(b_[:], None, YS[:, :], bass.IndirectOffsetOnAxis(ap=sl_i[:, 1, tt:tt + 1], axis=0), reads=ys_b + [sl_i.b], writes=[b_])
                    S.op("dve", lambda e: e.tensor_scalar(out=a_[:], in0=a_[:], scalar1=g12[:, 0, tt:tt + 1], scalar2=None, op0=ALU.mult), reads=[a_, g12], writes=[a_])
                    S.op("dve", lambda e: e.scalar_tensor_tensor(out=a_[:], in0=b_[:], scalar=g12[:, 1, tt:tt + 1], in1=a_[:], op0=ALU.mult, op1=ALU.add), reads=[b_, g12, a_], writes=[a_])
                    self.post_norm_residual(a_, x_, bc["ggt_f"], ss5[tt % 2], junk5[tt % 2], xo_)
                    S.dma("sp", xout[tt * 128:(tt + 1) * 128, :], xo_[:], reads=[xo_], writes=[xout_bufs[tt]])

    def convert_moe(self, w1, w3, w2):
        G, FW, NJ = 6, 512, 4
        out = {}
        for nm, src in (("mg1", w1), ("mg3", w3)):
            dst = self.nc.dram_tensor("wb_" + nm, [NEXP * G * 128, 8 * FW], BF16, kind="Internal").ap()
            bufs = []
            for ex in range(NEXP):
                for g in range(G):
                    r0 = (ex * G + g) * 128
                    b = Buf("%s_%d_%d" % (nm, ex, g))
                    fw = min(FW, DFF - g * FW)
                    self.S.dma("pool", dst[r0:r0 + 128, :].rearrange("p (c f) -> p c f", f=FW)[:, :, 0:fw],
                               src[ex * D:(ex + 1) * D, g * FW:g * FW + fw].rearrange("(c p) f -> p c f", p=128), writes=[b])
                    bufs.append(b)
            out[nm], out[nm + "_b"] = dst, bufs
        dst = self.nc.dram_tensor("wb_mg2", [NEXP * G * 128, NJ * D], BF16, kind="Internal").ap()
        bufs = []
        for ex in range(NEXP):
            for g in range(G):
                r0 = (ex * G + g) * 128
                b = Buf("mg2_%d_%d" % (ex, g))
                fw = min(FW, DFF - g * FW)
                self.S.dma("pool", dst[r0:r0 + 128, :].rearrange("p (j d) -> p j d", d=D)[:, 0:fw // 128, :],
                           w2[ex * DFF + g * FW:ex * DFF + g * FW + fw, :].rearrange("(j p) d -> p j d", p=128), writes=[b])
                bufs.append(b)
        out["mg2"], out["mg2_b"] = dst, bufs
        return out

    def conv_phase(self, xin, xin_bufs, xout, xout_bufs, bc, P):
        S, nc, T = self.S, self.nc, self.T
        NT5 = T // 512
        HW = T + 30
        hc = self.dscr("hc_scr", [8, 128, HW], BF16)
        hc_bufs = [Buf("hc%d" % i) for i in range(NT5)]
        hc_pad = Buf("hc_pad")
        with self.scope() as es:
            pw1 = self.sb(es, "c_pw1", [128, 8, 2 * D], BF16)
            S.dma("sp", pw1[:], P["pw1"].rearrange("(c p) n -> p c n", p=128), reads=P["pw1_b"], writes=[pw1])
            b1 = self.sb(es, "c_b1", [128, 16], F32)
            S.dma("sp", b1[:], self.inp["l1_conv_pw1_b"].rearrange("(c p) -> p c", p=128), writes=[b1], allow_slow_non_contiguous=True)
            b1h = self.sb(es, "c_b1h", [128, 8], F32)
            S.op("dve", lambda e: e.tensor_scalar(out=b1h[:], in0=b1[:, 8:16], scalar1=0.5, scalar2=None, op0=ALU.mult), reads=[b1], writes=[b1h])
            zt = self.sb(es, "c_z", [128, 8, 15], BF16)
            S.op("dve", lambda e: e.memset(zt[:], 0.0), writes=[zt])
            S.dma("sp", hc[:, :, 0:15].rearrange("c p t -> p c t"), zt[:], reads=[zt], writes=[hc_pad])
            S.dma("sp", hc[:, :, HW - 15:HW].rearrange("c p t -> p c t"), zt[:], reads=[zt], writes=[hc_pad])
            xt = [self.sb(es, "c_xt%d" % i, [128, D], F32) for i in range(2)]
            tmp = [self.sb(es, "c_tmp%d" % i, [128, D], F32) for i in range(2)]
            junk = self.sb(es, "c_junk", [128, D], BF16)
            u = [self.sb(es, "c_u%d" % i, [128, D], BF16) for i in range(2)]
            ss = [self.sb(es, "c_ss%d" % i, [128, 4], F32) for i in range(2)]
            uT = [self.sb(es, "c_uT%d" % i, [128, 8, 512], BF16) for i in range(2)]
            hf = [self.sb(es, "c_hf%d" % i, [128, 8, 512], BF16) for i in range(2)]
            sg = [self.sb(es, "c_sg%d" % i, [128, 512], F32) for i in range(2)]
            pT = [self.ps(es, "c_pT%d" % i, [128, 8, 128], BF16) for i in range(2)]
            pa = [self.ps(es, "c_pa%d" % i, [128, 512], F32) for i in range(2)]
            pg = [self.ps(es, "c_pg%d" % i, [128, 512], F32) for i in range(2)]
            k = 0
            for t5 in range(NT5):
                uT_ = uT[t5 % 2]
                for t4 in range(4):
                    ti = t5 * 4 + t4
                    x_ = xt[ti % 2]
                    S.dma("sp", x_[:], xin[ti * 128:(ti + 1) * 128, :], reads=[xin_bufs[ti]], writes=[x_])
                    self.norm_mod_T(x_, bc["gmod_m"], bc["shift_m"], uT_, t4 * 128, ss, tmp, u, pT, junk)
                h_ = hf[t5 % 2]
                for j in range(8):
                    a_, g_, s_ = pa[k % 2], pg[k % 2], sg[k % 2]
                    k += 1
                    for c in range(8):
                        S.op("pe", lambda e: e.matmul(a_[:], lhsT=pw1[:, c, j * 128:(j + 1) * 128], rhs=uT_[:, c, :], start=(c == 0), stop=(c == 7)),
                             reads=[pw1, uT_], writes=[a_])
                    for c in range(8):
                        S.op("pe", lambda e: e.matmul(g_[:], lhsT=pw1[:, c, D + j * 128:D + (j + 1) * 128], rhs=uT_[:, c, :], start=(c == 0), stop=(c == 7)),
                             reads=[pw1, uT_], writes=[g_])
                    S.op("act", lambda e: e.activation(out=s_[:], in_=g_[:], func=AF.Tanh, scale=0.5, bias=b1h[:, j:j + 1]), reads=[g_, b1h], writes=[s_])
                    S.op("dve", lambda e: e.tensor_scalar(out=s_[:], in0=s_[:], scalar1=0.5, scalar2=0.5, op0=ALU.mult, op1=ALU.add), reads=[s_], writes=[s_])
                    S.op("dve", lambda e: e.scalar_tensor_tensor(out=h_[:, j, :], in0=a_[:], scalar=b1[:, j:j + 1], in1=s_[:], op0=ALU.add, op1=ALU.mult),
                         reads=[a_, b1, s_], writes=[h_])
                S.dma("sp", hc[:, :, 15 + t5 * 512:15 + (t5 + 1) * 512].rearrange("c p t -> p c t"), h_[:], reads=[h_], writes=[hc_bufs[t5]])
        with self.scope() as es:
            pw2 = self.sb(es, "c_pw2", [128, 8, D], BF16)
            S.dma("sp", pw2[:], P["pw2"].rearrange("(c p) n -> p c n", p=128), reads=P["pw2_b"], writes=[pw2])
            dwT = self.sb(es, "c_dwT", [128, 8, 31], F32)
            for j in range(8):
                S.dma("sp", dwT[:, j, :], self.inp["l1_conv_dw_w"][:, j * 128:(j + 1) * 128].rearrange("k p -> p k"), writes=[dwT],
                      allow_slow_non_contiguous=True)
            vecs = self.sb(es, "c_vecs", [128, 3, 8], F32)
            for i, nm in enumerate(["l1_conv_dw_b", "l1_conv_ln_g", "l1_conv_ln_b"]):
                S.dma("sp", vecs[:, i, :], self.inp[nm].rearrange("(c p) -> p c", p=128), writes=[vecs], allow_slow_non_contiguous=True)
            diag = self.sb(es, "c_diag", [128, 8 * 31, 128], BF16)
            for j in range(8):
                for kk in range(31):
                    eng = "dve" if (kk % 2 == 0) else "pool"
                    S.op(eng, lambda e: e.tensor_scalar(out=diag[:, j * 31 + kk, :], in0=self.identf[:], scalar1=dwT[:, j, kk:kk + 1], scalar2=None, op0=ALU.mult),
                         reads=[self.identf, dwT], writes=[diag])
            pb2 = self.sb(es, "c_pb2", [128, D], F32)
            S.dma("sp", pb2[:], self.inp["l1_conv_pw2_b"].partition_broadcast(128), writes=[pb2])
            hw = [self.sb(es, "c_hw%d" % i, [128, 8, 542], BF16) for i in range(2)]
            cf = self.sb(es, "c_cf", [128, 8, 512], F32)
            cb = self.sb(es, "c_cb", [128, 8, 512], BF16)
            cq = self.sb(es, "c_cq", [128, 8, 512], BF16)
            hn = self.sb(es, "c_hn", [128, 8, 512], BF16)
            mean = self.sb(es, "c_mean", [128, 512], F32)
            rstd = self.sb(es, "c_rstd", [128, 512], F32)
            tq = [self.sb(es, "c_tq%d" % i, [128, 512], F32) for i in range(2)]
            xt = [self.sb(es, "c2_xt%d" % i, [128, D], F32) for i in range(2)]
            ys = [self.sb(es, "c2_y%d" % i, [128, D], F32) for i in range(2)]
            xos = [self.sb(es, "c2_xo%d" % i, [128, D], F32) for i in range(2)]
            junk = self.sb(es, "c2_junk", [128, D], BF16)
            sss = [self.sb(es, "c2_ss%d" % i, [128, 4], F32) for i in range(2)]
            pc = [self.ps(es, "c_pc%d" % i, [128, 512], F32) for i in range(2)]
            pm = self.ps(es, "c_pm", [128, 512], F32)
            pq = self.ps(es, "c_pq", [128, 512], F32)
            py = [self.ps(es, "c_py%d" % i, [128, 512], F32) for i in range(2)]
            k = 0
            ky = 0
            for t5 in range(NT5):
                hw_ = hw[t5 % 2]
                rd = [hc_pad] + [hc_bufs[i] for i in (t5 - 1, t5, t5 + 1) if 0 <= i < NT5]
                S.dma("sp", hw_[:], hc[:, :, t5 * 512:t5 * 512 + 542].rearrange("c p t -> p c t"), reads=rd, writes=[hw_])
                for j in range(8):
                    c_ = pc[k % 2]
                    k += 1
                    for kk in range(31):
                        S.op("pe", lambda e: e.matmul(c_[:], lhsT=diag[:, j * 31 + kk, :], rhs=hw_[:, j, kk:kk + 512], start=(kk == 0), stop=(kk == 30)),
                             reads=[diag, hw_], writes=[c_])
                    S.op("act", lambda e: e.activation(out=cf[:, j, :], in_=c_[:], func=AF.Identity, bias=vecs[:, 0, j:j + 1]), reads=[c_, vecs], writes=[cf])
                    S.op("act", lambda e: e.activation(out=cq[:, j, :], in_=c_[:], func=AF.Square, bias=vecs[:, 0, j:j + 1]), reads=[c_, vecs], writes=[cq])
                    S.op("dve", lambda e: e.tensor_copy(out=cb[:, j, :], in_=cf[:, j, :]), reads=[cf], writes=[cb])
                for j in range(8):
                    S.op("pe", lambda e: e.matmul(pm[:], lhsT=self.onesb[:], rhs=cb[:, j, :], start=(j == 0), stop=(j == 7)), reads=[self.onesb, cb], writes=[pm])
                for j in range(8):
                    S.op("pe", lambda e: e.matmul(pq[:], lhsT=self.onesb[:], rhs=cq[:, j, :], start=(j == 0), stop=(j == 7)), reads=[self.onesb, cq], writes=[pq])
                S.op("act", lambda e: e.copy(out=mean[:], in_=pm[:]), reads=[pm], writes=[mean])
                S.op("dve", lambda e: e.tensor_tensor(out=rstd[:], in0=mean[:], in1=mean[:], op=ALU.mult), reads=[mean], writes=[rstd])
                S.op("dve", lambda e: e.tensor_tensor(out=rstd[:], in0=pq[:], in1=rstd[:], op=ALU.subtract), reads=[pq, rstd], writes=[rstd])
                S.op("act", lambda e: e.activation(out=rstd[:], in_=rstd[:], func=AF.Ln, bias=self.epsc[:, 0:1]), reads=[rstd, self.epsc], writes=[rstd])
                S.op("act", lambda e: e.activation(out=rstd[:], in_=rstd[:], func=AF.Exp, scale=-0.5), reads=[rstd], writes=[rstd])
                for j in range(8):
                    t_ = tq[j % 2]
                    S.op("dve", lambda e: e.tensor_tensor(out=t_[:], in0=cf[:, j, :], in1=mean[:], op=ALU.subtract), reads=[cf, mean], writes=[t_])
                    S.op("dve", lambda e: e.tensor_tensor(out=t_[:], in0=t_[:], in1=rstd[:], op=ALU.mult), reads=[t_, rstd], writes=[t_])
                    S.op("act", lambda e: e.activation(out=hn[:, j, :], in_=t_[:], func=AF.Silu, scale=vecs[:, 1, j:j + 1], bias=vecs[:, 2, j:j + 1]),
                         reads=[t_, vecs], writes=[hn])
                for t4 in range(4):
                    ti = t5 * 4 + t4
                    x_ = xt[ti % 2]
                    y, xo, ss = ys[ti % 2], xos[ti % 2], sss[ti % 2]
                    S.dma("sp", x_[:], xin[ti * 128:(ti + 1) * 128, :], reads=[xin_bufs[ti]], writes=[x_])
                    for dg in range(2):
                        y_ = py[ky % 2]
                        ky += 1
                        for j in range(8):
                            S.op("pe", lambda e: e.matmul(y_[:], lhsT=hn[:, j, t4 * 128:(t4 + 1) * 128], rhs=pw2[:, j, dg * 512:(dg + 1) * 512], start=(j == 0), stop=(j == 7)),
                                 reads=[hn, pw2], writes=[y_])
                        S.op("dve", lambda e: e.tensor_tensor(out=y[:, dg * 512:(dg + 1) * 512], in0=y_[:], in1=pb2[:, dg * 512:(dg + 1) * 512], op=ALU.add),
                             reads=[y_, pb2], writes=[y])
                    self.post_norm_residual(y, x_, bc["ggt_m"], ss, junk, xo)
                    S.dma("sp", xout[ti * 128:(ti + 1) * 128, :], xo[:], reads=[xo], writes=[xout_bufs[ti]])


    def proj_phase(self, x, x_bufs, bc, SC):
        S, nc, T = self.S, self.nc, self.T
        I = self.inp
        NTL = T // 128
        with self.scope() as es:
            wfm = self.sb(es, "a_wfm", [128, 8, 1024], BF16)
            wtm = self.sb(es, "a_wtm", [128, 8, 2688], BF16)
            S.dma("pool", wfm[:], I["k_wfm"].rearrange("(c p) n -> p c n", p=128), writes=[wfm])
            S.dma("pool", wtm[:], I["k_wtm"].rearrange("(c p) n -> p c n", p=128), writes=[wtm])
            xt = [self.sb(es, "a_xt%d" % i, [128, D], F32) for i in range(2)]
            tmp = [self.sb(es, "a_tmp%d" % i, [128, D], F32) for i in range(2)]
            junk = self.sb(es, "a_junk", [128, D], BF16)
            u = [self.sb(es, "a_u%d" % i, [128, D], BF16) for i in range(2)]
            ss = [self.sb(es, "a_ss%d" % i, [128, 4], F32) for i in range(2)]
            uT = [self.sb(es, "a_uT%d" % i, [128, 8, 512], BF16) for i in range(2)]
            vt = [self.sb(es, "a_vt%d" % i, [128, 1024], BF16) for i in range(2)]
            ot = [self.sb(es, "a_ot%d" % i, [128, 512], F32) for i in range(2)]
            qk = self.sb(es, "a_qk", [128, 1024], F32)
            sq = self.sb(es, "a_sq", [128, 1024], F32)
            n2 = self.sb(es, "a_n2", [128, 16], F32)
            rot = self.sb(es, "a_rot", [128, 1024], BF16)
            rp = [self.sb(es, "a_rp%d" % i, [128, 64], F32) for i in range(2)]
            r1 = self.sb(es, "a_r1", [128, 16, 16], F32)
            r2 = self.sb(es, "a_r2", [128, 16, 16], F32)
            qkT = [self.sb(es, "a_qkT%d" % i, [128, 8, 512], BF16) for i in range(2)]
            gt = self.sb(es, "a_gt", [128, 128], F32)
            gT = [self.sb(es, "a_gT%d" % i, [64, 2, 512], F32) for i in range(2)]
            pq = [self.sb(es, "a_pq%d" % i, [128, 8, 512], F32) for i in range(1)]
            zc = self.sb(es, "a_zc", [128, 4, 1], F32)
            pT = self.ps(es, "a_pT", [128, 8, 128], BF16)
            pTn = self.ps(es, "a_pTn", [128, 8, 128], BF16)
            pG = self.ps(es, "a_pG", [64, 2, 128], F32)
            ptm = [self.ps(es, "a_ptm%d" % i, [128, 512], F32) for i in range(3)]
            pfm = [self.ps(es, "a_pfm%d" % i, [128, 512], F32) for i in range(2)]
            S.op("dve", lambda e: e.memset(zc[:], 0.0), writes=[zc])
            S.op("dve", lambda e: e.memset(self.q2max[:], 0.0), writes=[self.q2max])
            S.op("dve", lambda e: e.memset(self.k2max[:], 0.0), writes=[self.k2max])
            for (arr, L) in ((SC["PQ"], T), (SC["PK"], T), (SC["PKc"], CTX)):
                for col in (0, L + 1):
                    S.dma("sp", arr[:, :, col:col + 1].rearrange("c p t -> p c t"), zc[:], reads=[zc], writes=[SC["pad_b"]], allow_slow_non_contiguous=True)
            groups = [("c", 0, 2)] + [("l", g * 4, 4) for g in range(NTL // 4)]
            ktm = 0
            kfm = 0
            flat = [(gi, kind, t0 + tt, tt) for gi, (kind, t0, n) in enumerate(groups) for tt in range(n)]

            def do_norm(k):
                gi_, kind_, ti_, tt_ = flat[k]
                x_ = xt[k % 2]
                if kind_ == "c":
                    S.dma("sp", x_[:], I["ctx"][ti_ * 128:(ti_ + 1) * 128, :], writes=[x_])
                    self.norm_mod_T(x_, bc["gmod_c"], bc["shift_c"], uT[gi_ % 2], tt_ * 128, ss, tmp, u, pTn, junk)
                else:
                    S.dma("sp", x_[:], x[ti_ * 128:(ti_ + 1) * 128, :], reads=[x_bufs[ti_]], writes=[x_])
                    self.norm_mod_T(x_, bc["gmod_m"], bc["shift_m"], uT[gi_ % 2], tt_ * 128, ss, tmp, u, pTn, junk)

            do_norm(0)
            kflat = 0
            for gi, (kind, t0, n) in enumerate(groups):
                uT_ = uT[gi % 2]
                qkT_ = qkT[gi % 2]
                gT_ = gT[gi % 2]
                N = n * 128
                for tt in range(n):
                    ti = t0 + tt
                    if kflat + 1 < len(flat):
                        do_norm(kflat + 1)
                    kflat += 1
                    if kind == "c":
                        srow = ti * 128
                    else:
                        srow = CTX + ti * 128
                        rp_ = rp[ti % 2]
                        S.dma("sp", rp_[:], I["k_rope"][ti * 128:(ti + 1) * 128, :], writes=[rp_])
                    sb_i = srow // 128
                    vt_ = vt[ti % 2]
                    ot_ = ot[ti % 2]
                    for g in range(6):
                        c0, c1 = g * 512, min((g + 1) * 512, 2688)
                        if kind == "c" and g in (1, 3):
                            continue
                        p_ = ptm[ktm % 3]
                        ktm += 1
                        for c in range(8):
                            S.op("pe", lambda e: e.matmul(p_[:, 0:c1 - c0], lhsT=uT_[:, c, tt * 128:(tt + 1) * 128], rhs=wtm[:, c, c0:c1], start=(c == 0), stop=(c == 7)),
                                 reads=[uT_, wtm], writes=[p_])
                        if g == 0:
                            S.op("act", lambda e: e.copy(out=vt_[:, 0:512], in_=p_[:]), reads=[p_], writes=[vt_])
                        elif g == 1:
                            S.op("act", lambda e: e.copy(out=ot_[:], in_=p_[:]), reads=[p_], writes=[ot_])
                            S.dma("sp", SC["OM"][ti * 128:(ti + 1) * 128, :], ot_[:], reads=[ot_], writes=[SC["OM_b"][ti]])
                        elif g == 2:
                            S.op("act", lambda e: e.copy(out=vt_[:, 512:1024], in_=p_[:]), reads=[p_], writes=[vt_])
                            S.dma("sp", SC["VM"][srow:srow + 128, :], vt_[:, 0:512], reads=[vt_], writes=[SC["VM_b"][sb_i]])
                            S.dma("sp", SC["VD"][srow:srow + 128, :], vt_[:, 512:1024], reads=[vt_], writes=[SC["VD_b"][sb_i]])
                        elif g == 3:
                            S.op("act", lambda e: e.copy(out=qk[:, 0:512], in_=p_[:]), reads=[p_], writes=[qk])
                        elif g == 4:
                            S.op("act", lambda e: e.copy(out=qk[:, 512:1024], in_=p_[:]), reads=[p_], writes=[qk])
                        else:
                            S.op("act", lambda e: e.copy(out=gt[:], in_=p_[:, 0:128]), reads=[p_], writes=[gt])
                    lo = 512 if kind == "c" else 0
                    S.op("dve", lambda e: e.tensor_tensor(out=sq[:, lo:1024], in0=qk[:, lo:1024], in1=qk[:, lo:1024], op=ALU.mult), reads=[qk], writes=[sq])
                    S.op("dve", lambda e: e.tensor_reduce(out=n2[:, lo // 64:16], in_=sq[:, lo:1024].rearrange("p (g d) -> p g d", d=64), axis=AX.X, op=ALU.add),
                         reads=[sq], writes=[n2])
                    if kind != "c":
                        S.op("dve", lambda e: e.tensor_tensor(out=self.q2max[:], in0=self.q2max[:], in1=n2[:, 0:8], op=ALU.max), reads=[self.q2max, n2], writes=[self.q2max])
                    S.op("dve", lambda e: e.tensor_tensor(out=self.k2max[:], in0=self.k2max[:], in1=n2[:, 8:16], op=ALU.max), reads=[self.k2max, n2], writes=[self.k2max])
                    if kind == "c":
                        S.op("dve", lambda e: e.tensor_copy(out=rot[:, 512:1024], in_=qk[:, 512:1024]), reads=[qk], writes=[rot])
                    else:
                        q3 = qk[:].rearrange("p (g d) -> p g d", d=64)
                        o3 = rot[:].rearrange("p (g d) -> p g d", d=64)
                        for a in range(2):
                            b0 = a * 32
                            cosb = rp_[:, b0:b0 + 16].unsqueeze(1).to_broadcast([128, 16, 16])
                            sinb = rp_[:, b0 + 16:b0 + 32].unsqueeze(1).to_broadcast([128, 16, 16])
                            x1 = q3[:, :, b0:b0 + 16]
                            x2 = q3[:, :, b0 + 16:b0 + 32]
                            S.op("dve", lambda e: e.tensor_tensor(out=r1[:], in0=x1, in1=cosb, op=ALU.mult), reads=[qk, rp_], writes=[r1])
                            S.op("dve", lambda e: e.tensor_tensor(out=r2[:], in0=x2, in1=sinb, op=ALU.mult), reads=[qk, rp_], writes=[r2])
                            S.op("dve", lambda e: e.tensor_tensor(out=o3[:, :, b0:b0 + 16], in0=r1[:], in1=r2[:], op=ALU.subtract), reads=[r1, r2], writes=[rot])
                            S.op("dve", lambda e: e.tensor_tensor(out=r1[:], in0=x2, in1=cosb, op=ALU.mult), reads=[qk, rp_], writes=[r1])
                            S.op("dve", lambda e: e.tensor_tensor(out=r2[:], in0=x1, in1=sinb, op=ALU.mult), reads=[qk, rp_], writes=[r2])
                            S.op("dve", lambda e: e.tensor_tensor(out=o3[:, :, b0 + 16:b0 + 32], in0=r1[:], in1=r2[:], op=ALU.add), reads=[r1, r2], writes=[rot])
                    c_lo = 4 if kind == "c" else 0
                    for c in range(c_lo, 8):
                        S.op("pe", lambda e: e.transpose(out=pT[:, c, :], in_=rot[:, c * 128:(c + 1) * 128], identity=self.identb[:]), reads=[rot, self.identb], writes=[pT], sig=(c == 7))
                    S.op("act", lambda e: e.copy(out=qkT_[:, c_lo:8, tt * 128:(tt + 1) * 128], in_=pT[:, c_lo:8, :]), reads=[pT], writes=[qkT_])
                    for hh in range(2):
                        S.op("pe", lambda e: e.transpose(out=pG[:, hh, :], in_=gt[:, hh * 64:(hh + 1) * 64], identity=self.identf[:]), reads=[gt, self.identf], writes=[pG], sig=(hh == 1))
                    S.op("act", lambda e: e.copy(out=gT_[:, :, tt * 128:(tt + 1) * 128], in_=pG[:]), reads=[pG], writes=[gT_])
                s0 = 0 if kind == "c" else CTX + t0 * 128
                if kind != "c":
                    S.dma("sp", SC["QD"][:, :, t0 * 128:t0 * 128 + N].rearrange("c p t -> p c t"), qkT_[:, 0:4, 0:N], reads=[qkT_], writes=[SC["QD_b"][gi]])
                S.dma("sp", SC["KD"][:, :, s0:s0 + N].rearrange("c p t -> p c t"), qkT_[:, 4:8, 0:N], reads=[qkT_], writes=[SC["KD_b"][gi]])
                S.dma("sp", SC["GT"][:, :, s0:s0 + N].rearrange("g p t -> p g t"), gT_[:, :, 0:N], reads=[gT_], writes=[SC["GT_b"][gi]])
                pq_ = pq[0]
                for ch in range(c_lo, 8):
                    f_ = pfm[kfm % 2]
                    kfm += 1
                    for c in range(8):
                        S.op("pe", lambda e: e.matmul(f_[:, 0:N], lhsT=wfm[:, c, ch * 128:(ch + 1) * 128], rhs=uT_[:, c, 0:N], start=(c == 0), stop=(c == 7)),
                             reads=[wfm, uT_], writes=[f_])
                    S.op("act", lambda e: e.copy(out=pq_[:, ch, 0:N], in_=f_[:, 0:N]), reads=[f_], writes=[pq_])
                if kind == "c":
                    S.dma("sp", SC["PKc"][:, :, 1:1 + N].rearrange("c p t -> p c t"), pq_[:, 4:8, 0:N], reads=[pq_], writes=[SC["PKc_b"]])
                else:
                    S.dma("sp", SC["PQ"][:, :, 1 + t0 * 128:1 + t0 * 128 + N].rearrange("c p t -> p c t"), pq_[:, 0:4, 0:N], reads=[pq_], writes=[SC["PQ_b"][gi - 1]])
                    S.dma("sp", SC["PK"][:, :, 1 + t0 * 128:1 + t0 * 128 + N].rearrange("c p t -> p c t"), pq_[:, 4:8, 0:N], reads=[pq_], writes=[SC["PK_b"][gi - 1]])


    def mlstm_phase(self, SC):
        S, nc, T = self.S, self.nc, self.T
        I = self.inp
        Sall = CTX + T
        NC = Sall // 128
        NCX = CTX // 128
        with self.scope() as es:
            cw = self.sb(es, "b_cw", [128, 8, 3], F32)
            for j in range(8):
                S.dma("sp", cw[:, j, :], I["l0_mlstm_conv_w"][:, j * 128:(j + 1) * 128].rearrange("k p -> p k"), writes=[cw], allow_slow_non_contiguous=True)
            win = [self.sb(es, "b_win%d" % i, [128, 4, 514], F32) for i in range(2)]
            acc = [self.sb(es, "b_acc%d" % i, [128, 512], F32) for i in range(2)]
            qs = [self.sb(es, "b_qs%d" % i, [128, 4, 512], BF16) for i in range(2)]
            ktm = [self.sb(es, "b_ktm%d" % i, [128, 512], BF16) for i in range(2)]
            pT = self.ps(es, "b_pT", [128, 4, 128], BF16)
            jobs = [("kc", SC["PKc"], SC["KMT"], 0, CTX, 4)]
            for g in range(T // 512):
                jobs.append(("q", SC["PQ"], SC["QMT"], g * 512, 512, 0))
                jobs.append(("k", SC["PK"], SC["KMT"], g * 512, 512, 4))
            for ji, (kind, src, dst, t0, N, cb0) in enumerate(jobs):
                w_ = win[ji % 2]
                q_ = qs[ji % 2]
                if kind == "kc":
                    rd = [SC["pad_b"], SC["PKc_b"]]
                else:
                    bl = SC["PQ_b"] if kind == "q" else SC["PK_b"]
                    g = t0 // 512
                    rd = [SC["pad_b"]] + [bl[i] for i in (g - 1, g, g + 1) if 0 <= i < len(bl)]
                S.dma("sp", w_[:, :, 0:N + 2], src[:, :, t0:t0 + N + 2].rearrange("c p t -> p c t"), reads=rd, writes=[w_])
                eng = "dve"
                for c in range(4):
                    a_ = acc[c % 2]
                    S.op(eng, lambda e: e.tensor_scalar(out=a_[:, 0:N], in0=w_[:, c, 0:N], scalar1=cw[:, cb0 + c, 0:1], scalar2=None, op0=ALU.mult), reads=[w_, cw], writes=[a_])
                    S.op(eng, lambda e: e.scalar_tensor_tensor(out=a_[:, 0:N], in0=w_[:, c, 1:N + 1], scalar=cw[:, cb0 + c, 1:2], in1=a_[:, 0:N], op0=ALU.mult, op1=ALU.add),
                         reads=[w_, cw, a_], writes=[a_])
                    S.op(eng, lambda e: e.scalar_tensor_tensor(out=a_[:, 0:N], in0=w_[:, c, 2:N + 2], scalar=cw[:, cb0 + c, 2:3], in1=a_[:, 0:N], op0=ALU.mult, op1=ALU.add),
                         reads=[w_, cw, a_], writes=[a_])
                    S.op("act", lambda e: e.activation(out=q_[:, c, 0:N], in_=a_[:, 0:N], func=AF.Silu), reads=[a_], writes=[q_])
                if kind == "q":
                    S.dma("sp", dst[:, :, t0:t0 + N].rearrange("c p t -> p c t"), q_[:, :, 0:N], reads=[q_], writes=[SC["QMT_b"][t0 // 512]])
                else:
                    s0 = t0 if kind == "kc" else CTX + t0
                    bi = 0 if kind == "kc" else 1 + t0 // 512
                    S.dma("sp", dst[:, :, s0:s0 + N].rearrange("c p t -> p c t"), q_[:, :, 0:N], reads=[q_], writes=[SC["KMT_b"][bi]])
                    for tt in range(N // 128):
                        k_ = ktm[tt % 2]
                        for c in range(4):
                            S.op("pe", lambda e: e.transpose(out=pT[:, c, :], in_=q_[:, c, tt * 128:(tt + 1) * 128], identity=self.identb[:]), reads=[q_, self.identb], writes=[pT], sig=(c == 3))
                        S.op("act", lambda e: e.copy(out=k_[:].rearrange("p (c d) -> p c d", c=4), in_=pT[:]), reads=[pT], writes=[k_])
                        S.dma("sp", SC["KM"][s0 + tt * 128:s0 + (tt + 1) * 128, :], k_[:], reads=[k_], writes=[SC["KM_b"][(s0 // 128) + tt]])
        with self.scope() as es:
            alT = self.sb(es, "b_alT", [128, NC, 64], F32)
            eeT = self.sb(es, "b_eeT", [128, NC, 64], F32)
            dl = self.sb(es, "b_dl", [128, 8, NC], F32)
            with self.scope() as g_es:
                fg = self.sb(g_es, "g_fg", [64, Sall], F32)
                w1 = self.sb(g_es, "g_w1", [64, Sall], F32)
                w2 = self.sb(g_es, "g_w2", [64, Sall], F32)
                onesr = self.sb(g_es, "g_ones", [64, 1], F32)
                gb = self.sb(g_es, "g_gb", [64, 2], F32)
                gco = self.sb(g_es, "g_co", [64, 4], F32)
                sel = self.sb(g_es, "g_sel", [64, 8, 128], F32)
                cm = self.sb(g_es, "g_cm", [64, NC], F32)
                rend = self.sb(g_es, "g_rend", [64, NC], F32)
                rst = self.sb(g_es, "g_rst", [64, NC], F32)
                dlt = self.sb(g_es, "g_dlt", [64, NC], F32)
                ac = self.sb(g_es, "g_ac", [64, 4], F32)
                pt8 = self.ps(g_es, "g_pt8", [128, 8, 64], F32)
                pdl = self.ps(g_es, "g_pdl", [128, NC], F32)
                S.dma("sp", fg[:], SC["GT"][1, :, :], reads=SC["GT_b"], writes=[fg])
                S.dma("sp", gb[:], I["k_gate_b"][:, :], writes=[gb])
                S.dma("sp", gco[:], I["k_gcoef"][:, :], writes=[gco])
                S.dma("sp", sel[:], I["k_sel"][:, :, :], writes=[sel])
                S.op("dve", lambda e: e.memset(onesr[:], 1.0), writes=[onesr])
                S.op("dve", lambda e: e.tensor_scalar(out=fg[:], in0=fg[:], scalar1=gb[:, 1:2], scalar2=None, op0=ALU.add), reads=[fg, gb], writes=[fg])
                S.op("dve", lambda e: e.scalar_tensor_tensor(out=w1[:], in0=fg[:], scalar=-1.0, in1=fg[:], op0=ALU.mult, op1=ALU.max), reads=[fg], writes=[w1])
                S.op("act", lambda e: e.activation(out=w1[:], in_=w1[:], func=AF.Exp, scale=-1.0), reads=[w1], writes=[w1])
                S.op("dve", lambda e: e.tensor_scalar(out=w1[:], in0=w1[:], scalar1=1.0, scalar2=None, op0=ALU.add), reads=[w1], writes=[w1])
                S.op("act", lambda e: e.activation(out=w1[:], in_=w1[:], func=AF.Ln), reads=[w1], writes=[w1])
                S.op("dve", lambda e: e.tensor_scalar(out=w2[:], in0=fg[:], scalar1=0.0, scalar2=None, op0=ALU.min), reads=[fg], writes=[w2])
                S.op("dve", lambda e: e.tensor_tensor(out=fg[:], in0=w2[:], in1=w1[:], op=ALU.subtract), reads=[w1, w2], writes=[fg])
                S.op("dve", lambda e: e.tensor_tensor_scan(out=w1[:], data0=onesr[:, 0:1].to_broadcast([64, Sall]), data1=fg[:], initial=0.0, op0=ALU.mult, op1=ALU.add), reads=[onesr, fg], writes=[w1])
                S.op("dve", lambda e: e.tensor_scalar(out=ac[:, 0:1], in0=w1[:, CTX - 1:CTX], scalar1=gco[:, 1:2], scalar2=None, op0=ALU.mult), reads=[w1, gco], writes=[ac])
                S.op("dve", lambda e: e.tensor_tensor(out=ac[:, 1:2], in0=w1[:, CTX - 1:CTX], in1=w1[:, Sall - 1:Sall], op=ALU.add), reads=[w1], writes=[ac])
                S.op("dve", lambda e: e.tensor_scalar(out=ac[:, 1:2], in0=ac[:, 1:2], scalar1=gco[:, 1:2], scalar2=None, op0=ALU.mult), reads=[ac, gco], writes=[ac])
                S.op("dve", lambda e: e.tensor_scalar(out=w1[:], in0=w1[:], scalar1=gco[:, 0:1], scalar2=None, op0=ALU.mult), reads=[w1, gco], writes=[w1])
                S.op("dve", lambda e: e.scalar_tensor_tensor(out=w1[:], in0=fg[:], scalar=gco[:, 1:2], in1=w1[:], op0=ALU.mult, op1=ALU.add), reads=[fg, gco, w1], writes=[w1])
                S.op("dve", lambda e: e.tensor_scalar(out=w1[:, 0:CTX], in0=w1[:, 0:CTX], scalar1=ac[:, 0:1], scalar2=None, op0=ALU.add), reads=[w1, ac], writes=[w1])
                S.op("dve", lambda e: e.tensor_scalar(out=w1[:, CTX:Sall], in0=w1[:, CTX:Sall], scalar1=ac[:, 1:2], scalar2=None, op0=ALU.add), reads=[w1, ac], writes=[w1])
                S.dma("sp", fg[:], SC["GT"][0, :, :], reads=SC["GT_b"], writes=[fg])
                S.op("dve", lambda e: e.scalar_tensor_tensor(out=w2[:], in0=fg[:], scalar=gb[:, 0:1], in1=w1[:], op0=ALU.add, op1=ALU.subtract),
                     reads=[fg, gb, w1], writes=[w2])
                S.op("dve", lambda e: e.tensor_reduce(out=cm[:], in_=w2[:].rearrange("p (c t) -> p c t", t=128), axis=AX.X, op=ALU.max), reads=[w2], writes=[cm])
                S.op("dve", lambda e: e.memset(rst[:], 0.0), writes=[rst])
                S.op("dve", lambda e: e.tensor_scalar(out=rend[0:32, 0:1], in0=cm[0:32, 0:1], scalar1=0.0, scalar2=None, op0=ALU.max), reads=[cm], writes=[rend])
                for c in range(1, NC):
                    S.op("dve", lambda e: e.tensor_tensor(out=rend[0:32, c:c + 1], in0=rend[0:32, c - 1:c], in1=cm[0:32, c:c + 1], op=ALU.max), reads=[rend, cm], writes=[rend])
                border = list(range(NCX - 1, -1, -1)) + list(range(NC - 1, NCX - 1, -1))
                S.op("dve", lambda e: e.tensor_scalar(out=rend[32:64, border[0]:border[0] + 1], in0=cm[32:64, border[0]:border[0] + 1], scalar1=0.0, scalar2=None, op0=ALU.max),
                     reads=[cm], writes=[rend])
                for pi in range(1, NC):
                    c, pc = border[pi], border[pi - 1]
                    S.op("dve", lambda e: e.tensor_tensor(out=rend[32:64, c:c + 1], in0=rend[32:64, pc:pc + 1], in1=cm[32:64, c:c + 1], op=ALU.max), reads=[rend, cm], writes=[rend])
                S.op("dve", lambda e: e.tensor_copy(out=rst[0:32, 1:NC], in_=rend[0:32, 0:NC - 1]), reads=[rend], writes=[rst])
                for pi in range(1, NC):
                    c, pc = border[pi], border[pi - 1]
                    if pi <= NCX or pi == NC:
                        S.op("dve", lambda e: e.tensor_copy(out=rst[32:64, c:c + 1], in_=rend[32:64, pc:pc + 1]), reads=[rend], writes=[rst])
                if NC - 1 > NCX:
                    S.op("dve", lambda e: e.tensor_copy(out=rst[32:64, NCX:NC - 1], in_=rend[32:64, NCX + 1:NC]), reads=[rend], writes=[rst])
                rb = rend[:].unsqueeze(2).to_broadcast([64, NC, 128])
                S.op("dve", lambda e: e.tensor_tensor(out=w2[:].rearrange("p (c t) -> p c t", t=128), in0=w2[:].rearrange("p (c t) -> p c t", t=128), in1=rb, op=ALU.subtract),
                     reads=[w2, rend], writes=[w2])
                S.op("act", lambda e: e.activation(out=w2[:], in_=w2[:], func=AF.Exp), reads=[w2], writes=[w2])
                S.op("dve", lambda e: e.tensor_tensor(out=w1[:].rearrange("p (c t) -> p c t", t=128), in0=w1[:].rearrange("p (c t) -> p c t", t=128), in1=rb, op=ALU.add),
                     reads=[w1, rend], writes=[w1])
                S.op("act", lambda e: e.activation(out=w1[:], in_=w1[:], func=AF.Exp, scale=-1.0), reads=[w1], writes=[w1])
                S.op("dve", lambda e: e.tensor_tensor(out=dlt[:], in0=rst[:], in1=rend[:], op=ALU.subtract), reads=[rst, rend], writes=[dlt])
                S.op("act", lambda e: e.activation(out=dlt[:], in_=dlt[:], func=AF.Exp), reads=[dlt], writes=[dlt])
                for (src, dstT) in ((w2, alT), (w1, eeT)):
                    for c0 in range(0, NC, 8):
                        nb = min(8, NC - c0)
                        for cc in range(nb):
                            S.op("pe", lambda e: e.transpose(out=pt8[:, cc, :], in_=src[:, (c0 + cc) * 128:(c0 + cc + 1) * 128], identity=self.identf[0:64, 0:64]),
                                 reads=[src, self.identf], writes=[pt8], sig=(cc == nb - 1))
                        S.op("act", lambda e: e.copy(out=dstT[:, c0:c0 + nb, :], in_=pt8[:, 0:nb, :]), reads=[pt8], writes=[dstT])
                for jd in range(8):
                    S.op("pe", lambda e: e.matmul(pdl[:], lhsT=sel[:, jd, :], rhs=dlt[:], start=True, stop=True), reads=[sel, dlt], writes=[pdl])
                    S.op("act", lambda e: e.copy(out=dl[:, jd, :], in_=pdl[:]), reads=[pdl], writes=[dl])
            msk = self.sb(es, "b_msk", [128, 2, 128], F32)
            S.dma("sp", msk[:], I["k_masks"].rearrange("d s t -> s d t"), writes=[msk])
            ng = self.sb(es, "b_ng", [128, 512], F32)
            S.dma("sp", ng[:], I["l0_mlstm_norm_g"].partition_broadcast(128), writes=[ng])
            kT = [self.sb(es, "b_kT%d" % i, [128, 4, 128], BF16) for i in range(4)]
            qT = [self.sb(es, "b_qT%d" % i, [128, 4, 128], BF16) for i in range(4)]
            kt = [self.sb(es, "b_kt%d" % i, [128, 8, 64], BF16) for i in range(4)]
            vt = [self.sb(es, "b_vt%d" % i, [128, 8, 64], BF16) for i in range(4)]
            vaug = [self.sb(es, "b_va%d" % i, [128, 8, 65], BF16) for i in range(4)]
            PT = [self.sb(es, "b_PT%d" % i, [128, 2, 128], BF16) for i in range(4)]
            chat2 = [[self.sb(es, "b_ch%d_%d" % (dd, i), [128, 130], F32) for i in range(4)] for dd in range(2)]
            ctb2 = [[self.sb(es, "b_cb%d_%d" % (dd, i), [128, 130], BF16) for i in range(4)] for dd in range(2)]
            hd = [self.sb(es, "b_hd%d" % i, [128, 8, 64], F32) for i in range(4)]
            hfl = [self.sb(es, "b_hf%d" % i, [128, 512], F32) for i in range(4)]
            ol = [self.sb(es, "b_ol%d" % i, [128, 512], F32) for i in range(4)]
            sq = self.sb(es, "b_sq", [128, 512], F32)
            dn = self.sb(es, "b_dn", [128, 8], F32)
            ssn = self.sb(es, "b_ssn", [128, 8], F32)
            mx = self.sb(es, "b_mx", [128, 512], BF16)
            mxT = [self.sb(es, "b_mxT%d" % i, [128, 4, 128], BF16) for i in range(4)]
            pST = [self.ps(es, "b_pST%d" % i, [128, 2, 128], F32) for i in range(2)]
            pND = [self.ps(es, "b_pND%d" % i, [128, 4, 65], F32) for i in range(2)]
            pU = [self.ps(es, "b_pU%d" % i, [128, 130], F32) for i in range(2)]
            pT4 = self.ps(es, "b_pT4", [128, 4, 128], BF16)
            kst = 0
            ku = 0
            it = 0
            orders = [list(range(NC)), list(range(NCX - 1, -1, -1)) + list(range(NC - 1, NCX - 1, -1))]
            stepof = [{c: i for i, c in enumerate(o)} for o in orders]
            for dd in range(2):
                for j in range(4):
                    S.op("dve", lambda e: e.memset(chat2[dd][j][:], 0.0), writes=[chat2[dd][j]])
            for step in range(NC):
                for d in range(2):
                    chat, ctb = chat2[d], ctb2[d]
                    c = orders[d][step]
                    lat = c >= NCX
                    first = stepof[d][c] < stepof[1 - d][c]
                    b_ = it % 4
                    it += 1
                    kt_, vt_, va_ = kt[b_], vt[b_], vaug[b_]
                    S.dma("sp", kt_[:].rearrange("p h d -> p (h d)"), SC["KM"][c * 128:(c + 1) * 128, :], reads=[SC["KM_b"][c]], writes=[kt_])
                    S.dma("sp", vt_[:].rearrange("p h d -> p (h d)"), SC["VM"][c * 128:(c + 1) * 128, :], reads=[SC["VM_b"][c]], writes=[vt_])
                    alc = alT[:, c, d * 32:d * 32 + 8]
                    S.op("dve", lambda e: e.tensor_tensor(out=va_[:, :, 0:64], in0=vt_[:], in1=alc.unsqueeze(2).to_broadcast([128, 8, 64]), op=ALU.mult), reads=[vt_, alT], writes=[va_])
                    S.op("dve", lambda e: e.tensor_copy(out=va_[:, :, 64:65], in_=alc.unsqueeze(2)), reads=[alT], writes=[va_])
                    for j in range(4):
                        S.op("pool", lambda e: e.tensor_scalar(out=ctb[j][:], in0=chat[j][:], scalar1=dl[:, d * 4 + j, c:c + 1], scalar2=0.125, op0=ALU.mult, op1=ALU.mult),
                             reads=[chat[j], dl], writes=[ctb[j]])
                    if lat:
                        tch = c - NCX
                        kT_, qT_ = kT[b_], qT[b_]
                        S.dma("sp", kT_[:], SC["KMT"][:, :, c * 128:(c + 1) * 128].rearrange("c p t -> p c t"), reads=[SC["KMT_b"][1 + tch // 4]], writes=[kT_])
                        S.dma("sp", qT_[:], SC["QMT"][:, :, tch * 128:(tch + 1) * 128].rearrange("c p t -> p c t"), reads=[SC["QMT_b"][tch // 4]], writes=[qT_])
                        for j in range(4):
                            st_ = pST[kst % 2]
                            kst += 1
                            for hh in range(2):
                                S.op("pe", lambda e: e.matmul(st_[:, hh, :], lhsT=kT_[hh * 64:(hh + 1) * 64, j, :], rhs=qT_[hh * 64:(hh + 1) * 64, j, :], start=True, stop=True),
                                     reads=[kT_, qT_], writes=[st_])
                            S.op("dve", lambda e: e.tensor_tensor(out=PT[j][:], in0=st_[:], in1=msk[:, d:d + 1, :].to_broadcast([128, 2, 128]), op=ALU.mult), reads=[st_, msk], writes=[PT[j]])
                        hd_ = hd[b_]
                        for gq in range(2):
                            nd = pND[gq]
                            for hl in range(4):
                                h = gq * 4 + hl
                                j, hh = h // 2, h % 2
                                S.op("pe", lambda e: e.matmul(nd[:, hl, :], lhsT=qT_[hh * 64:(hh + 1) * 64, j, :], rhs=ctb[j][hh * 64:(hh + 1) * 64, hh * 65:(hh + 1) * 65], start=True, stop=False),
                                     reads=[qT_, ctb[j]], writes=[nd])
                                S.op("pe", lambda e: e.matmul(nd[:, hl, :], lhsT=PT[j][:, hh, :], rhs=va_[:, h, :], start=False, stop=True), reads=[PT[j], va_], writes=[nd])
                            dsl = dn[:, gq * 4:(gq + 1) * 4]
                            S.op("act", lambda e: e.copy(out=dsl.unsqueeze(2), in_=nd[:, :, 64:65]), reads=[nd], writes=[dn])
                            S.op("dve", lambda e: e.scalar_tensor_tensor(out=dsl, in0=dsl, scalar=-1.0, in1=dsl, op0=ALU.mult, op1=ALU.max), reads=[dn], writes=[dn])
                            S.op("dve", lambda e: e.tensor_tensor(out=dsl, in0=dsl, in1=eeT[:, c, d * 32 + gq * 4:d * 32 + gq * 4 + 4], op=ALU.max), reads=[dn, eeT], writes=[dn])
                            S.op("dve", lambda e: e.reciprocal(out=dsl, in_=dsl), reads=[dn], writes=[dn])
                            S.op("dve", lambda e: e.tensor_tensor(out=hd_[:, gq * 4:(gq + 1) * 4, :], in0=nd[:, :, 0:64], in1=dsl.unsqueeze(2).to_broadcast([128, 4, 64]), op=ALU.mult),
                                 reads=[nd, dn], writes=[hd_])
                        trow = tch * 128
                        if first:
                            S.dma("sp", SC["HF"][trow:trow + 128, :], hd_[:].rearrange("p h d -> p (h d)"), reads=[hd_], writes=[SC["HF_b"][tch]])
                            o_ = ol[b_]
                            S.dma("sp", o_[:], SC["OM"][trow:trow + 128, :], reads=[SC["OM_b"][tch]], writes=[o_])
                            S.op("act", lambda e: e.activation(out=o_[:], in_=o_[:], func=AF.Exp, scale=-1.0), reads=[o_], writes=[o_])
                            S.op("act", lambda e: e.activation(out=o_[:], in_=o_[:], func=AF.Ln, bias=self.onec[:, 0:1]), reads=[o_, self.onec], writes=[o_])
                            S.op("act", lambda e: e.activation(out=o_[:], in_=o_[:], func=AF.Exp, scale=-1.0), reads=[o_], writes=[o_])
                            S.dma("sp", SC["OM"][trow:trow + 128, :], o_[:], reads=[o_], writes=[SC["OM_b"][tch]])
                        else:
                            hf_, o_ = hfl[b_], ol[b_]
                            S.dma("sp", hf_[:], SC["HF"][trow:trow + 128, :], reads=[SC["HF_b"][tch]], writes=[hf_])
                            S.dma("sp", o_[:], SC["OM"][trow:trow + 128, :], reads=[SC["OM_b"][tch]], writes=[o_])
                            hflat = hd_[:].rearrange("p h d -> p (h d)")
                            S.op("dve", lambda e: e.tensor_tensor(out=hf_[:], in0=hf_[:], in1=hflat, op=ALU.add), reads=[hf_, hd_], writes=[hf_])
                            S.op("act", lambda e: e.activation(out=sq[:], in_=hf_[:], func=AF.Square), reads=[hf_], writes=[sq])
                            S.op("dve", lambda e: e.tensor_reduce(out=ssn[:], in_=sq[:].rearrange("p (h d) -> p h d", d=64), axis=AX.X, op=ALU.add), reads=[sq], writes=[ssn])
                            self.rstd_cols(ssn, 8, 1.0 / 64)
                            S.op("dve", lambda e: e.tensor_tensor(out=hf_[:].rearrange("p (h d) -> p h d", d=64), in0=hf_[:].rearrange("p (h d) -> p h d", d=64),
                                                                  in1=ssn[:].unsqueeze(2).to_broadcast([128, 8, 64]), op=ALU.mult), reads=[hf_, ssn], writes=[hf_])
                            S.op("dve", lambda e: e.tensor_tensor(out=hf_[:], in0=hf_[:], in1=ng[:], op=ALU.mult), reads=[hf_, ng], writes=[hf_])
                            S.op("dve", lambda e: e.tensor_tensor(out=mx[:], in0=hf_[:], in1=o_[:], op=ALU.mult), reads=[hf_, o_], writes=[mx])
                            mT_ = mxT[b_]
                            for cc in range(4):
                                S.op("pe", lambda e: e.transpose(out=pT4[:, cc, :], in_=mx[:, cc * 128:(cc + 1) * 128], identity=self.identb[:]), reads=[mx, self.identb], writes=[pT4], sig=(cc == 3))
                            S.op("act", lambda e: e.copy(out=mT_[:], in_=pT4[:]), reads=[pT4], writes=[mT_])
                            S.dma("sp", SC["MIXT"][0:4, :, trow:trow + 128].rearrange("c p t -> p c t"), mT_[:], reads=[mT_], writes=[SC["MIXm_b"][tch]])
                    for j in range(4):
                        u_ = pU[ku % 2]
                        ku += 1
                        S.op("pe", lambda e: e.matmul(u_[:], lhsT=kt_[:, 2 * j:2 * j + 2, :].rearrange("p h d -> p (h d)"), rhs=va_[:, 2 * j:2 * j + 2, :].rearrange("p h d -> p (h d)"), start=True, stop=True),
                             reads=[kt_, va_], writes=[u_])
                        S.op("dve", lambda e: e.scalar_tensor_tensor(out=chat[j][:], in0=chat[j][:], scalar=dl[:, d * 4 + j, c:c + 1], in1=u_[:], op0=ALU.mult, op1=ALU.add),
                             reads=[chat[j], dl, u_], writes=[chat[j]])


    def attn_phase(self, SC):
        S, nc, T = self.S, self.nc, self.T
        I = self.inp
        Sall = CTX + T
        NS = Sall // 128
        with self.scope() as es:
            cb = self.sb(es, "c_cb", [128, 8], F32)
            nlam = self.sb(es, "c_nlam", [128, 1], F32)
            gd = self.sb(es, "c_gd", [128, 4], F32)
            ones_k = self.sb(es, "c_onesk", [128, 128], BF16)
            ones_d = self.sb(es, "c_onesd", [128, 128], BF16)
            S.op("dve", lambda e: e.memset(ones_k[:], 1.0), writes=[ones_k])
            S.op("dve", lambda e: e.memset(ones_d[:], 1.0 / 128), writes=[ones_d])
            S.dma("sp", gd[:], I["l0_diff_norm_g"].rearrange("(c p) -> p c", p=128), writes=[gd], allow_slow_non_contiguous=True)
            S.op("dve", lambda e: e.tensor_scalar(out=gd[:], in0=gd[:], scalar1=0.8, scalar2=None, op0=ALU.mult), reads=[gd], writes=[gd])
            with self.scope() as les:
                pa = self.ps(les, "c_pa", [8, 128], F32)
                pb = self.ps(les, "c_pb", [8, 128], F32)
                pc = self.ps(les, "c_pc", [128, 8], F32)
                pl = self.ps(les, "c_pl", [128, 1], F32)
                qm = self.sb(les, "c_qm", [8, 2], F32)
                dg = self.sb(les, "c_dg", [8, 8], F32)
                o8 = self.sb(les, "c_o8", [8, 128], F32)
                lv = self.sb(les, "c_lv", [1, 4, 64], F32)
                lr = self.sb(les, "c_lr", [1, 4], F32)
                S.op("pe", lambda e: e.transpose(out=pa[:], in_=self.q2max[:], identity=self.identf[:]), reads=[self.q2max, self.identf], writes=[pa])
                S.op("pe", lambda e: e.transpose(out=pb[:], in_=self.k2max[:], identity=self.identf[:]), reads=[self.k2max, self.identf], writes=[pb])
                S.op("dve", lambda e: e.reduce_max(out=qm[:, 0:1], in_=pa[:], axis=AX.X), reads=[pa], writes=[qm])
                S.op("dve", lambda e: e.reduce_max(out=qm[:, 1:2], in_=pb[:], axis=AX.X), reads=[pb], writes=[qm])
                S.op("dve", lambda e: e.tensor_tensor(out=qm[:, 0:1], in0=qm[:, 0:1], in1=qm[:, 1:2], op=ALU.mult), reads=[qm], writes=[qm])
                S.op("act", lambda e: e.activation(out=qm[:, 0:1], in_=qm[:, 0:1], func=AF.Ln, bias=self.epsc[0:8, 0:1]), reads=[qm, self.epsc], writes=[qm])
                S.op("act", lambda e: e.activation(out=qm[:, 0:1], in_=qm[:, 0:1], func=AF.Exp, scale=0.5), reads=[qm], writes=[qm])
                S.op("dve", lambda e: e.tensor_scalar(out=dg[:], in0=self.identf[0:8, 0:8], scalar1=qm[:, 0:1], scalar2=-0.125, op0=ALU.mult, op1=ALU.mult), reads=[self.identf, qm], writes=[dg])
                S.op("dve", lambda e: e.memset(o8[:], 1.0), writes=[o8])
                S.op("pe", lambda e: e.matmul(pc[:], lhsT=o8[:], rhs=dg[:], start=True, stop=True), reads=[o8, dg], writes=[pc])
                S.op("dve", lambda e: e.tensor_copy(out=cb[:], in_=pc[:]), reads=[pc], writes=[cb])
                for i, nm in enumerate(["l0_lambda_q1", "l0_lambda_k1", "l0_lambda_q2", "l0_lambda_k2"]):
                    S.dma("sp", lv[:, i, :], I[nm].unsqueeze(0), writes=[lv])
                S.op("dve", lambda e: e.tensor_tensor(out=lv[:, 0, :], in0=lv[:, 0, :], in1=lv[:, 1, :], op=ALU.mult), reads=[lv], writes=[lv])
                S.op("dve", lambda e: e.tensor_tensor(out=lv[:, 2, :], in0=lv[:, 2, :], in1=lv[:, 3, :], op=ALU.mult), reads=[lv], writes=[lv])
                S.op("dve", lambda e: e.reduce_sum(out=lr[:, 0:1], in_=lv[:, 0, :], axis=AX.X), reads=[lv], writes=[lr])
                S.op("dve", lambda e: e.reduce_sum(out=lr[:, 1:2], in_=lv[:, 2, :], axis=AX.X), reads=[lv], writes=[lr])
                S.op("act", lambda e: e.activation(out=lr[:, 0:2], in_=lr[:, 0:2], func=AF.Exp), reads=[lr], writes=[lr])
                S.op("dve", lambda e: e.tensor_tensor(out=lr[:, 2:3], in0=lr[:, 1:2], in1=lr[:, 0:1], op=ALU.subtract), reads=[lr], writes=[lr])
                S.op("dve", lambda e: e.tensor_scalar(out=lr[:, 2:3], in0=lr[:, 2:3], scalar1=-0.2, scalar2=None, op0=ALU.add), reads=[lr], writes=[lr])
                S.op("pe", lambda e: e.matmul(pl[:], lhsT=self.ones1[:], rhs=lr[:, 2:3], start=True, stop=True), reads=[self.ones1, lr], writes=[pl])
                S.op("dve", lambda e: e.tensor_copy(out=nlam[:], in_=pl[:]), reads=[pl], writes=[nlam])
            cbh = self.sb(es, "c_cbh", [128, 4], F32)
            cb2 = cb[:].rearrange("p (h b) -> p h b", b=2)
            S.op("dve", lambda e: e.tensor_tensor(out=cbh[:].unsqueeze(2), in0=cb2[:, :, 0:1], in1=cb2[:, :, 1:2], op=ALU.min), reads=[cb], writes=[cbh])
            Kh = [self.sb(es, "c_Kh%d" % i, [128, Sall], BF16) for i in range(2)]
            Vh = [self.sb(es, "c_Vh%d" % i, [128, NS, 128], BF16) for i in range(2)]
            Q = [self.sb(es, "c_Q%d" % i, [128, 512], BF16) for i in range(2)]
            PT = [self.sb(es, "c_PT%d" % i, [128, 2, 512], BF16) for i in range(3)]
            rr = self.sb(es, "c_rr", [128, 512], F32)
            lacc = self.sb(es, "c_lacc", [128, 2, 512], F32)
            ones_f = self.sb(es, "c_onesf", [128, 128], F32)
            S.op("dve", lambda e: e.memset(ones_f[:], 1.0), writes=[ones_f])
            O = [self.sb(es, "c_O%d" % i, [128, 512], F32) for i in range(2)]
            sqb = self.sb(es, "c_sqb", [128, 512], BF16)
            of = [self.sb(es, "c_of%d" % i, [128, 512], BF16) for i in range(2)]
            pS = []
            for i in range(2):
                t_ = es.enter_context(self.nc.psum_tensor(self._uniq("c_pS%d" % i), [128, 1024], F32))
                pS.append(Tile(t_[:].rearrange("p (a b) -> p a b", b=512), "c_pS%d" % i))
            pO = [self.ps(es, "c_pO%d" % i, [128, 512], F32) for i in range(2)]
            pL = [self.ps(es, "c_pL%d" % i, [128, 512], F32) for i in range(2)]
            kq = 0
            kk = 0
            for h in range(4):
                K_, V_ = Kh[h % 2], Vh[h % 2]
                S.dma("sp", K_[:], SC["KD"][h, :, :], reads=SC["KD_b"], writes=[K_])
                S.dma("sp", V_[:], SC["VD"][:, h * 128:(h + 1) * 128].rearrange("(n p) d -> p n d", p=128), reads=SC["VD_b"], writes=[V_])
                for qt in range(T // 512):
                    Q_ = Q[kq % 2]
                    of_ = of[kq % 2]
                    kq += 1
                    S.dma("sp", Q_[:], SC["QD"][h, :, qt * 512:(qt + 1) * 512], reads=[SC["QD_b"][1 + qt]], writes=[Q_])

                    def scores(kt, slot):
                        ps_ = pS[slot % 2]
                        for b in range(2):
                            S.op("pe", lambda e: e.matmul(ps_[:, b, :], lhsT=K_[b * 64:(b + 1) * 64, kt * 128:(kt + 1) * 128], rhs=Q_[b * 64:(b + 1) * 64, :], start=True, stop=True),
                                 reads=[K_, Q_], writes=[ps_], sig=(b == 1))

                    def expo(kt, slot):
                        ps_, pt_ = pS[slot % 2], PT[slot % 3]
                        S.op("act", lambda e: e.activation(out=pt_[:], in_=ps_[:], func=AF.Exp, scale=0.125, bias=cbh[:, h:h + 1]), reads=[ps_, cbh], writes=[pt_])

                    def consume(kt, slot):
                        pt_ = PT[slot % 3]
                        on_pe = (kt % 4 == 0)
                        for b in range(2):
                            S.op("pe", lambda e: e.matmul(pO[b][:], lhsT=V_[:, kt, :], rhs=pt_[:, b, :], start=(kt == 0), stop=(kt == NS - 1)), reads=[V_, pt_], writes=[pO[b]],
                                 sig=((not on_pe) and b == 1))
                        if on_pe:
                            for b in range(2):
                                S.op("pe", lambda e: e.matmul(pL[b][:], lhsT=ones_k[:], rhs=pt_[:, b, :], start=(kt == 0), stop=False), reads=[ones_k, pt_], writes=[pL[b]], sig=(b == 1))
                        elif kt == 1:
                            S.op("dve", lambda e: e.tensor_copy(out=lacc[:], in_=pt_[:]), reads=[pt_], writes=[lacc])
                        else:
                            S.op("dve", lambda e: e.tensor_tensor(out=lacc[:], in0=pt_[:], in1=lacc[:], op=ALU.add), reads=[pt_, lacc], writes=[lacc])

                    scores(0, kk)
                    if NS > 1:
                        scores(1, kk + 1)
                    for kt in range(NS):
                        expo(kt, kk + kt)
                        if kt + 2 < NS:
                            scores(kt + 2, kk + kt + 2)
                        consume(kt, kk + kt)
                    kk += NS
                    for b in range(2):
                        S.op("pe", lambda e: e.matmul(pL[b][:], lhsT=ones_f[:], rhs=lacc[:, b, :], start=False, stop=True), reads=[ones_f, lacc], writes=[pL[b]], sig=True)
                        S.op("dve", lambda e: e.reciprocal(out=rr[:], in_=pL[b][:]), reads=[pL[b]], writes=[rr])
                        S.op("dve", lambda e: e.tensor_tensor(out=O[b][:], in0=pO[b][:], in1=rr[:], op=ALU.mult), reads=[pO[b], rr], writes=[O[b]])
                    pM = pL[0]
                    S.op("dve", lambda e: e.scalar_tensor_tensor(out=O[0][:], in0=O[1][:], scalar=nlam[:, 0:1], in1=O[0][:], op0=ALU.mult, op1=ALU.add), reads=[O[1], nlam, O[0]], writes=[O[0]])
                    S.op("act", lambda e: e.activation(out=sqb[:], in_=O[0][:], func=AF.Square), reads=[O[0]], writes=[sqb])
                    S.op("pe", lambda e: e.matmul(pM[:], lhsT=ones_d[:], rhs=sqb[:], start=True, stop=True), reads=[ones_d, sqb], writes=[pM])
                    S.op("act", lambda e: e.activation(out=rr[:], in_=pM[:], func=AF.Ln, bias=self.epsc[:, 0:1]), reads=[pM, self.epsc], writes=[rr])
                    S.op("act", lambda e: e.activation(out=rr[:], in_=rr[:], func=AF.Exp, scale=-0.5), reads=[rr], writes=[rr])
                    S.op("dve", lambda e: e.scalar_tensor_tensor(out=of_[:], in0=O[0][:], scalar=gd[:, h:h + 1], in1=rr[:], op0=ALU.mult, op1=ALU.mult), reads=[O[0], gd, rr], writes=[of_])
                    S.dma("sp", SC["MIXT"][4 + h, :, qt * 512:(qt + 1) * 512], of_[:], reads=[of_], writes=[SC["MIXd_b"][h * (T // 512) + qt]])

    def wout_phase(self, x, x_bufs, xout, xout_bufs, bc, SC, wout, wout_b):
        S, nc, T = self.S, self.nc, self.T
        with self.scope() as es:
            wo = self.sb(es, "d_wo", [128, 8, D], BF16)
            S.dma("sp", wo[:], wout.rearrange("(c p) n -> p c n", p=128), reads=wout_b, writes=[wo])
            mT = [self.sb(es, "d_mT%d" % i, [128, 8, 128], BF16) for i in range(4)]
            xt = [self.sb(es, "d_xt%d" % i, [128, D], F32) for i in range(4)]
            ys = [self.sb(es, "d_y%d" % i, [128, D], F32) for i in range(4)]
            xos = [self.sb(es, "d_xo%d" % i, [128, D], F32) for i in range(4)]
            junks = [self.sb(es, "d_junk%d" % i, [128, D], BF16) for i in range(4)]
            sss = [self.sb(es, "d_ss%d" % i, [128, 4], F32) for i in range(4)]
            py = [self.ps(es, "d_py%d" % i, [128, 512], F32) for i in range(2)]
            ky = 0
            nq = T // 512
            for ti in range(T // 128):
                m_, x_ = mT[ti % 4], xt[ti % 4]
                y, xo, junk, ss = ys[ti % 4], xos[ti % 4], junks[ti % 4], sss[ti % 4]
                rd = [SC["MIXm_b"][ti]] + [SC["MIXd_b"][h * nq + ti // 4] for h in range(4)]
                S.dma("sp", m_[:], SC["MIXT"][:, :, ti * 128:(ti + 1) * 128].rearrange("c p t -> p c t"), reads=rd, writes=[m_])
                S.dma("sp", x_[:], x[ti * 128:(ti + 1) * 128, :], reads=[x_bufs[ti]], writes=[x_])
                for dg in range(2):
                    y_ = py[ky % 2]
                    ky += 1
                    for c in range(8):
                        S.op("pe", lambda e: e.matmul(y_[:], lhsT=m_[:, c, :], rhs=wo[:, c, dg * 512:(dg + 1) * 512], start=(c == 0), stop=(c == 7)), reads=[m_, wo], writes=[y_])
                    S.op("act", lambda e: e.copy(out=y[:, dg * 512:(dg + 1) * 512], in_=y_[:]), reads=[y_], writes=[y])
                self.post_norm_residual(y, x_, bc["ggt_m"], ss, junk, xo)
                S.dma("sp", xout[ti * 128:(ti + 1) * 128, :], xo[:], reads=[xo], writes=[xout_bufs[ti]])

    def layer0_scratch(self):
        T = self.T
        Sall = CTX + T
        NTL, NS, NG = T // 128, Sall // 128, T // 512
        SC = {}
        d = self.dscr
        SC["PQ"] = d("s_PQ", [4, 128, T + 2], F32); SC["PK"] = d("s_PK", [4, 128, T + 2], F32); SC["PKc"] = d("s_PKc", [4, 128, CTX + 2], F32)
        SC["QMT"] = d("s_QMT", [4, 128, T], BF16, dbg=True); SC["KMT"] = d("s_KMT", [4, 128, Sall], BF16, dbg=True); SC["KM"] = d("s_KM", [Sall, 512], BF16)
        SC["VM"] = d("s_VM", [Sall, 512], BF16, dbg=True); SC["OM"] = d("s_OM", [T, 512], F32, dbg=True); SC["VD"] = d("s_VD", [Sall, 512], BF16)
        SC["QD"] = d("s_QD", [4, 128, T], BF16, dbg=True); SC["KD"] = d("s_KD", [4, 128, Sall], BF16, dbg=True); SC["GT"] = d("s_GT", [2, 64, Sall], F32, dbg=True)
        SC["HF"] = d("s_HF", [T, 512], F32, dbg=True); SC["MIXT"] = d("s_MIXT", [8, 128, T], BF16, dbg=True)
        SC["pad_b"] = Buf("pad"); SC["PKc_b"] = Buf("PKc")
        SC["PQ_b"] = [Buf("PQ%d" % i) for i in range(NG)]; SC["PK_b"] = [Buf("PK%d" % i) for i in range(NG)]
        SC["QMT_b"] = [Buf("QMT%d" % i) for i in range(NG)]; SC["KMT_b"] = [Buf("KMT%d" % i) for i in range(NG + 1)]
        SC["KM_b"] = [Buf("KM%d" % i) for i in range(NS)]; SC["VM_b"] = [Buf("VM%d" % i) for i in range(NS)]; SC["VD_b"] = [Buf("VD%d" % i) for i in range(NS)]
        SC["OM_b"] = [Buf("OM%d" % i) for i in range(NTL)]; SC["HF_b"] = [Buf("HF%d" % i) for i in range(NTL)]
        SC["QD_b"] = [Buf("QD%d" % i) for i in range(NG + 1)]; SC["KD_b"] = [Buf("KD%d" % i) for i in range(NG + 1)]; SC["GT_b"] = [Buf("GT%d" % i) for i in range(NG + 1)]
        SC["MIXm_b"] = [Buf("MXm%d" % i) for i in range(NTL)]; SC["MIXd_b"] = [Buf("MXd%d" % i) for i in range(4 * NG)]
        return SC

    def build(self):
        nc, T = self.nc, self.T
        x = self.din("x", [T, D])
        self.din("c", [D])
        self.din("ctx", [CTX, D])
        self.din("c_ctx", [D])
        shapes = {
            "l0_mod_w": [D, 6 * D], "l0_mod_b": [6 * D], "l0_mix_pre_g": [D], "l0_mix_post_g": [D], "l0_w_in": [D, IN0],
            "l0_mlstm_gate_b": [32], "l0_mlstm_conv_w": [3, D], "l0_mlstm_norm_g": [512], "l0_lambda_q1": [64], "l0_lambda_k1": [64],
            "l0_lambda_q2": [64], "l0_lambda_k2": [64], "l0_diff_norm_g": [512], "l0_w_out": [D, D], "l0_ffn_pre_g": [D],
            "l0_ffn_post_g": [D], "l0_ffn_w1": [D, DFF], "l0_ffn_w3": [D, DFF], "l0_ffn_w2": [DFF, D],
            "l1_mod_w": [D, 6 * D], "l1_mod_b": [6 * D], "l1_mix_pre_g": [D], "l1_mix_post_g": [D], "l1_conv_pw1_w": [D, 2 * D],
            "l1_conv_pw1_b": [2 * D], "l1_conv_dw_w": [31, D], "l1_conv_dw_b": [D], "l1_conv_ln_g": [D], "l1_conv_ln_b": [D],
            "l1_conv_pw2_w": [D, D], "l1_conv_pw2_b": [D], "l1_ffn_pre_g": [D], "l1_ffn_post_g": [D], "l1_router_w": [D, NEXP],
            "l1_moe_w1": [NEXP * D, DFF], "l1_moe_w3": [NEXP * D, DFF], "l1_moe_w2": [NEXP * DFF, D],
        }
        for n, s in shapes.items():
            self.din(n, s)
        ident_in = self.din("k_ident", [128, 128])
        self.din("k_wfm", [D, 1024]); self.din("k_wtm", [D, 2688]); self.din("k_gate_b", [64, 2]); self.din("k_gcoef", [64, 4])
        self.din("k_sel", [64, 8, 128]); self.din("k_masks", [2, 128, 128]); self.din("k_rope", [T, 64])
        self.din("k_tri", [128, 128]); self.din("k_sbpos", [64]); self.din("k_wbase", [128, 6])
        out = nc.dram_tensor("out", [T, D], F32, kind="ExternalOutput").ap()
        I = self.inp
        NTL = T // 128
        with ExitStack() as es:
            self.S = S = Sched(nc, es)
            self.identf = self.sb(es, "identf", [128, 128], F32)
            self.identb = self.sb(es, "identb", [128, 128], BF16)
            self.ones1 = self.sb(es, "ones1", [1, 128], F32)
            self.onesb = self.sb(es, "onesb", [128, 128], BF16)
            self.epsc = self.sb(es, "epsc", [128, 1], F32)
            self.onec = self.sb(es, "onec", [128, 1], F32)
            S.dma("sp", self.identf[:], ident_in[:, :], writes=[self.identf])
            S.op("dve", lambda e: e.tensor_copy(out=self.identb[:], in_=self.identf[:]), reads=[self.identf], writes=[self.identb])
            S.op("dve", lambda e: e.memset(self.ones1[:], 1.0), writes=[self.ones1])
            S.op("dve", lambda e: e.memset(self.onesb[:], 1.0 / 1024), writes=[self.onesb])
            S.op("dve", lambda e: e.memset(self.epsc[:], EPS), writes=[self.epsc])
            S.op("dve", lambda e: e.memset(self.onec[:], 1.0), writes=[self.onec])
            W = {}
            self.q2max = self.sb(es, "q2max", [128, 8], F32)
            self.k2max = self.sb(es, "k2max", [128, 8], F32)
            if 0 in self.layers:
                W["wo"], W["wo_b"] = self.convert(I["l0_w_out"], "wb_wo", D, D)
                W["f1"], W["f1_b"] = self.convert(I["l0_ffn_w1"], "wb_f1", D, DFF)
                W["f3"], W["f3_b"] = self.convert(I["l0_ffn_w3"], "wb_f3", D, DFF)
                W["f2"], W["f2_b"] = self.convert(I["l0_ffn_w2"], "wb_f2", DFF, D)
            if 1 in self.layers:
                W["pw1"], W["pw1_b"] = self.convert(I["l1_conv_pw1_w"], "wb_pw1", D, 2 * D)
                W["pw2"], W["pw2_b"] = self.convert(I["l1_conv_pw2_w"], "wb_pw2", D, D)
                if 0 not in self.layers:
                    W.update(self.convert_moe(I["l1_moe_w1"], I["l1_moe_w3"], I["l1_moe_w2"]))
            xa = self.dscr("xa_scr", [T, D], F32, dbg=True)
            xb = self.dscr("xb_scr", [T, D], F32, dbg=True)
            xa_b = [Buf("xa%d" % i) for i in range(NTL)]
            xb_b = [Buf("xb%d" % i) for i in range(NTL)]
            x_b = [Buf("x%d" % i) for i in range(NTL)]
            out_b = [Buf("out%d" % i) for i in range(NTL)]
            cur, cur_b = x, x_b
            if 0 in self.layers:
                SC = self.layer0_scratch()
                with self.scope() as les:
                    bc = self.adaln(les, I["c"], I["l0_mod_w"], I["l0_mod_b"], None, [
                        ("gmod_m", 1, "gmod", I["l0_mix_pre_g"]), ("shift_m", 0, "shift", None), ("ggt_m", 2, "ggt", I["l0_mix_post_g"])])
                    bc.update(self.adaln(les, I["c_ctx"], I["l0_mod_w"], I["l0_mod_b"], None, [
                        ("gmod_c", 1, "gmod", I["l0_mix_pre_g"]), ("shift_c", 0, "shift", None)]))
                    self.proj_phase(x, x_b, bc, SC)
                    self.mlstm_phase(SC)
                    if 1 in self.layers:
                        W.update(self.convert_moe(I["l1_moe_w1"], I["l1_moe_w3"], I["l1_moe_w2"]))
                    self.attn_phase(SC)
                    self.wout_phase(x, x_b, xa, xa_b, bc, SC, W["wo"], W["wo_b"])
                with self.scope() as les:
                    bc = self.adaln(les, I["c"], I["l0_mod_w"], I["l0_mod_b"], None, [
                        ("gmod_f", 4, "gmod", I["l0_ffn_pre_g"]), ("shift_f", 3, "shift", None), ("ggt_f", 5, "ggt", I["l0_ffn_post_g"])])
                    dst, dst_b = (xb, xb_b) if 1 in self.layers else (out, out_b)
                    self.ffn_phase("f_", xa, xa_b, dst, dst_b, bc, W["f1"], W["f1_b"], W["f3"], W["f3_b"], W["f2"], W["f2_b"], 1)
                cur, cur_b = xb, xb_b
            if 1 in self.layers:
                with self.scope() as les:
                    bc = self.adaln(les, I["c"], I["l1_mod_w"], I["l1_mod_b"], None, [
                        ("gmod_m", 1, "gmod", I["l1_mix_pre_g"]), ("shift_m", 0, "shift", None), ("ggt_m", 2, "ggt", I["l1_mix_post_g"]),
                        ("gmod_f", 4, "gmod", I["l1_ffn_pre_g"]), ("shift_f", 3, "shift", None), ("ggt_f", 5, "ggt", I["l1_ffn_post_g"])])
                    self.conv_phase(cur, cur_b, xa, xa_b, bc, W)
                    self.moe_phase(xa, xa_b, out, out_b, bc, W)
            S.finish(out_b, "sp")
            self.stats = (dict(S.n_inst), S.n_wait, dict(S.cnt))
        return nc


_CACHE = {}


def _in_map(inputs, b):
    m = {}
    for k, v in inputs.items():
        v = np.asarray(v, dtype=np.float32)
        if k in ("x", "c", "ctx"):
            m[k] = np.ascontiguousarray(v[b])
        elif k in ("l1_moe_w1", "l1_moe_w3", "l1_moe_w2"):
            m[k] = np.ascontiguousarray(v.reshape(v.shape[0] * v.shape[1], v.shape[2]))
        else:
            m[k] = np.ascontiguousarray(v)
    m["k_ident"] = np.eye(128, dtype=np.float32)
    m.update(_consts(np.asarray(inputs["l0_w_in"], np.float32), np.asarray(inputs["l0_mlstm_gate_b"], np.float32), m["x"].shape[0]))
    return m


def _consts(w_in, gate_b, T):
    c = {}
    c["k_wfm"] = np.ascontiguousarray(w_in[:, 0:1024])
    g = np.zeros((D, 128), np.float32)
    g[:, 0:8] = w_in[:, 2048:2056]
    g[:, 32:40] = w_in[:, 2064:2072]
    g[:, 64:72] = w_in[:, 2056:2064]
    g[:, 96:104] = w_in[:, 2072:2080]
    c["k_wtm"] = np.ascontiguousarray(np.concatenate([w_in[:, 1024:1536], w_in[:, 1536:2048], w_in[:, 3104:3616], w_in[:, 2080:2592], w_in[:, 2592:3104], g], axis=1))
    gb = np.zeros((64, 2), np.float32)
    gb[0:8, 0] = gate_b[0:8]
    gb[32:40, 0] = gate_b[16:24]
    gb[0:8, 1] = gate_b[8:16]
    gb[32:40, 1] = gate_b[24:32]
    c["k_gate_b"] = gb
    co = np.zeros((64, 4), np.float32)
    co[0:32, 0] = 1.0
    co[32:64, 0] = -1.0
    co[32:64, 1] = 1.0
    c["k_gcoef"] = co
    sel = np.zeros((64, 8, 128), np.float32)
    for d in range(2):
        for j in range(4):
            sel[d * 32 + 2 * j, d * 4 + j, 0:64] = 1.0
            sel[d * 32 + 2 * j + 1, d * 4 + j, 64:128] = 1.0
    c["k_sel"] = sel
    sidx = np.arange(128)
    mk = np.zeros((2, 128, 128), np.float32)
    mk[0] = (sidx[:, None] <= sidx[None, :]) * 0.125
    mk[1] = (sidx[:, None] >= sidx[None, :]) * 0.125
    c["k_masks"] = mk
    c["k_tri"] = (sidx[:, None] < sidx[None, :]).astype(np.float32)
    c["k_sbpos"] = (np.arange(64) * 512).astype(np.float32)
    c["k_wbase"] = (np.arange(6)[None, :] * 128 + np.arange(128)[:, None]).astype(np.float32)
    t = np.arange(T)
    inv = (np.float32(10000.0) ** (-np.arange(0, 32, 2, dtype=np.float32) / np.float32(32))).astype(np.float32)
    ar = (t // 64).astype(np.float32)[:, None] * inv
    ac = (t % 64).astype(np.float32)[:, None] * inv
    c["k_rope"] = np.concatenate([np.cos(ar), np.sin(ar), np.cos(ac), np.sin(ac)], axis=1).astype(np.float32)
    return c


def kernel(**inputs):
    B, T, _ = inputs["x"].shape
    key = (T,)
    if key not in _CACHE:
        _CACHE[key] = K(T).build()
    nc = _CACHE[key]
    in_maps = [_in_map(inputs, b) for b in range(B)]
    res = run_bass_kernel_spmd(nc, in_maps, core_ids=list(range(B)))
    return np.stack([np.asarray(r["out"], dtype=np.float32) for r in res.results], axis=0)
```

```python
import numpy as np
from contextlib import ExitStack
import concourse.bass as bass
import concourse.mybir as mybir
from concourse.bass_utils import run_bass_kernel_spmd

F32 = mybir.dt.float32
BF16 = mybir.dt.bfloat16
AF = mybir.ActivationFunctionType
ALU = mybir.AluOpType
AX = mybir.AxisListType

D = 1024
DFF = 2816
NEXP = 8
CTX = 256
EPS = 1e-6
IN0 = 3616
NCH_FF = DFF // 128


class Buf:
    __slots__ = ("name", "w", "r")

    def __init__(self, name):
        self.name = name
        self.w = None
        self.r = []


class Tile:
    def __init__(self, t, name):
        self.t = t
        self.b = Buf(name)

    def __getitem__(self, k):
        return self.t[k]


class _PEProxy:
    def __init__(self, eng):
        self.eng = eng
        self.stop = True

    def matmul(self, *a, **kw):
        self.stop = kw.get("stop", True) is not False
        return self.eng.matmul(*a, **kw)

    def transpose(self, *a, **kw):
        return self.eng.transpose(*a, **kw)


class Sched:
    EPOCH = 20000
    NPOOL = 24
    STRICT = True

    def __init__(self, nc, es):
        self.nc = nc
        self.es = es
        self.engs = {"pe": nc.tensor, "act": nc.scalar, "dve": nc.vector, "pool": nc.gpsimd, "sp": nc.sync}
        self.cnt = {e: 0 for e in self.engs}
        self.pend = {e: [] for e in self.engs}
        self.sems = {}
        self.seen = {e: {} for e in self.engs}
        self.dma_pool = {}
        self.dma_idx = {q: 0 for q in self.engs}
        self.n_inst = {e: 0 for e in self.engs}
        self.n_wait = 0

    def _eng_sem(self, e, epoch):
        k = (e, epoch)
        if k not in self.sems:
            self.sems[k] = self.es.enter_context(self.nc.semaphore("s_%s_%d" % (e, epoch)))
        return self.sems[k]

    def _sem_of(self, key):
        if key[0] == "dma":
            return self.dma_pool[key[1]][key[2]][0]
        return self._eng_sem(key[0], key[1])

    def _wait(self, e, dep):
        key, val = dep
        if self.seen[e].get(key, 0) >= val:
            return
        self.engs[e].wait_ge(self._sem_of(key), val)
        self.seen[e][key] = val
        self.n_wait += 1

    def _need(self, e, d):
        if d[0][0] != e:
            return True
        if not self.STRICT:
            return False
        c = self.cnt[e]
        epoch, idx = divmod(c, self.EPOCH)
        return not (d[0][1] == epoch and d[1] > idx)

    def _deps(self, e, reads, writes):
        for b in reads:
            if b.w is not None:
                self._wait(e, b.w)
        for b in writes:
            if b.w is not None and self._need(e, b.w):
                self._wait(e, b.w)
            for d in b.r:
                if self._need(e, d):
                    self._wait(e, d)

    @staticmethod
    def _record(stamp, reads, writes):
        for b in reads:
            b.r.append(stamp)
        for b in writes:
            b.w = stamp
            b.r = []

    def op(self, e, fn, reads=(), writes=(), sig=None):
        reads = [x.b if isinstance(x, Tile) else x for x in reads]
        writes = [x.b if isinstance(x, Tile) else x for x in writes]
        self._deps(e, reads, writes)
        if e == "pe":
            px = _PEProxy(self.engs[e])
            inst = fn(px)
            if sig is None:
                sig = px.stop
        else:
            inst = fn(self.engs[e])
            if sig is None:
                sig = True
        self.n_inst[e] += 1
        c = self.cnt[e]
        epoch, idx = divmod(c, self.EPOCH)
        stamp = ((e, epoch), idx + 1)
        self._record(stamp, reads, writes)
        if sig:
            inst.then_inc(self._eng_sem(e, epoch), 1)
            self.cnt[e] = c + 1
        return inst

    def dma(self, q, out, in_, reads=(), writes=(), **kw):
        reads = [x.b if isinstance(x, Tile) else x for x in reads]
        writes = [x.b if isinstance(x, Tile) else x for x in writes]
        pool = self.dma_pool.setdefault(q, [])
        i = self.dma_idx[q]
        slot = i % self.NPOOL
        if slot >= len(pool):
            pool.append([self.es.enter_context(self.nc.semaphore("d_%s_%d" % (q, slot))), 0])
        sem, uses = pool[slot]
        if uses > 0:
            self._wait(q, (("dma", q, slot), 16 * uses))
        self._deps(q, reads, writes)
        inst = self.engs[q].dma_start(out=out, in_=in_, **kw)
        inst.then_inc(sem, 16)
        pool[slot][1] = uses + 1
        self.dma_idx[q] = i + 1
        self.n_inst[q] += 1
        self._record((("dma", q, slot), 16 * (uses + 1)), reads, writes)
        return inst

    def idma(self, out, out_off, in_, in_off, reads=(), writes=()):
        q = "pool"
        reads = [x.b if isinstance(x, Tile) else x for x in reads]
        writes = [x.b if isinstance(x, Tile) else x for x in writes]
        pool = self.dma_pool.setdefault(q, [])
        i = self.dma_idx[q]
        slot = i % self.NPOOL
        if slot >= len(pool):
            pool.append([self.es.enter_context(self.nc.semaphore("d_%s_%d" % (q, slot))), 0])
        sem, uses = pool[slot]
        if uses > 0:
            self._wait(q, (("dma", q, slot), 16 * uses))
        self._deps(q, reads, writes)
        inst = self.engs[q].indirect_dma_start(out=out, out_offset=out_off, in_=in_, in_offset=in_off)
        inst.then_inc(sem, 16)
        pool[slot][1] = uses + 1
        self.dma_idx[q] = i + 1
        self.n_inst[q] += 1
        self._record((("dma", q, slot), 16 * (uses + 1)), reads, writes)
        return inst

    def barrier(self, queues=("sp", "act")):
        stamps = []
        for e in ("pe", "act", "dve", "pool"):
            c = self.cnt[e]
            if c > 0:
                epoch, idx = divmod(c - 1, self.EPOCH)
                stamps.append(((e, epoch), idx + 1))
        for q in queues:
            for slot, (sem, uses) in enumerate(self.dma_pool.get(q, [])):
                if uses > 0:
                    stamps.append((("dma", q, slot), 16 * uses))
        for e in ("pe", "act", "dve", "pool", "sp"):
            for st in stamps:
                if st[0][0] == e:
                    continue
                self._wait(e, st)

    def finish(self, bufs, e="sp"):
        for b in bufs:
            b = b.b if isinstance(b, Tile) else b
            if b.w is not None:
                self._wait(e, b.w)


class K:
    def __init__(self, T, layers=(0, 1), debug=False):
        self.T = T
        self.layers = layers
        self.debug = debug
        self.nc = bass.Bass("TRN2", target_bir_lowering=False)
        self.inp = {}
        self.dbg_out = []

    def din(self, name, shape):
        t = self.nc.dram_tensor(name, list(shape), F32, kind="ExternalInput").ap()
        self.inp[name] = t
        return t

    def dscr(self, name, shape, dt, dbg=False):
        kind = "ExternalOutput" if (dbg and self.debug) else "Internal"
        t = self.nc.dram_tensor(name, list(shape), dt, kind=kind).ap()
        if dbg and self.debug:
            self.dbg_out.append(name)
        return t

    def _uniq(self, name):
        self._nid = getattr(self, "_nid", 0) + 1
        return "%s_%d" % (name, self._nid)

    def sb(self, es, name, shape, dt):
        name = self._uniq(name)
        return Tile(es.enter_context(self.nc.sbuf_tensor(name, list(shape), dt)), name)

    def ps(self, es, name, shape, dt):
        name = self._uniq(name)
        full = es.enter_context(self.nc.psum_tensor(name, [128, 512], F32))
        ap = full[:]
        n = int(np.prod(shape[1:]))
        if dt == BF16:
            ap = ap.bitcast(BF16)
            assert n <= 1024
        else:
            assert n <= 512
        ap = ap[0:shape[0], 0:n]
        if len(shape) == 3:
            ap = ap.rearrange("p (a b) -> p a b", b=shape[2])
        return Tile(ap, name)

    def scope(self):
        k = self

        class _Scope(ExitStack):
            def __exit__(self, *a):
                if a[0] is None:
                    k.S.barrier()
                return super().__exit__(*a)
        return _Scope()

    def convert(self, src, name, rows, cols, rb=128):
        dst = self.nc.dram_tensor(name, [rows, cols], BF16, kind="Internal").ap()
        bufs = []
        for r0 in range(0, rows, rb):
            b = Buf("%s_%d" % (name, r0))
            self.S.dma("pool", dst[r0:r0 + rb, :], src[r0:r0 + rb, :], writes=[b])
            bufs.append(b)
        return dst, bufs

    def rstd_cols(self, ss, n, scale):
        S = self.S
        S.op("act", lambda e: e.activation(out=ss[:, 0:n], in_=ss[:, 0:n], func=AF.Ln, scale=scale, bias=self.epsc[:, 0:1]),
             reads=[ss, self.epsc], writes=[ss])
        S.op("act", lambda e: e.activation(out=ss[:, 0:n], in_=ss[:, 0:n], func=AF.Exp, scale=-0.5), reads=[ss], writes=[ss])

    def norm_mod_T(self, xt, gmod, shift, uT_dst, col0, ss, tmp, u, pT, junk):
        S = self.S
        self._nm = getattr(self, "_nm", 0) + 1
        pick = lambda b: b[self._nm % len(b)] if isinstance(b, list) else b
        ss, tmp, u, pT, junk = pick(ss), pick(tmp), pick(u), pick(pT), pick(junk)
        S.op("act", lambda e: e.activation(out=junk[:], in_=xt[:], func=AF.Square, accum_out=ss[:, 0:1]),
             reads=[xt], writes=[junk, ss])
        self.rstd_cols(ss, 1, 1.0 / D)
        S.op("dve", lambda e: e.scalar_tensor_tensor(out=tmp[:], in0=xt[:], scalar=ss[:, 0:1], in1=gmod[:], op0=ALU.mult, op1=ALU.mult),
             reads=[xt, ss, gmod], writes=[tmp])
        S.op("dve", lambda e: e.tensor_tensor(out=u[:], in0=tmp[:], in1=shift[:], op=ALU.add), reads=[tmp, shift], writes=[u])
        for c in range(8):
            S.op("pe", lambda e: e.transpose(out=pT[:, c, :], in_=u[:, c * 128:(c + 1) * 128], identity=self.identb[:]),
                 reads=[u, self.identb], writes=[pT], sig=(c == 7))
        S.op("act", lambda e: e.copy(out=uT_dst[:, :, col0:col0 + 128], in_=pT[:]), reads=[pT], writes=[uT_dst])
        return u

    def post_norm_residual(self, y, xt, ggt, ss, junk, xo):
        S = self.S
        S.op("act", lambda e: e.activation(out=junk[:], in_=y[:], func=AF.Square, accum_out=ss[:, 0:1]), reads=[y], writes=[junk, ss])
        self.rstd_cols(ss, 1, 1.0 / D)
        S.op("dve", lambda e: e.scalar_tensor_tensor(out=y[:], in0=y[:], scalar=ss[:, 0:1], in1=ggt[:], op0=ALU.mult, op1=ALU.mult),
             reads=[y, ss, ggt], writes=[y])
        S.op("dve", lambda e: e.tensor_tensor(out=xo[:], in0=y[:], in1=xt[:], op=ALU.add), reads=[y, xt], writes=[xo])

    def adaln(self, es, cvec, mod_w, mod_b, gains, want):
        S, nc = self.S, self.nc
        self._adn = getattr(self, "_adn", 0) + 1
        sfx = "_%d" % self._adn
        out = {}
        for (name, idx, kind, gain) in want:
            out[name] = self.sb(es, "bc_" + name + sfx, [128, D], F32)
        with self.scope() as les:
            cT = self.sb(les, "ad_cT" + sfx, [128, 8], F32)
            sc = self.sb(les, "ad_sc" + sfx, [128, 8], F32)
            row = self.sb(les, "ad_row" + sfx, [1, 6 * D], F32)
            brow = self.sb(les, "ad_brow" + sfx, [1, 6 * D], F32)
            wbuf = [self.sb(les, "ad_w%d" % i + sfx, [128, 8, 512], F32) for i in range(2)]
            gt = self.sb(les, "ad_g" + sfx, [128, D], F32)
            pr = self.ps(les, "ad_pr" + sfx, [1, 512], F32)
            pb = self.ps(les, "ad_pb" + sfx, [128, 512], F32)
            S.dma("sp", cT[:], cvec.rearrange("(c p) -> p c", p=128), writes=[cT], allow_slow_non_contiguous=True)
            S.dma("sp", brow[:], mod_b.unsqueeze(0), writes=[brow])
            S.op("act", lambda e: e.activation(out=sc[:], in_=cT[:], func=AF.Silu), reads=[cT], writes=[sc])
            need = sorted(set(i for (_, i, _, _) in want))
            for gi in range(12):
                if gi // 2 not in need:
                    continue
                wb = wbuf[gi % 2]
                S.dma("sp", wb[:], mod_w.rearrange("(c p) n -> p c n", p=128)[:, :, gi * 512:(gi + 1) * 512], writes=[wb])
                for c in range(8):
                    S.op("pe", lambda e: e.matmul(pr[:], lhsT=sc[:, c:c + 1], rhs=wb[:, c, :], start=(c == 0), stop=(c == 7)),
                         reads=[sc, wb], writes=[pr])
                S.op("dve", lambda e: e.tensor_tensor(out=row[:, gi * 512:(gi + 1) * 512], in0=pr[:], in1=brow[:, gi * 512:(gi + 1) * 512], op=ALU.add),
                     reads=[pr, brow], writes=[row])
            for (name, idx, kind, gain) in want:
                dst = out[name]
                if gain is not None:
                    S.dma("sp", gt[:], gain.partition_broadcast(128), writes=[gt])
                for h in range(2):
                    S.op("pe", lambda e: e.matmul(pb[:], lhsT=self.ones1[:], rhs=row[:, idx * D + h * 512: idx * D + (h + 1) * 512], start=True, stop=True),
                         reads=[self.ones1, row], writes=[pb])
                    sl = slice(h * 512, (h + 1) * 512)
                    if kind == "gmod":
                        S.op("dve", lambda e: e.scalar_tensor_tensor(out=dst[:, sl], in0=pb[:], scalar=1.0, in1=gt[:, sl], op0=ALU.add, op1=ALU.mult),
                             reads=[pb, gt], writes=[dst])
                    elif kind == "shift":
                        S.op("dve", lambda e: e.tensor_copy(out=dst[:, sl], in_=pb[:]), reads=[pb], writes=[dst])
                    else:
                        S.op("dve", lambda e: e.tensor_tensor(out=dst[:, sl], in0=pb[:], in1=gt[:, sl], op=ALU.mult), reads=[pb, gt], writes=[dst])
        return out

    def ffn_phase(self, pfx, xin, xin_bufs, xout, xout_bufs, bc, w1, w1b, w3, w3b, w2, w2b, nexp, router=None):
        S, nc, T = self.S, self.nc, self.T
        TS = min(2048, T)
        NT = TS // 128
        FG = 4
        grp = [(g0, min(FG, NCH_FF - g0)) for g0 in range(0, NCH_FF, FG)]
        with self.scope() as es:
            xt = [self.sb(es, pfx + "xt%d" % i, [128, D], F32) for i in range(2)]
            tmp = self.sb(es, pfx + "tmp", [128, D], F32)
            junk = self.sb(es, pfx + "junk", [128, D], BF16)
            u = [self.sb(es, pfx + "u%d" % i, [128, D], BF16) for i in range(2)]
            ss = self.sb(es, pfx + "ss", [128, 4], F32)
            ssn = [self.sb(es, pfx + "ssn%d" % i, [128, 4], F32) for i in range(2)]
            uT = self.sb(es, pfx + "uT", [128, 8, TS], BF16)
            yacc = self.sb(es, pfx + "yacc", [128, NT, D], F32)
            w1g = [self.sb(es, pfx + "w1g%d" % i, [128, 8, FG * 128], BF16) for i in range(2)]
            w3g = [self.sb(es, pfx + "w3g%d" % i, [128, 8, FG * 128], BF16) for i in range(2)]
            w2g = [self.sb(es, pfx + "w2g%d" % i, [128, FG, D], BF16) for i in range(2)]
            hT = [self.sb(es, pfx + "hT%d" % i, [128, FG, 512], BF16) for i in range(2)]
            sl_ = [self.sb(es, pfx + "sl%d" % i, [128, 512], F32) for i in range(2)]
            xos = [self.sb(es, pfx + "xo%d" % i, [128, D], F32) for i in range(2)]
            pT = self.ps(es, pfx + "pT", [128, 8, 128], BF16)
            p1 = [self.ps(es, pfx + "p1_%d" % i, [128, 512], F32) for i in range(2)]
            p3 = [self.ps(es, pfx + "p3_%d" % i, [128, 512], F32) for i in range(2)]
            py = [self.ps(es, pfx + "py_%d" % i, [128, 512], F32) for i in range(2)]
            if router is not None:
                rw = self.sb(es, pfx + "rw", [128, 8, NEXP], BF16)
                S.dma("pool", rw[:], router.rearrange("(c p) n -> p c n", p=128), writes=[rw])
                gates = self.sb(es, pfx + "gates", [128, NT, NEXP], F32)
                lg = self.sb(es, pfx + "lg", [128, NEXP], F32)
                l2 = self.sb(es, pfx + "l2", [128, NEXP], F32)
                mk = self.sb(es, pfx + "mk", [128, NEXP], F32)
                m1 = self.sb(es, pfx + "m1", [128, 4], F32)
                pl = self.ps(es, pfx + "pl", [128, NEXP], F32)
            gi = 0
            cnt_s1 = 0
            cnt_y = 0
            kh = 0
            for st in range(T // TS):
                for tt in range(NT):
                    tile_i = st * NT + tt
                    x_ = xt[tt % 2]
                    S.dma("sp", x_[:], xin[tile_i * 128:(tile_i + 1) * 128, :], reads=[xin_bufs[tile_i]], writes=[x_])
                    self.norm_mod_T(x_, bc["gmod_f"], bc["shift_f"], uT, tt * 128, ssn, tmp, u, pT, junk)
                    if router is not None:
                        for c in range(8):
                            S.op("pe", lambda e: e.matmul(pl[:], lhsT=uT[:, c, tt * 128:(tt + 1) * 128], rhs=rw[:, c, :], start=(c == 0), stop=(c == 7)),
                                 reads=[uT, rw], writes=[pl])
                        S.op("dve", lambda e: e.tensor_copy(out=lg[:], in_=pl[:]), reads=[pl], writes=[lg])
                        S.op("dve", lambda e: e.reduce_max(out=m1[:, 0:1], in_=lg[:], axis=AX.X), reads=[lg], writes=[m1])
                        S.op("dve", lambda e: e.tensor_scalar(out=mk[:], in0=lg[:], scalar1=m1[:, 0:1], scalar2=-1e30, op0=ALU.is_ge, op1=ALU.mult),
                             reads=[lg, m1], writes=[mk])
                        S.op("dve", lambda e: e.tensor_tensor(out=l2[:], in0=lg[:], in1=mk[:], op=ALU.add), reads=[lg, mk], writes=[l2])
                        S.op("dve", lambda e: e.reduce_max(out=m1[:, 1:2], in_=l2[:], axis=AX.X), reads=[l2], writes=[m1])
                        S.op("dve", lambda e: e.tensor_scalar(out=mk[:], in0=lg[:], scalar1=m1[:, 1:2], scalar2=None, op0=ALU.is_ge),
                             reads=[lg, m1], writes=[mk])
                        S.op("dve", lambda e: e.tensor_scalar(out=m1[:, 2:3], in0=m1[:, 0:1], scalar1=-1.0, scalar2=None, op0=ALU.mult),
                             reads=[m1], writes=[m1])
                        S.op("act", lambda e: e.activation(out=l2[:], in_=lg[:], func=AF.Exp, bias=m1[:, 2:3]), reads=[lg, m1], writes=[l2])
                        S.op("dve", lambda e: e.tensor_tensor(out=l2[:], in0=l2[:], in1=mk[:], op=ALU.mult), reads=[l2, mk], writes=[l2])
                        S.op("dve", lambda e: e.reduce_sum(out=m1[:, 3:4], in_=l2[:], axis=AX.X), reads=[l2], writes=[m1])
                        S.op("dve", lambda e: e.reciprocal(out=m1[:, 3:4], in_=m1[:, 3:4]), reads=[m1], writes=[m1])
                        S.op("dve", lambda e: e.tensor_scalar(out=gates[:, tt, :], in0=l2[:], scalar1=m1[:, 3:4], scalar2=None, op0=ALU.mult),
                             reads=[l2, m1], writes=[gates])
                for ex in range(nexp):
                    for g, (g0, nv) in enumerate(grp):
                        f0 = g0 * 128
                        wa, wb_, wc = w1g[gi % 2], w3g[gi % 2], w2g[gi % 2]
                        gi += 1
                        rb = [w1b[(ex * D) // 128 + c] for c in range(8)]
                        S.dma("sp", wa[:, :, 0:nv * 128], w1[ex * D:(ex + 1) * D, f0:f0 + nv * 128].rearrange("(c p) f -> p c f", p=128), reads=rb, writes=[wa])
                        rb = [w3b[(ex * D) // 128 + c] for c in range(8)]
                        S.dma("sp", wb_[:, :, 0:nv * 128], w3[ex * D:(ex + 1) * D, f0:f0 + nv * 128].rearrange("(c p) f -> p c f", p=128), reads=rb, writes=[wb_])
                        r0 = ex * DFF + f0
                        rb = [w2b[(r0 // 128) + c] for c in range(nv)]
                        S.dma("sp", wc[:, 0:nv, :], w2[r0:r0 + nv * 128, :].rearrange("(c p) d -> p c d", p=128), reads=rb, writes=[wc])
                        for sub in range(TS // 512):
                            h_ = hT[kh % 2]
                            kh += 1
                            for j in range(nv):
                                a1, a3, s_ = p1[cnt_s1 % 2], p3[cnt_s1 % 2], sl_[cnt_s1 % 2]
                                cnt_s1 += 1
                                for c in range(8):
                                    S.op("pe", lambda e: e.matmul(a1[:], lhsT=wa[:, c, j * 128:(j + 1) * 128], rhs=uT[:, c, sub * 512:(sub + 1) * 512],
                                                                  start=(c == 0), stop=(c == 7)), reads=[wa, uT], writes=[a1])
                                for c in range(8):
                                    S.op("pe", lambda e: e.matmul(a3[:], lhsT=wb_[:, c, j * 128:(j + 1) * 128], rhs=uT[:, c, sub * 512:(sub + 1) * 512],
                                                                  start=(c == 0), stop=(c == 7)), reads=[wb_, uT], writes=[a3])
                                S.op("act", lambda e: e.activation(out=s_[:], in_=a1[:], func=AF.Silu), reads=[a1], writes=[s_])
                                S.op("dve", lambda e: e.tensor_tensor(out=h_[:, j, :], in0=s_[:], in1=a3[:], op=ALU.mult), reads=[s_, a3], writes=[h_])
                            for t4 in range(4):
                                tt = sub * 4 + t4
                                for dg in range(2):
                                    y_ = py[cnt_y % 2]
                                    cnt_y += 1
                                    for j in range(nv):
                                        S.op("pe", lambda e: e.matmul(y_[:], lhsT=h_[:, j, t4 * 128:(t4 + 1) * 128], rhs=wc[:, j, dg * 512:(dg + 1) * 512],
                                                                      start=(j == 0), stop=(j == nv - 1)), reads=[h_, wc], writes=[y_])
                                    ya = yacc[:, tt, dg * 512:(dg + 1) * 512]
                                    first = (ex == 0 and g == 0)
                                    if router is None:
                                        if first:
                                            S.op("dve", lambda e: e.tensor_copy(out=ya, in_=y_[:]), reads=[y_], writes=[yacc])
                                        else:
                                            S.op("dve", lambda e: e.tensor_tensor(out=ya, in0=y_[:], in1=ya, op=ALU.add), reads=[y_, yacc], writes=[yacc])
                                    else:
                                        gs = gates[:, tt, ex:ex + 1]
                                        if first:
                                            S.op("dve", lambda e: e.tensor_scalar(out=ya, in0=y_[:], scalar1=gs, scalar2=None, op0=ALU.mult),
                                                 reads=[y_, gates], writes=[yacc])
                                        else:
                                            S.op("dve", lambda e: e.scalar_tensor_tensor(out=ya, in0=y_[:], scalar=gs, in1=ya, op0=ALU.mult, op1=ALU.add),
                                                 reads=[y_, gates, yacc], writes=[yacc])
                if self.debug and st == 0:
                    dg_ = self.dscr(pfx + "dbg_yacc", [128, NT, D], F32, dbg=True)
                    S.dma("sp", dg_[:, :, :], yacc[:], reads=[yacc], writes=[Buf("dbgy")])
                    du_ = self.dscr(pfx + "dbg_uT", [128, 8, TS], BF16, dbg=True)
                    S.dma("sp", du_[:, :, :], uT[:], reads=[uT], writes=[Buf("dbgu")])
                    if router is not None:
                        dgt_ = self.dscr(pfx + "dbg_gates", [128, NT, NEXP], F32, dbg=True)
                        S.dma("sp", dgt_[:, :, :], gates[:], reads=[gates], writes=[Buf("dbgg")])
                for tt in range(NT):
                    tile_i = st * NT + tt
                    x_ = xt[tt % 2]
                    xo = xos[tt % 2]
                    ss = ssn[tt % 2]
                    S.dma("sp", x_[:], xin[tile_i * 128:(tile_i + 1) * 128, :], reads=[xin_bufs[tile_i]], writes=[x_])
                    S.op("act", lambda e: e.activation(out=junk[:], in_=yacc[:, tt, :], func=AF.Square, accum_out=ss[:, 0:1]),
                         reads=[yacc], writes=[junk, ss])
                    self.rstd_cols(ss, 1, 1.0 / D)
                    S.op("dve", lambda e: e.scalar_tensor_tensor(out=tmp[:], in0=yacc[:, tt, :], scalar=ss[:, 0:1], in1=bc["ggt_f"][:], op0=ALU.mult, op1=ALU.mult),
                         reads=[yacc, ss, bc["ggt_f"]], writes=[tmp])
                    S.op("dve", lambda e: e.tensor_tensor(out=xo[:], in0=tmp[:], in1=x_[:], op=ALU.add), reads=[tmp, x_], writes=[xo])
                    S.dma("sp", xout[tile_i * 128:(tile_i + 1) * 128, :], xo[:], reads=[xo], writes=[xout_bufs[tile_i]])


    def moe_phase(self, xin, xin_bufs, xout, xout_bufs, bc, W):
        S, nc, T = self.S, self.nc, self.T
        I = self.inp
        I32 = mybir.dt.int32
        NTL = T // 128
        NSB = (2 * T) // 512 + NEXP
        G, FW, NJ = 6, 512, 4
        nval = [4, 4, 4, 4, 4, 2]
        NE = NEXP * NTL
        assert NE <= 512
        U2 = self.dscr("m_U2", [T, D], BF16)
        XS = self.dscr("m_XS", [NSB * 512, D], BF16)
        YS = self.dscr("m_YS", [NSB * 512, D], F32)
        u2_b = [Buf("u2_%d" % i) for i in range(NTL)]
        xs_b = [Buf("xs_%d" % i) for i in range(2 * NTL)]
        ys_b = [Buf("ys_%d" % i) for i in range(NSB)]
        with self.scope() as es:
            gatesE = self.sb(es, "m_gE", [128, NEXP, NTL], F32)
            sl_i = self.sb(es, "m_sli", [128, 2, NTL], I32)
            g12 = self.sb(es, "m_g12", [128, 2, NTL], F32)
            idxw = self.sb(es, "m_idxw", [128, NSB, G], I32)
            with self.scope() as p1:
                rw = self.sb(p1, "m_rw", [128, 8, NEXP], BF16)
                S.dma("pool", rw[:], I["l1_router_w"].rearrange("(c p) n -> p c n", p=128), writes=[rw])
                xt = [self.sb(p1, "m_xt%d" % i, [128, D], F32) for i in range(2)]
                tmp = [self.sb(p1, "m_tmp%d" % i, [128, D], F32) for i in range(2)]
                junk = self.sb(p1, "m_junk", [128, D], BF16)
                u = [self.sb(p1, "m_u%d" % i, [128, D], BF16) for i in range(2)]
                ss = [self.sb(p1, "m_ss%d" % i, [128, 4], F32) for i in range(2)]
                uT = [self.sb(p1, "m_uT%d" % i, [128, 8, 128], BF16) for i in range(2)]
                lg = self.sb(p1, "m_lg", [128, NEXP], F32)
                l2 = self.sb(p1, "m_l2", [128, NEXP], F32)
                mk = self.sb(p1, "m_mk", [128, NEXP], F32)
                m1 = self.sb(p1, "m_m1", [128, 4], F32)
                pT = [self.ps(p1, "m_pT%d" % i, [128, 8, 128], BF16) for i in range(2)]
                pl = self.ps(p1, "m_pl", [128, NEXP], F32)
                def front(tt):
                    x_, uT_ = xt[tt % 2], uT[tt % 2]
                    S.dma("sp", x_[:], xin[tt * 128:(tt + 1) * 128, :], reads=[xin_bufs[tt]], writes=[x_])
                    u_ = self.norm_mod_T(x_, bc["gmod_f"], bc["shift_f"], uT_, 0, ss, tmp, u, pT, junk)
                    S.dma("sp", U2[tt * 128:(tt + 1) * 128, :], u_[:], reads=[u_], writes=[u2_b[tt]])

                front(0)
                for tt in range(NTL):
                    uT_ = uT[tt % 2]
                    if tt + 1 < NTL:
                        front(tt + 1)
                    for c in range(8):
                        S.op("pe", lambda e: e.matmul(pl[:], lhsT=uT_[:, c, :], rhs=rw[:, c, :], start=(c == 0), stop=(c == 7)), reads=[uT_, rw], writes=[pl])
                    S.op("dve", lambda e: e.tensor_copy(out=lg[:], in_=pl[:]), reads=[pl], writes=[lg])
                    S.op("dve", lambda e: e.reduce_max(out=m1[:, 0:1], in_=lg[:], axis=AX.X), reads=[lg], writes=[m1])
                    S.op("dve", lambda e: e.tensor_scalar(out=mk[:], in0=lg[:], scalar1=m1[:, 0:1], scalar2=-1e30, op0=ALU.is_ge, op1=ALU.mult), reads=[lg, m1], writes=[mk])
                    S.op("dve", lambda e: e.tensor_tensor(out=l2[:], in0=lg[:], in1=mk[:], op=ALU.add), reads=[lg, mk], writes=[l2])
                    S.op("dve", lambda e: e.reduce_max(out=m1[:, 1:2], in_=l2[:], axis=AX.X), reads=[l2], writes=[m1])
                    S.op("dve", lambda e: e.tensor_scalar(out=mk[:], in0=lg[:], scalar1=m1[:, 1:2], scalar2=None, op0=ALU.is_ge), reads=[lg, m1], writes=[mk])
                    S.op("dve", lambda e: e.tensor_scalar(out=m1[:, 2:3], in0=m1[:, 0:1], scalar1=-1.0, scalar2=None, op0=ALU.mult), reads=[m1], writes=[m1])
                    S.op("act", lambda e: e.activation(out=l2[:], in_=lg[:], func=AF.Exp, bias=m1[:, 2:3]), reads=[lg, m1], writes=[l2])
                    S.op("dve", lambda e: e.tensor_tensor(out=l2[:], in0=l2[:], in1=mk[:], op=ALU.mult), reads=[l2, mk], writes=[l2])
                    S.op("dve", lambda e: e.reduce_sum(out=m1[:, 3:4], in_=l2[:], axis=AX.X), reads=[l2], writes=[m1])
                    S.op("dve", lambda e: e.reciprocal(out=m1[:, 3:4], in_=m1[:, 3:4]), reads=[m1], writes=[m1])
                    S.op("dve", lambda e: e.tensor_scalar(out=gatesE[:, :, tt], in0=l2[:], scalar1=m1[:, 3:4], scalar2=None, op0=ALU.mult), reads=[l2, m1], writes=[gatesE])
            with self.scope() as p2:
                f3 = lambda n: self.sb(p2, n, [128, NEXP, NTL], F32)
                sel, Wt, Ct, Ic, S3, cs, mm, pr = f3("m_sel"), f3("m_Wt"), f3("m_Ct"), f3("m_Ic"), f3("m_S3"), f3("m_cs"), f3("m_mm"), f3("m_pr")
                selb = self.sb(p2, "m_selb", [128, NE], BF16)
                tri = self.sb(p2, "m_tri", [128, 128], BF16)
                onek = self.sb(p2, "m_onek", [128, 128], BF16)
                one1 = self.sb(p2, "m_one1", [128, 1], F32)
                cnt = self.sb(p2, "m_cnt", [128, NEXP], F32)
                pad = self.sb(p2, "m_pad", [128, NEXP], F32)
                pend = self.sb(p2, "m_pend", [128, NEXP], F32)
                pst = self.sb(p2, "m_pst", [128, NEXP], F32)
                sbpos = self.sb(p2, "m_sbpos", [128, NSB], F32)
                cmp_ = self.sb(p2, "m_cmp", [128, NSB, NEXP], F32)
                esb = self.sb(p2, "m_esb", [128, NSB], F32)
                wbase = self.sb(p2, "m_wbase", [128, G], F32)
                idxf = self.sb(p2, "m_idxf", [128, NSB, G], F32)
                slf = self.sb(p2, "m_slf", [128, 2, NTL], F32)
                pw = self.ps(p2, "m_pw", [128, NE], F32)
                pc = self.ps(p2, "m_pc", [128, NE], F32)
                fl = lambda t: t[:].rearrange("p e n -> p (e n)")
                S.dma("pool", tri[:], I["k_tri"][:, :], writes=[tri])
                S.dma("sp", sbpos[:], I["k_sbpos"][0:NSB].partition_broadcast(128), writes=[sbpos])
                S.dma("sp", wbase[:], I["k_wbase"][:, :], writes=[wbase])
                S.op("dve", lambda e: e.memset(onek[:], 1.0), writes=[onek])
                S.op("dve", lambda e: e.memset(one1[:], 1.0), writes=[one1])
                S.op("dve", lambda e: e.tensor_scalar(out=fl(sel), in0=fl(gatesE), scalar1=0.0, scalar2=None, op0=ALU.is_gt), reads=[gatesE], writes=[sel])
                S.op("dve", lambda e: e.tensor_copy(out=selb[:], in_=fl(sel)), reads=[sel], writes=[selb])
                S.op("pe", lambda e: e.matmul(pw[:], lhsT=tri[:], rhs=selb[:], start=True, stop=True), reads=[tri, selb], writes=[pw])
                S.op("pe", lambda e: e.matmul(pc[:], lhsT=onek[:], rhs=selb[:], start=True, stop=True), reads=[onek, selb], writes=[pc])
                S.op("act", lambda e: e.copy(out=fl(Wt), in_=pw[:]), reads=[pw], writes=[Wt])
                S.op("act", lambda e: e.copy(out=fl(Ct), in_=pc[:]), reads=[pc], writes=[Ct])
                for ex in range(NEXP):
                    S.op("dve", lambda e: e.tensor_tensor_scan(out=Ic[:, ex, :], data0=one1[:, 0:1].to_broadcast([128, NTL]), data1=Ct[:, ex, :], initial=0.0, op0=ALU.mult, op1=ALU.add),
                         reads=[one1, Ct], writes=[Ic])
                S.op("dve", lambda e: e.tensor_copy(out=cnt[:], in_=Ic[:, :, NTL - 1]), reads=[Ic], writes=[cnt])
                S.op("dve", lambda e: e.tensor_scalar(out=pad[:], in0=cnt[:], scalar1=1.0 / 512, scalar2=511.0 / 512 - 0.5 + 1.0 / 1024, op0=ALU.mult, op1=ALU.add), reads=[cnt], writes=[pad])
                S.op("dve", lambda e: e.tensor_scalar(out=pad[:], in0=pad[:], scalar1=8388608.0, scalar2=None, op0=ALU.add), reads=[pad], writes=[pad])
                S.op("dve", lambda e: e.tensor_scalar(out=pad[:], in0=pad[:], scalar1=-8388608.0, scalar2=512.0, op0=ALU.add, op1=ALU.mult), reads=[pad], writes=[pad])
                S.op("dve", lambda e: e.tensor_tensor_scan(out=pend[:], data0=one1[:, 0:1].to_broadcast([128, NEXP]), data1=pad[:], initial=0.0, op0=ALU.mult, op1=ALU.add),
                     reads=[one1, pad], writes=[pend])
                S.op("dve", lambda e: e.tensor_tensor(out=pst[:], in0=pend[:], in1=pad[:], op=ALU.subtract), reads=[pend, pad], writes=[pst])
                S.op("dve", lambda e: e.tensor_tensor(out=fl(S3), in0=fl(Ic), in1=fl(Ct), op=ALU.subtract), reads=[Ic, Ct], writes=[S3])
                S.op("dve", lambda e: e.tensor_tensor(out=fl(S3), in0=fl(S3), in1=fl(Wt), op=ALU.add), reads=[S3, Wt], writes=[S3])
                S.op("dve", lambda e: e.tensor_tensor(out=S3[:], in0=S3[:], in1=pst[:].unsqueeze(2).to_broadcast([128, NEXP, NTL]), op=ALU.add), reads=[S3, pst], writes=[S3])
                S.op("dve", lambda e: e.tensor_copy(out=cs[:, 0, :], in_=sel[:, 0, :]), reads=[sel], writes=[cs])
                for ex in range(1, NEXP):
                    S.op("dve", lambda e: e.tensor_tensor(out=cs[:, ex, :], in0=cs[:, ex - 1, :], in1=sel[:, ex, :], op=ALU.add), reads=[cs, sel], writes=[cs])
                for k in range(2):
                    S.op("dve", lambda e: e.tensor_scalar(out=fl(mm), in0=fl(cs), scalar1=float(k + 1), scalar2=None, op0=ALU.is_equal), reads=[cs], writes=[mm])
                    S.op("dve", lambda e: e.tensor_tensor(out=fl(mm), in0=fl(mm), in1=fl(sel), op=ALU.mult), reads=[mm, sel], writes=[mm])
                    S.op("dve", lambda e: e.tensor_tensor(out=fl(pr), in0=fl(mm), in1=fl(S3), op=ALU.mult), reads=[mm, S3], writes=[pr])
                    S.op("dve", lambda e: e.tensor_reduce(out=slf[:, k, :], in_=pr[:].rearrange("p e n -> p n e"), axis=AX.X, op=ALU.add), reads=[pr], writes=[slf])
                    S.op("dve", lambda e: e.tensor_tensor(out=fl(pr), in0=fl(mm), in1=fl(gatesE), op=ALU.mult), reads=[mm, gatesE], writes=[pr])
                    S.op("dve", lambda e: e.tensor_reduce(out=g12[:, k, :], in_=pr[:].rearrange("p e n -> p n e"), axis=AX.X, op=ALU.add), reads=[pr], writes=[g12])
                S.op("dve", lambda e: e.tensor_copy(out=sl_i[:], in_=slf[:]), reads=[slf], writes=[sl_i])
                S.op("dve", lambda e: e.tensor_tensor(out=cmp_[:], in0=pend[:].unsqueeze(1).to_broadcast([128, NSB, NEXP]), in1=sbpos[:].unsqueeze(2).to_broadcast([128, NSB, NEXP]), op=ALU.is_le),
                     reads=[pend, sbpos], writes=[cmp_])
                S.op("dve", lambda e: e.tensor_reduce(out=esb[:], in_=cmp_[:], axis=AX.X, op=ALU.add), reads=[cmp_], writes=[esb])
                S.op("dve", lambda e: e.tensor_scalar(out=esb[:], in0=esb[:], scalar1=float(NEXP - 1), scalar2=float(G * 128), op0=ALU.min, op1=ALU.mult), reads=[esb], writes=[esb])
                S.op("dve", lambda e: e.tensor_tensor(out=idxf[:], in0=esb[:].unsqueeze(2).to_broadcast([128, NSB, G]), in1=wbase[:].unsqueeze(1).to_broadcast([128, NSB, G]), op=ALU.add),
                     reads=[esb, wbase], writes=[idxf])
                S.op("dve", lambda e: e.tensor_copy(out=idxw[:], in_=idxf[:]), reads=[idxf], writes=[idxw])
            with self.scope() as p3:
                ut = [self.sb(p3, "m_ut%d" % i, [128, D], BF16) for i in range(3)]
                for tt in range(NTL):
                    u_ = ut[tt % 3]
                    S.dma("sp", u_[:], U2[tt * 128:(tt + 1) * 128, :], reads=[u2_b[tt]], writes=[u_])
                    for k in range(2):
                        S.idma(XS[:, :], bass.IndirectOffsetOnAxis(ap=sl_i[:, k, tt:tt + 1], axis=0), u_[:], None, reads=[u_, sl_i], writes=[xs_b[tt * 2 + k]])
            with self.scope() as p4:
                xs = [self.sb(p4, "m_xs%d" % i, [128, 4, D], BF16) for i in range(2)]
                uT = [self.sb(p4, "m_uT4_%d" % i, [128, 8, 512], BF16) for i in range(2)]
                w1g = [self.sb(p4, "m_w1g%d" % i, [128, 8, FW], BF16) for i in range(2)]
                w3g = [self.sb(p4, "m_w3g%d" % i, [128, 8, FW], BF16) for i in range(2)]
                w2g = [self.sb(p4, "m_w2g%d" % i, [128, NJ, D], BF16) for i in range(2)]
                hT = [self.sb(p4, "m_hT%d" % i, [128, NJ, 512], BF16) for i in range(2)]
                sl_ = [self.sb(p4, "m_sl%d" % i, [128, 512], F32) for i in range(2)]
                yacc = [self.sb(p4, "m_yacc%d" % i, [128, 4, D], F32) for i in range(2)]
                pTs = [self.ps(p4, "m_pT4_%d" % i, [128, 8, 128], BF16) for i in range(2)]
                pp1 = [self.ps(p4, "m_p1_%d" % i, [128, 512], F32) for i in range(2)]
                pp3 = [self.ps(p4, "m_p3_%d" % i, [128, 512], F32) for i in range(2)]
                py = [self.ps(p4, "m_py_%d" % i, [128, 512], F32) for i in range(2)]
                gi = 0
                k1 = 0
                ky = 0
                kt_ = 0
                for sb in range(NSB):
                    xs_, uT_, ya = xs[sb % 2], uT[sb % 2], yacc[sb % 2]
                    S.dma("sp", xs_[:], XS[sb * 512:(sb + 1) * 512, :].rearrange("(a p) d -> p a d", p=128), reads=xs_b, writes=[xs_])
                    for t4 in range(4):
                        pT = pTs[kt_ % 2]
                        kt_ += 1
                        for c in range(8):
                            S.op("pe", lambda e: e.transpose(out=pT[:, c, :], in_=xs_[:, t4, c * 128:(c + 1) * 128], identity=self.identb[:]), reads=[xs_, self.identb], writes=[pT], sig=(c == 7))
                        S.op("act", lambda e: e.copy(out=uT_[:, :, t4 * 128:(t4 + 1) * 128], in_=pT[:]), reads=[pT], writes=[uT_])
                    for g in range(G):
                        wa, wb_, wc = w1g[gi % 2], w3g[gi % 2], w2g[gi % 2]
                        gi += 1
                        off = bass.IndirectOffsetOnAxis(ap=idxw[:, sb, g:g + 1], axis=0)
                        S.idma(wa[:].rearrange("p c f -> p (c f)"), None, W["mg1"][:, :], off, reads=[idxw] + [W["mg1_b"][ex * G + g] for ex in range(NEXP)], writes=[wa])
                        off = bass.IndirectOffsetOnAxis(ap=idxw[:, sb, g:g + 1], axis=0)
                        S.idma(wb_[:].rearrange("p c f -> p (c f)"), None, W["mg3"][:, :], off, reads=[idxw] + [W["mg3_b"][ex * G + g] for ex in range(NEXP)], writes=[wb_])
                        off = bass.IndirectOffsetOnAxis(ap=idxw[:, sb, g:g + 1], axis=0)
                        S.idma(wc[:].rearrange("p j d -> p (j d)"), None, W["mg2"][:, :], off, reads=[idxw] + [W["mg2_b"][ex * G + g] for ex in range(NEXP)], writes=[wc])
                        h_ = hT[g % 2]
                        nv = nval[g]
                        for j in range(nv):
                            a1, a3, s_ = pp1[k1 % 2], pp3[k1 % 2], sl_[k1 % 2]
                            k1 += 1
                            for c in range(8):
                                S.op("pe", lambda e: e.matmul(a1[:], lhsT=wa[:, c, j * 128:(j + 1) * 128], rhs=uT_[:, c, :], start=(c == 0), stop=(c == 7)), reads=[wa, uT_], writes=[a1])
                            for c in range(8):
                                S.op("pe", lambda e: e.matmul(a3[:], lhsT=wb_[:, c, j * 128:(j + 1) * 128], rhs=uT_[:, c, :], start=(c == 0), stop=(c == 7)), reads=[wb_, uT_], writes=[a3])
                            S.op("act", lambda e: e.activation(out=s_[:], in_=a1[:], func=AF.Silu), reads=[a1], writes=[s_])
                            S.op("dve", lambda e: e.tensor_tensor(out=h_[:, j, :], in0=s_[:], in1=a3[:], op=ALU.mult), reads=[s_, a3], writes=[h_])
                        for t4 in range(4):
                            for dg in range(2):
                                y_ = py[ky % 2]
                                ky += 1
                                for j in range(nv):
                                    S.op("pe", lambda e: e.matmul(y_[:], lhsT=h_[:, j, t4 * 128:(t4 + 1) * 128], rhs=wc[:, j, dg * 512:(dg + 1) * 512], start=(j == 0), stop=(j == nv - 1)), reads=[h_, wc], writes=[y_])
                                yv = ya[:, t4, dg * 512:(dg + 1) * 512]
                                if g == 0:
                                    S.op("act", lambda e: e.copy(out=yv, in_=y_[:]), reads=[y_], writes=[ya])
                                else:
                                    S.op("dve", lambda e: e.tensor_tensor(out=yv, in0=y_[:], in1=yv, op=ALU.add), reads=[y_, ya], writes=[ya])
                    S.dma("sp", YS[sb * 512:(sb + 1) * 512, :].rearrange("(a p) d -> p a d", p=128), ya[:], reads=[ya], writes=[ys_b[sb]])
            with self.scope() as p5:
                ya_ = [self.sb(p5, "m_ya%d" % i, [128, D], F32) for i in range(4)]
                yb_ = [self.sb(p5, "m_yb%d" % i, [128, D], F32) for i in range(4)]
                xt = [self.sb(p5, "m_x5_%d" % i, [128, D], F32) for i in range(4)]
                junk5 = [self.sb(p5, "m_junk5_%d" % i, [128, D], BF16) for i in range(2)]
                ss5 = [self.sb(p5, "m_ss5_%d" % i, [128, 4], F32) for i in range(2)]
                xo = [self.sb(p5, "m_xo%d" % i, [128, D], F32) for i in range(4)]
                for tt in range(NTL):
                    a_, b_, x_, xo_ = ya_[tt % 4], yb_[tt % 4], xt[tt % 4], xo[tt % 4]
                    S.dma("sp", x_[:], xin[tt * 128:(tt + 1) * 128, :], reads=[xin_bufs[tt]], writes=[x_])
                    S.idma(a_[:], None, YS[:, :], bass.IndirectOffsetOnAxis(ap=sl_i[:, 0, tt:tt + 1], axis=0), reads=ys_b + [sl_i.b], writes=[a_])
                    S.idma(b_[:], None, YS[:, :], bass.IndirectOffsetOnAxis(ap=sl_i[:, 1, tt:tt + 1], axis=0), reads=ys_b + [sl_i.b], writes=[b_])
                    S.op("dve", lambda e: e.tensor_scalar(out=a_[:], in0=a_[:], scalar1=g12[:, 0, tt:tt + 1], scalar2=None, op0=ALU.mult), reads=[a_, g12], writes=[a_])
                    S.op("dve", lambda e: e.scalar_tensor_tensor(out=a_[:], in0=b_[:], scalar=g12[:, 1, tt:tt + 1], in1=a_[:], op0=ALU.mult, op1=ALU.add), reads=[b_, g12, a_], writes=[a_])
                    self.post_norm_residual(a_, x_, bc["ggt_f"], ss5[tt % 2], junk5[tt % 2], xo_)
                    S.dma("sp", xout[tt * 128:(tt + 1) * 128, :], xo_[:], reads=[xo_], writes=[xout_bufs[tt]])

    def convert_moe(self, w1, w3, w2):
        G, FW, NJ = 6, 512, 4
        out = {}
        for nm, src in (("mg1", w1), ("mg3", w3)):
            dst = self.nc.dram_tensor("wb_" + nm, [NEXP * G * 128, 8 * FW], BF16, kind="Internal").ap()
            bufs = []
            for ex in range(NEXP):
                for g in range(G):
                    r0 = (ex * G + g) * 128
                    b = Buf("%s_%d_%d" % (nm, ex, g))
                    fw = min(FW, DFF - g * FW)
                    self.S.dma("pool", dst[r0:r0 + 128, :].rearrange("p (c f) -> p c f", f=FW)[:, :, 0:fw],
                               src[ex * D:(ex + 1) * D, g * FW:g * FW + fw].rearrange("(c p) f -> p c f", p=128), writes=[b])
                    bufs.append(b)
            out[nm], out[nm + "_b"] = dst, bufs
        dst = self.nc.dram_tensor("wb_mg2", [NEXP * G * 128, NJ * D], BF16, kind="Internal").ap()
        bufs = []
        for ex in range(NEXP):
            for g in range(G):
                r0 = (ex * G + g) * 128
                b = Buf("mg2_%d_%d" % (ex, g))
                fw = min(FW, DFF - g * FW)
                self.S.dma("pool", dst[r0:r0 + 128, :].rearrange("p (j d) -> p j d", d=D)[:, 0:fw // 128, :],
                           w2[ex * DFF + g * FW:ex * DFF + g * FW + fw, :].rearrange("(j p) d -> p j d", p=128), writes=[b])
                bufs.append(b)
        out["mg2"], out["mg2_b"] = dst, bufs
        return out

    def conv_phase(self, xin, xin_bufs, xout, xout_bufs, bc, P):
        S, nc, T = self.S, self.nc, self.T
        NT5 = T // 512
        HW = T + 30
        hc = self.dscr("hc_scr", [8, 128, HW], BF16)
        hc_bufs = [Buf("hc%d" % i) for i in range(NT5)]
        hc_pad = Buf("hc_pad")
        with self.scope() as es:
            pw1 = self.sb(es, "c_pw1", [128, 8, 2 * D], BF16)
            S.dma("sp", pw1[:], P["pw1"].rearrange("(c p) n -> p c n", p=128), reads=P["pw1_b"], writes=[pw1])
            b1 = self.sb(es, "c_b1", [128, 16], F32)
            S.dma("sp", b1[:], self.inp["l1_conv_pw1_b"].rearrange("(c p) -> p c", p=128), writes=[b1], allow_slow_non_contiguous=True)
            b1h = self.sb(es, "c_b1h", [128, 8], F32)
            S.op("dve", lambda e: e.tensor_scalar(out=b1h[:], in0=b1[:, 8:16], scalar1=0.5, scalar2=None, op0=ALU.mult), reads=[b1], writes=[b1h])
            zt = self.sb(es, "c_z", [128, 8, 15], BF16)
            S.op("dve", lambda e: e.memset(zt[:], 0.0), writes=[zt])
            S.dma("sp", hc[:, :, 0:15].rearrange("c p t -> p c t"), zt[:], reads=[zt], writes=[hc_pad])
            S.dma("sp", hc[:, :, HW - 15:HW].rearrange("c p t -> p c t"), zt[:], reads=[zt], writes=[hc_pad])
            xt = [self.sb(es, "c_xt%d" % i, [128, D], F32) for i in range(2)]
            tmp = [self.sb(es, "c_tmp%d" % i, [128, D], F32) for i in range(2)]
            junk = self.sb(es, "c_junk", [128, D], BF16)
            u = [self.sb(es, "c_u%d" % i, [128, D], BF16) for i in range(2)]
            ss = [self.sb(es, "c_ss%d" % i, [128, 4], F32) for i in range(2)]
            uT = [self.sb(es, "c_uT%d" % i, [128, 8, 512], BF16) for i in range(2)]
            hf = [self.sb(es, "c_hf%d" % i, [128, 8, 512], BF16) for i in range(2)]
            sg = [self.sb(es, "c_sg%d" % i, [128, 512], F32) for i in range(2)]
            pT = [self.ps(es, "c_pT%d" % i, [128, 8, 128], BF16) for i in range(2)]
            pa = [self.ps(es, "c_pa%d" % i, [128, 512], F32) for i in range(2)]
            pg = [self.ps(es, "c_pg%d" % i, [128, 512], F32) for i in range(2)]
            k = 0
            for t5 in range(NT5):
                uT_ = uT[t5 % 2]
                for t4 in range(4):
                    ti = t5 * 4 + t4
                    x_ = xt[ti % 2]
                    S.dma("sp", x_[:], xin[ti * 128:(ti + 1) * 128, :], reads=[xin_bufs[ti]], writes=[x_])
                    self.norm_mod_T(x_, bc["gmod_m"], bc["shift_m"], uT_, t4 * 128, ss, tmp, u, pT, junk)
                h_ = hf[t5 % 2]
                for j in range(8):
                    a_, g_, s_ = pa[k % 2], pg[k % 2], sg[k % 2]
                    k += 1
                    for c in range(8):
                        S.op("pe", lambda e: e.matmul(a_[:], lhsT=pw1[:, c, j * 128:(j + 1) * 128], rhs=uT_[:, c, :], start=(c == 0), stop=(c == 7)),
                             reads=[pw1, uT_], writes=[a_])
                    for c in range(8):
                        S.op("pe", lambda e: e.matmul(g_[:], lhsT=pw1[:, c, D + j * 128:D + (j + 1) * 128], rhs=uT_[:, c, :], start=(c == 0), stop=(c == 7)),
                             reads=[pw1, uT_], writes=[g_])
                    S.op("act", lambda e: e.activation(out=s_[:], in_=g_[:], func=AF.Tanh, scale=0.5, bias=b1h[:, j:j + 1]), reads=[g_, b1h], writes=[s_])
                    S.op("dve", lambda e: e.tensor_scalar(out=s_[:], in0=s_[:], scalar1=0.5, scalar2=0.5, op0=ALU.mult, op1=ALU.add), reads=[s_], writes=[s_])
                    S.op("dve", lambda e: e.scalar_tensor_tensor(out=h_[:, j, :], in0=a_[:], scalar=b1[:, j:j + 1], in1=s_[:], op0=ALU.add, op1=ALU.mult),
                         reads=[a_, b1, s_], writes=[h_])
                S.dma("sp", hc[:, :, 15 + t5 * 512:15 + (t5 + 1) * 512].rearrange("c p t -> p c t"), h_[:], reads=[h_], writes=[hc_bufs[t5]])
        with self.scope() as es:
            pw2 = self.sb(es, "c_pw2", [128, 8, D], BF16)
            S.dma("sp", pw2[:], P["pw2"].rearrange("(c p) n -> p c n", p=128), reads=P["pw2_b"], writes=[pw2])
            dwT = self.sb(es, "c_dwT", [128, 8, 31], F32)
            for j in range(8):
                S.dma("sp", dwT[:, j, :], self.inp["l1_conv_dw_w"][:, j * 128:(j + 1) * 128].rearrange("k p -> p k"), writes=[dwT],
                      allow_slow_non_contiguous=True)
            vecs = self.sb(es, "c_vecs", [128, 3, 8], F32)
            for i, nm in enumerate(["l1_conv_dw_b", "l1_conv_ln_g", "l1_conv_ln_b"]):
                S.dma("sp", vecs[:, i, :], self.inp[nm].rearrange("(c p) -> p c", p=128), writes=[vecs], allow_slow_non_contiguous=True)
            diag = self.sb(es, "c_diag", [128, 8 * 31, 128], BF16)
            for j in range(8):
                for kk in range(31):
                    eng = "dve" if (kk % 2 == 0) else "pool"
                    S.op(eng, lambda e: e.tensor_scalar(out=diag[:, j * 31 + kk, :], in0=self.identf[:], scalar1=dwT[:, j, kk:kk + 1], scalar2=None, op0=ALU.mult),
                         reads=[self.identf, dwT], writes=[diag])
            pb2 = self.sb(es, "c_pb2", [128, D], F32)
            S.dma("sp", pb2[:], self.inp["l1_conv_pw2_b"].partition_broadcast(128), writes=[pb2])
            hw = [self.sb(es, "c_hw%d" % i, [128, 8, 542], BF16) for i in range(2)]
            cf = self.sb(es, "c_cf", [128, 8, 512], F32)
            cb = self.sb(es, "c_cb", [128, 8, 512], BF16)
            cq = self.sb(es, "c_cq", [128, 8, 512], BF16)
            hn = self.sb(es, "c_hn", [128, 8, 512], BF16)
            mean = self.sb(es, "c_mean", [128, 512], F32)
            rstd = self.sb(es, "c_rstd", [128, 512], F32)
            tq = [self.sb(es, "c_tq%d" % i, [128, 512], F32) for i in range(2)]
            xt = [self.sb(es, "c2_xt%d" % i, [128, D], F32) for i in range(2)]
            ys = [self.sb(es, "c2_y%d" % i, [128, D], F32) for i in range(2)]
            xos = [self.sb(es, "c2_xo%d" % i, [128, D], F32) for i in range(2)]
            junk = self.sb(es, "c2_junk", [128, D], BF16)
            sss = [self.sb(es, "c2_ss%d" % i, [128, 4], F32) for i in range(2)]
            pc = [self.ps(es, "c_pc%d" % i, [128, 512], F32) for i in range(2)]
            pm = self.ps(es, "c_pm", [128, 512], F32)
            pq = self.ps(es, "c_pq", [128, 512], F32)
            py = [self.ps(es, "c_py%d" % i, [128, 512], F32) for i in range(2)]
            k = 0
            ky = 0
            for t5 in range(NT5):
                hw_ = hw[t5 % 2]
                rd = [hc_pad] + [hc_bufs[i] for i in (t5 - 1, t5, t5 + 1) if 0 <= i < NT5]
                S.dma("sp", hw_[:], hc[:, :, t5 * 512:t5 * 512 + 542].rearrange("c p t -> p c t"), reads=rd, writes=[hw_])
                for j in range(8):
                    c_ = pc[k % 2]
                    k += 1
                    for kk in range(31):
                        S.op("pe", lambda e: e.matmul(c_[:], lhsT=diag[:, j * 31 + kk, :], rhs=hw_[:, j, kk:kk + 512], start=(kk == 0), stop=(kk == 30)),
                             reads=[diag, hw_], writes=[c_])
                    S.op("act", lambda e: e.activation(out=cf[:, j, :], in_=c_[:], func=AF.Identity, bias=vecs[:, 0, j:j + 1]), reads=[c_, vecs], writes=[cf])
                    S.op("act", lambda e: e.activation(out=cq[:, j, :], in_=c_[:], func=AF.Square, bias=vecs[:, 0, j:j + 1]), reads=[c_, vecs], writes=[cq])
                    S.op("dve", lambda e: e.tensor_copy(out=cb[:, j, :], in_=cf[:, j, :]), reads=[cf], writes=[cb])
                for j in range(8):
                    S.op("pe", lambda e: e.matmul(pm[:], lhsT=self.onesb[:], rhs=cb[:, j, :], start=(j == 0), stop=(j == 7)), reads=[self.onesb, cb], writes=[pm])
                for j in range(8):
                    S.op("pe", lambda e: e.matmul(pq[:], lhsT=self.onesb[:], rhs=cq[:, j, :], start=(j == 0), stop=(j == 7)), reads=[self.onesb, cq], writes=[pq])
                S.op("act", lambda e: e.copy(out=mean[:], in_=pm[:]), reads=[pm], writes=[mean])
                S.op("dve", lambda e: e.tensor_tensor(out=rstd[:], in0=mean[:], in1=mean[:], op=ALU.mult), reads=[mean], writes=[rstd])
                S.op("dve", lambda e: e.tensor_tensor(out=rstd[:], in0=pq[:], in1=rstd[:], op=ALU.subtract), reads=[pq, rstd], writes=[rstd])
                S.op("act", lambda e: e.activation(out=rstd[:], in_=rstd[:], func=AF.Ln, bias=self.epsc[:, 0:1]), reads=[rstd, self.epsc], writes=[rstd])
                S.op("act", lambda e: e.activation(out=rstd[:], in_=rstd[:], func=AF.Exp, scale=-0.5), reads=[rstd], writes=[rstd])
                for j in range(8):
                    t_ = tq[j % 2]
                    S.op("dve", lambda e: e.tensor_tensor(out=t_[:], in0=cf[:, j, :], in1=mean[:], op=ALU.subtract), reads=[cf, mean], writes=[t_])
                    S.op("dve", lambda e: e.tensor_tensor(out=t_[:], in0=t_[:], in1=rstd[:], op=ALU.mult), reads=[t_, rstd], writes=[t_])
                    S.op("act", lambda e: e.activation(out=hn[:, j, :], in_=t_[:], func=AF.Silu, scale=vecs[:, 1, j:j + 1], bias=vecs[:, 2, j:j + 1]),
                         reads=[t_, vecs], writes=[hn])
                for t4 in range(4):
                    ti = t5 * 4 + t4
                    x_ = xt[ti % 2]
                    y, xo, ss = ys[ti % 2], xos[ti % 2], sss[ti % 2]
                    S.dma("sp", x_[:], xin[ti * 128:(ti + 1) * 128, :], reads=[xin_bufs[ti]], writes=[x_])
                    for dg in range(2):
                        y_ = py[ky % 2]
                        ky += 1
                        for j in range(8):
                            S.op("pe", lambda e: e.matmul(y_[:], lhsT=hn[:, j, t4 * 128:(t4 + 1) * 128], rhs=pw2[:, j, dg * 512:(dg + 1) * 512], start=(j == 0), stop=(j == 7)),
                                 reads=[hn, pw2], writes=[y_])
                        S.op("dve", lambda e: e.tensor_tensor(out=y[:, dg * 512:(dg + 1) * 512], in0=y_[:], in1=pb2[:, dg * 512:(dg + 1) * 512], op=ALU.add),
                             reads=[y_, pb2], writes=[y])
                    self.post_norm_residual(y, x_, bc["ggt_m"], ss, junk, xo)
                    S.dma("sp", xout[ti * 128:(ti + 1) * 128, :], xo[:], reads=[xo], writes=[xout_bufs[ti]])


    def proj_phase(self, x, x_bufs, bc, SC):
        S, nc, T = self.S, self.nc, self.T
        I = self.inp
        NTL = T // 128
        with self.scope() as es:
            wfm = self.sb(es, "a_wfm", [128, 8, 1024], BF16)
            wtm = self.sb(es, "a_wtm", [128, 8, 2688], BF16)
            S.dma("pool", wfm[:], I["k_wfm"].rearrange("(c p) n -> p c n", p=128), writes=[wfm])
            S.dma("pool", wtm[:], I["k_wtm"].rearrange("(c p) n -> p c n", p=128), writes=[wtm])
            xt = [self.sb(es, "a_xt%d" % i, [128, D], F32) for i in range(2)]
            tmp = [self.sb(es, "a_tmp%d" % i, [128, D], F32) for i in range(2)]
            junk = self.sb(es, "a_junk", [128, D], BF16)
            u = [self.sb(es, "a_u%d" % i, [128, D], BF16) for i in range(2)]
            ss = [self.sb(es, "a_ss%d" % i, [128, 4], F32) for i in range(2)]
            uT = [self.sb(es, "a_uT%d" % i, [128, 8, 512], BF16) for i in range(2)]
            vt = [self.sb(es, "a_vt%d" % i, [128, 1024], BF16) for i in range(2)]
            ot = [self.sb(es, "a_ot%d" % i, [128, 512], F32) for i in range(2)]
            qk = self.sb(es, "a_qk", [128, 1024], F32)
            sq = self.sb(es, "a_sq", [128, 1024], F32)
            n2 = self.sb(es, "a_n2", [128, 16], F32)
            rot = self.sb(es, "a_rot", [128, 1024], BF16)
            rp = [self.sb(es, "a_rp%d" % i, [128, 64], F32) for i in range(2)]
            r1 = self.sb(es, "a_r1", [128, 16, 16], F32)
            r2 = self.sb(es, "a_r2", [128, 16, 16], F32)
            qkT = [self.sb(es, "a_qkT%d" % i, [128, 8, 512], BF16) for i in range(2)]
            gt = self.sb(es, "a_gt", [128, 128], F32)
            gT = [self.sb(es, "a_gT%d" % i, [64, 2, 512], F32) for i in range(2)]
            pq = [self.sb(es, "a_pq%d" % i, [128, 8, 512], F32) for i in range(1)]
            zc = self.sb(es, "a_zc", [128, 4, 1], F32)
            pT = self.ps(es, "a_pT", [128, 8, 128], BF16)
            pTn = self.ps(es, "a_pTn", [128, 8, 128], BF16)
            pG = self.ps(es, "a_pG", [64, 2, 128], F32)
            ptm = [self.ps(es, "a_ptm%d" % i, [128, 512], F32) for i in range(3)]
            pfm = [self.ps(es, "a_pfm%d" % i, [128, 512], F32) for i in range(2)]
            S.op("dve", lambda e: e.memset(zc[:], 0.0), writes=[zc])
            S.op("dve", lambda e: e.memset(self.q2max[:], 0.0), writes=[self.q2max])
            S.op("dve", lambda e: e.memset(self.k2max[:], 0.0), writes=[self.k2max])
            for (arr, L) in ((SC["PQ"], T), (SC["PK"], T), (SC["PKc"], CTX)):
                for col in (0, L + 1):
                    S.dma("sp", arr[:, :, col:col + 1].rearrange("c p t -> p c t"), zc[:], reads=[zc], writes=[SC["pad_b"]], allow_slow_non_contiguous=True)
            groups = [("c", 0, 2)] + [("l", g * 4, 4) for g in range(NTL // 4)]
            ktm = 0
            kfm = 0
            flat = [(gi, kind, t0 + tt, tt) for gi, (kind, t0, n) in enumerate(groups) for tt in range(n)]

            def do_norm(k):
                gi_, kind_, ti_, tt_ = flat[k]
                x_ = xt[k % 2]
                if kind_ == "c":
                    S.dma("sp", x_[:], I["ctx"][ti_ * 128:(ti_ + 1) * 128, :], writes=[x_])
                    self.norm_mod_T(x_, bc["gmod_c"], bc["shift_c"], uT[gi_ % 2], tt_ * 128, ss, tmp, u, pTn, junk)
                else:
                    S.dma("sp", x_[:], x[ti_ * 128:(ti_ + 1) * 128, :], reads=[x_bufs[ti_]], writes=[x_])
                    self.norm_mod_T(x_, bc["gmod_m"], bc["shift_m"], uT[gi_ % 2], tt_ * 128, ss, tmp, u, pTn, junk)

            do_norm(0)
            kflat = 0
            for gi, (kind, t0, n) in enumerate(groups):
                uT_ = uT[gi % 2]
                qkT_ = qkT[gi % 2]
                gT_ = gT[gi % 2]
                N = n * 128
                for tt in range(n):
                    ti = t0 + tt
                    if kflat + 1 < len(flat):
                        do_norm(kflat + 1)
                    kflat += 1
                    if kind == "c":
                        srow = ti * 128
                    else:
                        srow = CTX + ti * 128
                        rp_ = rp[ti % 2]
                        S.dma("sp", rp_[:], I["k_rope"][ti * 128:(ti + 1) * 128, :], writes=[rp_])
                    sb_i = srow // 128
                    vt_ = vt[ti % 2]
                    ot_ = ot[ti % 2]
                    for g in range(6):
                        c0, c1 = g * 512, min((g + 1) * 512, 2688)
                        if kind == "c" and g in (1, 3):
                            continue
                        p_ = ptm[ktm % 3]
                        ktm += 1
                        for c in range(8):
                            S.op("pe", lambda e: e.matmul(p_[:, 0:c1 - c0], lhsT=uT_[:, c, tt * 128:(tt + 1) * 128], rhs=wtm[:, c, c0:c1], start=(c == 0), stop=(c == 7)),
                                 reads=[uT_, wtm], writes=[p_])
                        if g == 0:
                            S.op("act", lambda e: e.copy(out=vt_[:, 0:512], in_=p_[:]), reads=[p_], writes=[vt_])
                        elif g == 1:
                            S.op("act", lambda e: e.copy(out=ot_[:], in_=p_[:]), reads=[p_], writes=[ot_])
                            S.dma("sp", SC["OM"][ti * 128:(ti + 1) * 128, :], ot_[:], reads=[ot_], writes=[SC["OM_b"][ti]])
                        elif g == 2:
                            S.op("act", lambda e: e.copy(out=vt_[:, 512:1024], in_=p_[:]), reads=[p_], writes=[vt_])
                            S.dma("sp", SC["VM"][srow:srow + 128, :], vt_[:, 0:512], reads=[vt_], writes=[SC["VM_b"][sb_i]])
                            S.dma("sp", SC["VD"][srow:srow + 128, :], vt_[:, 512:1024], reads=[vt_], writes=[SC["VD_b"][sb_i]])
                        elif g == 3:
                            S.op("act", lambda e: e.copy(out=qk[:, 0:512], in_=p_[:]), reads=[p_], writes=[qk])
                        elif g == 4:
                            S.op("act", lambda e: e.copy(out=qk[:, 512:1024], in_=p_[:]), reads=[p_], writes=[qk])
                        else:
                            S.op("act", lambda e: e.copy(out=gt[:], in_=p_[:, 0:128]), reads=[p_], writes=[gt])
                    lo = 512 if kind == "c" else 0
                    S.op("dve", lambda e: e.tensor_tensor(out=sq[:, lo:1024], in0=qk[:, lo:1024], in1=qk[:, lo:1024], op=ALU.mult), reads=[qk], writes=[sq])
                    S.op("dve", lambda e: e.tensor_reduce(out=n2[:, lo // 64:16], in_=sq[:, lo:1024].rearrange("p (g d) -> p g d", d=64), axis=AX.X, op=ALU.add),
                         reads=[sq], writes=[n2])
                    if kind != "c":
                        S.op("dve", lambda e: e.tensor_tensor(out=self.q2max[:], in0=self.q2max[:], in1=n2[:, 0:8], op=ALU.max), reads=[self.q2max, n2], writes=[self.q2max])
                    S.op("dve", lambda e: e.tensor_tensor(out=self.k2max[:], in0=self.k2max[:], in1=n2[:, 8:16], op=ALU.max), reads=[self.k2max, n2], writes=[self.k2max])
                    if kind == "c":
                        S.op("dve", lambda e: e.tensor_copy(out=rot[:, 512:1024], in_=qk[:, 512:1024]), reads=[qk], writes=[rot])
                    else:
                        q3 = qk[:].rearrange("p (g d) -> p g d", d=64)
                        o3 = rot[:].rearrange("p (g d) -> p g d", d=64)
                        for a in range(2):
                            b0 = a * 32
                            cosb = rp_[:, b0:b0 + 16].unsqueeze(1).to_broadcast([128, 16, 16])
                            sinb = rp_[:, b0 + 16:b0 + 32].unsqueeze(1).to_broadcast([128, 16, 16])
                            x1 = q3[:, :, b0:b0 + 16]
                            x2 = q3[:, :, b0 + 16:b0 + 32]
                            S.op("dve", lambda e: e.tensor_tensor(out=r1[:], in0=x1, in1=cosb, op=ALU.mult), reads=[qk, rp_], writes=[r1])
                            S.op("dve", lambda e: e.tensor_tensor(out=r2[:], in0=x2, in1=sinb, op=ALU.mult), reads=[qk, rp_], writes=[r2])
                            S.op("dve", lambda e: e.tensor_tensor(out=o3[:, :, b0:b0 + 16], in0=r1[:], in1=r2[:], op=ALU.subtract), reads=[r1, r2], writes=[rot])
                            S.op("dve", lambda e: e.tensor_tensor(out=r1[:], in0=x2, in1=cosb, op=ALU.mult), reads=[qk, rp_], writes=[r1])
                            S.op("dve", lambda e: e.tensor_tensor(out=r2[:], in0=x1, in1=sinb, op=ALU.mult), reads=[qk, rp_], writes=[r2])
                            S.op("dve", lambda e: e.tensor_tensor(out=o3[:, :, b0 + 16:b0 + 32], in0=r1[:], in1=r2[:], op=ALU.add), reads=[r1, r2], writes=[rot])
                    c_lo = 4 if kind == "c" else 0
                    for c in range(c_lo, 8):
                        S.op("pe", lambda e: e.transpose(out=pT[:, c, :], in_=rot[:, c * 128:(c + 1) * 128], identity=self.identb[:]), reads=[rot, self.identb], writes=[pT], sig=(c == 7))
                    S.op("act", lambda e: e.copy(out=qkT_[:, c_lo:8, tt * 128:(tt + 1) * 128], in_=pT[:, c_lo:8, :]), reads=[pT], writes=[qkT_])
                    for hh in range(2):
                        S.op("pe", lambda e: e.transpose(out=pG[:, hh, :], in_=gt[:, hh * 64:(hh + 1) * 64], identity=self.identf[:]), reads=[gt, self.identf], writes=[pG], sig=(hh == 1))
                    S.op("act", lambda e: e.copy(out=gT_[:, :, tt * 128:(tt + 1) * 128], in_=pG[:]), reads=[pG], writes=[gT_])
                s0 = 0 if kind == "c" else CTX + t0 * 128
                if kind != "c":
                    S.dma("sp", SC["QD"][:, :, t0 * 128:t0 * 128 + N].rearrange("c p t -> p c t"), qkT_[:, 0:4, 0:N], reads=[qkT_], writes=[SC["QD_b"][gi]])
                S.dma("sp", SC["KD"][:, :, s0:s0 + N].rearrange("c p t -> p c t"), qkT_[:, 4:8, 0:N], reads=[qkT_], writes=[SC["KD_b"][gi]])
                S.dma("sp", SC["GT"][:, :, s0:s0 + N].rearrange("g p t -> p g t"), gT_[:, :, 0:N], reads=[gT_], writes=[SC["GT_b"][gi]])
                pq_ = pq[0]
                for ch in range(c_lo, 8):
                    f_ = pfm[kfm % 2]
                    kfm += 1
                    for c in range(8):
                        S.op("pe", lambda e: e.matmul(f_[:, 0:N], lhsT=wfm[:, c, ch * 128:(ch + 1) * 128], rhs=uT_[:, c, 0:N], start=(c == 0), stop=(c == 7)),
                             reads=[wfm, uT_], writes=[f_])
                    S.op("act", lambda e: e.copy(out=pq_[:, ch, 0:N], in_=f_[:, 0:N]), reads=[f_], writes=[pq_])
                if kind == "c":
                    S.dma("sp", SC["PKc"][:, :, 1:1 + N].rearrange("c p t -> p c t"), pq_[:, 4:8, 0:N], reads=[pq_], writes=[SC["PKc_b"]])
                else:
                    S.dma("sp", SC["PQ"][:, :, 1 + t0 * 128:1 + t0 * 128 + N].rearrange("c p t -> p c t"), pq_[:, 0:4, 0:N], reads=[pq_], writes=[SC["PQ_b"][gi - 1]])
                    S.dma("sp", SC["PK"][:, :, 1 + t0 * 128:1 + t0 * 128 + N].rearrange("c p t -> p c t"), pq_[:, 4:8, 0:N], reads=[pq_], writes=[SC["PK_b"][gi - 1]])


    def mlstm_phase(self, SC):
        S, nc, T = self.S, self.nc, self.T
        I = self.inp
        Sall = CTX + T
        NC = Sall // 128
        NCX = CTX // 128
        with self.scope() as es:
            cw = self.sb(es, "b_cw", [128, 8, 3], F32)
            for j in range(8):
                S.dma("sp", cw[:, j, :], I["l0_mlstm_conv_w"][:, j * 128:(j + 1) * 128].rearrange("k p -> p k"), writes=[cw], allow_slow_non_contiguous=True)
            win = [self.sb(es, "b_win%d" % i, [128, 4, 514], F32) for i in range(2)]
            acc = [self.sb(es, "b_acc%d" % i, [128, 512], F32) for i in range(2)]
            qs = [self.sb(es, "b_qs%d" % i, [128, 4, 512], BF16) for i in range(2)]
            ktm = [self.sb(es, "b_ktm%d" % i, [128, 512], BF16) for i in range(2)]
            pT = self.ps(es, "b_pT", [128, 4, 128], BF16)
            jobs = [("kc", SC["PKc"], SC["KMT"], 0, CTX, 4)]
            for g in range(T // 512):
                jobs.append(("q", SC["PQ"], SC["QMT"], g * 512, 512, 0))
                jobs.append(("k", SC["PK"], SC["KMT"], g * 512, 512, 4))
            for ji, (kind, src, dst, t0, N, cb0) in enumerate(jobs):
                w_ = win[ji % 2]
                q_ = qs[ji % 2]
                if kind == "kc":
                    rd = [SC["pad_b"], SC["PKc_b"]]
                else:
                    bl = SC["PQ_b"] if kind == "q" else SC["PK_b"]
                    g = t0 // 512
                    rd = [SC["pad_b"]] + [bl[i] for i in (g - 1, g, g + 1) if 0 <= i < len(bl)]
                S.dma("sp", w_[:, :, 0:N + 2], src[:, :, t0:t0 + N + 2].rearrange("c p t -> p c t"), reads=rd, writes=[w_])
                eng = "dve"
                for c in range(4):
                    a_ = acc[c % 2]
                    S.op(eng, lambda e: e.tensor_scalar(out=a_[:, 0:N], in0=w_[:, c, 0:N], scalar1=cw[:, cb0 + c, 0:1], scalar2=None, op0=ALU.mult), reads=[w_, cw], writes=[a_])
                    S.op(eng, lambda e: e.scalar_tensor_tensor(out=a_[:, 0:N], in0=w_[:, c, 1:N + 1], scalar=cw[:, cb0 + c, 1:2], in1=a_[:, 0:N], op0=ALU.mult, op1=ALU.add),
                         reads=[w_, cw, a_], writes=[a_])
                    S.op(eng, lambda e: e.scalar_tensor_tensor(out=a_[:, 0:N], in0=w_[:, c, 2:N + 2], scalar=cw[:, cb0 + c, 2:3], in1=a_[:, 0:N], op0=ALU.mult, op1=ALU.add),
                         reads=[w_, cw, a_], writes=[a_])
                    S.op("act", lambda e: e.activation(out=q_[:, c, 0:N], in_=a_[:, 0:N], func=AF.Silu), reads=[a_], writes=[q_])
                if kind == "q":
                    S.dma("sp", dst[:, :, t0:t0 + N].rearrange("c p t -> p c t"), q_[:, :, 0:N], reads=[q_], writes=[SC["QMT_b"][t0 // 512]])
                else:
                    s0 = t0 if kind == "kc" else CTX + t0
                    bi = 0 if kind == "kc" else 1 + t0 // 512
                    S.dma("sp", dst[:, :, s0:s0 + N].rearrange("c p t -> p c t"), q_[:, :, 0:N], reads=[q_], writes=[SC["KMT_b"][bi]])
                    for tt in range(N // 128):
                        k_ = ktm[tt % 2]
                        for c in range(4):
                            S.op("pe", lambda e: e.transpose(out=pT[:, c, :], in_=q_[:, c, tt * 128:(tt + 1) * 128], identity=self.identb[:]), reads=[q_, self.identb], writes=[pT], sig=(c == 3))
                        S.op("act", lambda e: e.copy(out=k_[:].rearrange("p (c d) -> p c d", c=4), in_=pT[:]), reads=[pT], writes=[k_])
                        S.dma("sp", SC["KM"][s0 + tt * 128:s0 + (tt + 1) * 128, :], k_[:], reads=[k_], writes=[SC["KM_b"][(s0 // 128) + tt]])
        with self.scope() as es:
            alT = self.sb(es, "b_alT", [128, NC, 64], F32)
            eeT = self.sb(es, "b_eeT", [128, NC, 64], F32)
            dl = self.sb(es, "b_dl", [128, 8, NC], F32)
            with self.scope() as g_es:
                fg = self.sb(g_es, "g_fg", [64, Sall], F32)
                w1 = self.sb(g_es, "g_w1", [64, Sall], F32)
                w2 = self.sb(g_es, "g_w2", [64, Sall], F32)
                onesr = self.sb(g_es, "g_ones", [64, 1], F32)
                gb = self.sb(g_es, "g_gb", [64, 2], F32)
                gco = self.sb(g_es, "g_co", [64, 4], F32)
                sel = self.sb(g_es, "g_sel", [64, 8, 128], F32)
                cm = self.sb(g_es, "g_cm", [64, NC], F32)
                rend = self.sb(g_es, "g_rend", [64, NC], F32)
                rst = self.sb(g_es, "g_rst", [64, NC], F32)
                dlt = self.sb(g_es, "g_dlt", [64, NC], F32)
                ac = self.sb(g_es, "g_ac", [64, 4], F32)
                pt8 = self.ps(g_es, "g_pt8", [128, 8, 64], F32)
                pdl = self.ps(g_es, "g_pdl", [128, NC], F32)
                S.dma("sp", fg[:], SC["GT"][1, :, :], reads=SC["GT_b"], writes=[fg])
                S.dma("sp", gb[:], I["k_gate_b"][:, :], writes=[gb])
                S.dma("sp", gco[:], I["k_gcoef"][:, :], writes=[gco])
                S.dma("sp", sel[:], I["k_sel"][:, :, :], writes=[sel])
                S.op("dve", lambda e: e.memset(onesr[:], 1.0), writes=[onesr])
                S.op("dve", lambda e: e.tensor_scalar(out=fg[:], in0=fg[:], scalar1=gb[:, 1:2], scalar2=None, op0=ALU.add), reads=[fg, gb], writes=[fg])
                S.op("dve", lambda e: e.scalar_tensor_tensor(out=w1[:], in0=fg[:], scalar=-1.0, in1=fg[:], op0=ALU.mult, op1=ALU.max), reads=[fg], writes=[w1])
                S.op("act", lambda e: e.activation(out=w1[:], in_=w1[:], func=AF.Exp, scale=-1.0), reads=[w1], writes=[w1])
                S.op("dve", lambda e: e.tensor_scalar(out=w1[:], in0=w1[:], scalar1=1.0, scalar2=None, op0=ALU.add), reads=[w1], writes=[w1])
                S.op("act", lambda e: e.activation(out=w1[:], in_=w1[:], func=AF.Ln), reads=[w1], writes=[w1])
                S.op("dve", lambda e: e.tensor_scalar(out=w2[:], in0=fg[:], scalar1=0.0, scalar2=None, op0=ALU.min), reads=[fg], writes=[w2])
                S.op("dve", lambda e: e.tensor_tensor(out=fg[:], in0=w2[:], in1=w1[:], op=ALU.subtract), reads=[w1, w2], writes=[fg])
                S.op("dve", lambda e: e.tensor_tensor_scan(out=w1[:], data0=onesr[:, 0:1].to_broadcast([64, Sall]), data1=fg[:], initial=0.0, op0=ALU.mult, op1=ALU.add), reads=[onesr, fg], writes=[w1])
                S.op("dve", lambda e: e.tensor_scalar(out=ac[:, 0:1], in0=w1[:, CTX - 1:CTX], scalar1=gco[:, 1:2], scalar2=None, op0=ALU.mult), reads=[w1, gco], writes=[ac])
                S.op("dve", lambda e: e.tensor_tensor(out=ac[:, 1:2], in0=w1[:, CTX - 1:CTX], in1=w1[:, Sall - 1:Sall], op=ALU.add), reads=[w1], writes=[ac])
                S.op("dve", lambda e: e.tensor_scalar(out=ac[:, 1:2], in0=ac[:, 1:2], scalar1=gco[:, 1:2], scalar2=None, op0=ALU.mult), reads=[ac, gco], writes=[ac])
                S.op("dve", lambda e: e.tensor_scalar(out=w1[:], in0=w1[:], scalar1=gco[:, 0:1], scalar2=None, op0=ALU.mult), reads=[w1, gco], writes=[w1])
                S.op("dve", lambda e: e.scalar_tensor_tensor(out=w1[:], in0=fg[:], scalar=gco[:, 1:2], in1=w1[:], op0=ALU.mult, op1=ALU.add), reads=[fg, gco, w1], writes=[w1])
                S.op("dve", lambda e: e.tensor_scalar(out=w1[:, 0:CTX], in0=w1[:, 0:CTX], scalar1=ac[:, 0:1], scalar2=None, op0=ALU.add), reads=[w1, ac], writes=[w1])
                S.op("dve", lambda e: e.tensor_scalar(out=w1[:, CTX:Sall], in0=w1[:, CTX:Sall], scalar1=ac[:, 1:2], scalar2=None, op0=ALU.add), reads=[w1, ac], writes=[w1])
                S.dma("sp", fg[:], SC["GT"][0, :, :], reads=SC["GT_b"], writes=[fg])
                S.op("dve", lambda e: e.scalar_tensor_tensor(out=w2[:], in0=fg[:], scalar=gb[:, 0:1], in1=w1[:], op0=ALU.add, op1=ALU.subtract),
                     reads=[fg, gb, w1], writes=[w2])
                S.op("dve", lambda e: e.tensor_reduce(out=cm[:], in_=w2[:].rearrange("p (c t) -> p c t", t=128), axis=AX.X, op=ALU.max), reads=[w2], writes=[cm])
                S.op("dve", lambda e: e.memset(rst[:], 0.0), writes=[rst])
                S.op("dve", lambda e: e.tensor_scalar(out=rend[0:32, 0:1], in0=cm[0:32, 0:1], scalar1=0.0, scalar2=None, op0=ALU.max), reads=[cm], writes=[rend])
                for c in range(1, NC):
                    S.op("dve", lambda e: e.tensor_tensor(out=rend[0:32, c:c + 1], in0=rend[0:32, c - 1:c], in1=cm[0:32, c:c + 1], op=ALU.max), reads=[rend, cm], writes=[rend])
                border = list(range(NCX - 1, -1, -1)) + list(range(NC - 1, NCX - 1, -1))
                S.op("dve", lambda e: e.tensor_scalar(out=rend[32:64, border[0]:border[0] + 1], in0=cm[32:64, border[0]:border[0] + 1], scalar1=0.0, scalar2=None, op0=ALU.max),
                     reads=[cm], writes=[rend])
                for pi in range(1, NC):
                    c, pc = border[pi], border[pi - 1]
                    S.op("dve", lambda e: e.tensor_tensor(out=rend[32:64, c:c + 1], in0=rend[32:64, pc:pc + 1], in1=cm[32:64, c:c + 1], op=ALU.max), reads=[rend, cm], writes=[rend])
                S.op("dve", lambda e: e.tensor_copy(out=rst[0:32, 1:NC], in_=rend[0:32, 0:NC - 1]), reads=[rend], writes=[rst])
                for pi in range(1, NC):
                    c, pc = border[pi], border[pi - 1]
                    if pi <= NCX or pi == NC:
                        S.op("dve", lambda e: e.tensor_copy(out=rst[32:64, c:c + 1], in_=rend[32:64, pc:pc + 1]), reads=[rend], writes=[rst])
                if NC - 1 > NCX:
                    S.op("dve", lambda e: e.tensor_copy(out=rst[32:64, NCX:NC - 1], in_=rend[32:64, NCX + 1:NC]), reads=[rend], writes=[rst])
                rb = rend[:].unsqueeze(2).to_broadcast([64, NC, 128])
                S.op("dve", lambda e: e.tensor_tensor(out=w2[:].rearrange("p (c t) -> p c t", t=128), in0=w2[:].rearrange("p (c t) -> p c t", t=128), in1=rb, op=ALU.subtract),
                     reads=[w2, rend], writes=[w2])
                S.op("act", lambda e: e.activation(out=w2[:], in_=w2[:], func=AF.Exp), reads=[w2], writes=[w2])
                S.op("dve", lambda e: e.tensor_tensor(out=w1[:].rearrange("p (c t) -> p c t", t=128), in0=w1[:].rearrange("p (c t) -> p c t", t=128), in1=rb, op=ALU.add),
                     reads=[w1, rend], writes=[w1])
                S.op("act", lambda e: e.activation(out=w1[:], in_=w1[:], func=AF.Exp, scale=-1.0), reads=[w1], writes=[w1])
                S.op("dve", lambda e: e.tensor_tensor(out=dlt[:], in0=rst[:], in1=rend[:], op=ALU.subtract), reads=[rst, rend], writes=[dlt])
                S.op("act", lambda e: e.activation(out=dlt[:], in_=dlt[:], func=AF.Exp), reads=[dlt], writes=[dlt])
                for (src, dstT) in ((w2, alT), (w1, eeT)):
                    for c0 in range(0, NC, 8):
                        nb = min(8, NC - c0)
                        for cc in range(nb):
                            S.op("pe", lambda e: e.transpose(out=pt8[:, cc, :], in_=src[:, (c0 + cc) * 128:(c0 + cc + 1) * 128], identity=self.identf[0:64, 0:64]),
                                 reads=[src, self.identf], writes=[pt8], sig=(cc == nb - 1))
                        S.op("act", lambda e: e.copy(out=dstT[:, c0:c0 + nb, :], in_=pt8[:, 0:nb, :]), reads=[pt8], writes=[dstT])
                for jd in range(8):
                    S.op("pe", lambda e: e.matmul(pdl[:], lhsT=sel[:, jd, :], rhs=dlt[:], start=True, stop=True), reads=[sel, dlt], writes=[pdl])
                    S.op("act", lambda e: e.copy(out=dl[:, jd, :], in_=pdl[:]), reads=[pdl], writes=[dl])
            msk = self.sb(es, "b_msk", [128, 2, 128], F32)
            S.dma("sp", msk[:], I["k_masks"].rearrange("d s t -> s d t"), writes=[msk])
            ng = self.sb(es, "b_ng", [128, 512], F32)
            S.dma("sp", ng[:], I["l0_mlstm_norm_g"].partition_broadcast(128), writes=[ng])
            kT = [self.sb(es, "b_kT%d" % i, [128, 4, 128], BF16) for i in range(4)]
            qT = [self.sb(es, "b_qT%d" % i, [128, 4, 128], BF16) for i in range(4)]
            kt = [self.sb(es, "b_kt%d" % i, [128, 8, 64], BF16) for i in range(4)]
            vt = [self.sb(es, "b_vt%d" % i, [128, 8, 64], BF16) for i in range(4)]
            vaug = [self.sb(es, "b_va%d" % i, [128, 8, 65], BF16) for i in range(4)]
            PT = [self.sb(es, "b_PT%d" % i, [128, 2, 128], BF16) for i in range(4)]
            chat2 = [[self.sb(es, "b_ch%d_%d" % (dd, i), [128, 130], F32) for i in range(4)] for dd in range(2)]
            ctb2 = [[self.sb(es, "b_cb%d_%d" % (dd, i), [128, 130], BF16) for i in range(4)] for dd in range(2)]
            hd = [self.sb(es, "b_hd%d" % i, [128, 8, 64], F32) for i in range(4)]
            hfl = [self.sb(es, "b_hf%d" % i, [128, 512], F32) for i in range(4)]
            ol = [self.sb(es, "b_ol%d" % i, [128, 512], F32) for i in range(4)]
            sq = self.sb(es, "b_sq", [128, 512], F32)
            dn = self.sb(es, "b_dn", [128, 8], F32)
            ssn = self.sb(es, "b_ssn", [128, 8], F32)
            mx = self.sb(es, "b_mx", [128, 512], BF16)
            mxT = [self.sb(es, "b_mxT%d" % i, [128, 4, 128], BF16) for i in range(4)]
            pST = [self.ps(es, "b_pST%d" % i, [128, 2, 128], F32) for i in range(2)]
            pND = [self.ps(es, "b_pND%d" % i, [128, 4, 65], F32) for i in range(2)]
            pU = [self.ps(es, "b_pU%d" % i, [128, 130], F32) for i in range(2)]
            pT4 = self.ps(es, "b_pT4", [128, 4, 128], BF16)
            kst = 0
            ku = 0
            it = 0
            orders = [list(range(NC)), list(range(NCX - 1, -1, -1)) + list(range(NC - 1, NCX - 1, -1))]
            stepof = [{c: i for i, c in enumerate(o)} for o in orders]
            for dd in range(2):
                for j in range(4):
                    S.op("dve", lambda e: e.memset(chat2[dd][j][:], 0.0), writes=[chat2[dd][j]])
            for step in range(NC):
                for d in range(2):
                    chat, ctb = chat2[d], ctb2[d]
                    c = orders[d][step]
                    lat = c >= NCX
                    first = stepof[d][c] < stepof[1 - d][c]
                    b_ = it % 4
                    it += 1
                    kt_, vt_, va_ = kt[b_], vt[b_], vaug[b_]
                    S.dma("sp", kt_[:].rearrange("p h d -> p (h d)"), SC["KM"][c * 128:(c + 1) * 128, :], reads=[SC["KM_b"][c]], writes=[kt_])
                    S.dma("sp", vt_[:].rearrange("p h d -> p (h d)"), SC["VM"][c * 128:(c + 1) * 128, :], reads=[SC["VM_b"][c]], writes=[vt_])
                    alc = alT[:, c, d * 32:d * 32 + 8]
                    S.op("dve", lambda e: e.tensor_tensor(out=va_[:, :, 0:64], in0=vt_[:], in1=alc.unsqueeze(2).to_broadcast([128, 8, 64]), op=ALU.mult), reads=[vt_, alT], writes=[va_])
                    S.op("dve", lambda e: e.tensor_copy(out=va_[:, :, 64:65], in_=alc.unsqueeze(2)), reads=[alT], writes=[va_])
                    for j in range(4):
                        S.op("pool", lambda e: e.tensor_scalar(out=ctb[j][:], in0=chat[j][:], scalar1=dl[:, d * 4 + j, c:c + 1], scalar2=0.125, op0=ALU.mult, op1=ALU.mult),
                             reads=[chat[j], dl], writes=[ctb[j]])
                    if lat:
                        tch = c - NCX
                        kT_, qT_ = kT[b_], qT[b_]
                        S.dma("sp", kT_[:], SC["KMT"][:, :, c * 128:(c + 1) * 128].rearrange("c p t -> p c t"), reads=[SC["KMT_b"][1 + tch // 4]], writes=[kT_])
                        S.dma("sp", qT_[:], SC["QMT"][:, :, tch * 128:(tch + 1) * 128].rearrange("c p t -> p c t"), reads=[SC["QMT_b"][tch // 4]], writes=[qT_])
                        for j in range(4):
                            st_ = pST[kst % 2]
                            kst += 1
                            for hh in range(2):
                                S.op("pe", lambda e: e.matmul(st_[:, hh, :], lhsT=kT_[hh * 64:(hh + 1) * 64, j, :], rhs=qT_[hh * 64:(hh + 1) * 64, j, :], start=True, stop=True),
                                     reads=[kT_, qT_], writes=[st_])
                            S.op("dve", lambda e: e.tensor_tensor(out=PT[j][:], in0=st_[:], in1=msk[:, d:d + 1, :].to_broadcast([128, 2, 128]), op=ALU.mult), reads=[st_, msk], writes=[PT[j]])
                        hd_ = hd[b_]
                        for gq in range(2):
                            nd = pND[gq]
                            for hl in range(4):
                                h = gq * 4 + hl
                                j, hh = h // 2, h % 2
                                S.op("pe", lambda e: e.matmul(nd[:, hl, :], lhsT=qT_[hh * 64:(hh + 1) * 64, j, :], rhs=ctb[j][hh * 64:(hh + 1) * 64, hh * 65:(hh + 1) * 65], start=True, stop=False),
                                     reads=[qT_, ctb[j]], writes=[nd])
                                S.op("pe", lambda e: e.matmul(nd[:, hl, :], lhsT=PT[j][:, hh, :], rhs=va_[:, h, :], start=False, stop=True), reads=[PT[j], va_], writes=[nd])
                            dsl = dn[:, gq * 4:(gq + 1) * 4]
                            S.op("act", lambda e: e.copy(out=dsl.unsqueeze(2), in_=nd[:, :, 64:65]), reads=[nd], writes=[dn])
                            S.op("dve", lambda e: e.scalar_tensor_tensor(out=dsl, in0=dsl, scalar=-1.0, in1=dsl, op0=ALU.mult, op1=ALU.max), reads=[dn], writes=[dn])
                            S.op("dve", lambda e: e.tensor_tensor(out=dsl, in0=dsl, in1=eeT[:, c, d * 32 + gq * 4:d * 32 + gq * 4 + 4], op=ALU.max), reads=[dn, eeT], writes=[dn])
                            S.op("dve", lambda e: e.reciprocal(out=dsl, in_=dsl), reads=[dn], writes=[dn])
                            S.op("dve", lambda e: e.tensor_tensor(out=hd_[:, gq * 4:(gq + 1) * 4, :], in0=nd[:, :, 0:64], in1=dsl.unsqueeze(2).to_broadcast([128, 4, 64]), op=ALU.mult),
                                 reads=[nd, dn], writes=[hd_])
                        trow = tch * 128
                        if first:
                            S.dma("act", SC["HF"][trow:trow + 128, :], hd_[:].rearrange("p h d -> p (h d)"), reads=[hd_], writes=[SC["HF_b"][tch]])
                            o_ = ol[b_]
                            S.dma("sp", o_[:], SC["OM"][trow:trow + 128, :], reads=[SC["OM_b"][tch]], writes=[o_])
                            S.op("act", lambda e: e.activation(out=o_[:], in_=o_[:], func=AF.Exp, scale=-1.0), reads=[o_], writes=[o_])
                            S.op("act", lambda e: e.activation(out=o_[:], in_=o_[:], func=AF.Ln, bias=self.onec[:, 0:1]), reads=[o_, self.onec], writes=[o_])
                            S.op("act", lambda e: e.activation(out=o_[:], in_=o_[:], func=AF.Exp, scale=-1.0), reads=[o_], writes=[o_])
                            S.dma("act", SC["OM"][trow:trow + 128, :], o_[:], reads=[o_], writes=[SC["OM_b"][tch]])
                        else:
                            hf_, o_ = hfl[b_], ol[b_]
                            S.dma("sp", hf_[:], SC["HF"][trow:trow + 128, :], reads=[SC["HF_b"][tch]], writes=[hf_])
                            S.dma("sp", o_[:], SC["OM"][trow:trow + 128, :], reads=[SC["OM_b"][tch]], writes=[o_])
                            hflat = hd_[:].rearrange("p h d -> p (h d)")
                            S.op("dve", lambda e: e.tensor_tensor(out=hf_[:], in0=hf_[:], in1=hflat, op=ALU.add), reads=[hf_, hd_], writes=[hf_])
                            S.op("act", lambda e: e.activation(out=sq[:], in_=hf_[:], func=AF.Square), reads=[hf_], writes=[sq])
                            S.op("dve", lambda e: e.tensor_reduce(out=ssn[:], in_=sq[:].rearrange("p (h d) -> p h d", d=64), axis=AX.X, op=ALU.add), reads=[sq], writes=[ssn])
                            self.rstd_cols(ssn, 8, 1.0 / 64)
                            S.op("dve", lambda e: e.tensor_tensor(out=hf_[:].rearrange("p (h d) -> p h d", d=64), in0=hf_[:].rearrange("p (h d) -> p h d", d=64),
                                                                  in1=ssn[:].unsqueeze(2).to_broadcast([128, 8, 64]), op=ALU.mult), reads=[hf_, ssn], writes=[hf_])
                            S.op("dve", lambda e: e.tensor_tensor(out=hf_[:], in0=hf_[:], in1=ng[:], op=ALU.mult), reads=[hf_, ng], writes=[hf_])
                            S.op("dve", lambda e: e.tensor_tensor(out=mx[:], in0=hf_[:], in1=o_[:], op=ALU.mult), reads=[hf_, o_], writes=[mx])
                            mT_ = mxT[b_]
                            for cc in range(4):
                                S.op("pe", lambda e: e.transpose(out=pT4[:, cc, :], in_=mx[:, cc * 128:(cc + 1) * 128], identity=self.identb[:]), reads=[mx, self.identb], writes=[pT4], sig=(cc == 3))
                            S.op("act", lambda e: e.copy(out=mT_[:], in_=pT4[:]), reads=[pT4], writes=[mT_])
                            S.dma("act", SC["MIXT"][0:4, :, trow:trow + 128].rearrange("c p t -> p c t"), mT_[:], reads=[mT_], writes=[SC["MIXm_b"][tch]])
                    for j in range(4):
                        u_ = pU[ku % 2]
                        ku += 1
                        S.op("pe", lambda e: e.matmul(u_[:], lhsT=kt_[:, 2 * j:2 * j + 2, :].rearrange("p h d -> p (h d)"), rhs=va_[:, 2 * j:2 * j + 2, :].rearrange("p h d -> p (h d)"), start=True, stop=True),
                             reads=[kt_, va_], writes=[u_])
                        S.op("dve", lambda e: e.scalar_tensor_tensor(out=chat[j][:], in0=chat[j][:], scalar=dl[:, d * 4 + j, c:c + 1], in1=u_[:], op0=ALU.mult, op1=ALU.add),
                             reads=[chat[j], dl, u_], writes=[chat[j]])


    def attn_phase(self, SC):
        S, nc, T = self.S, self.nc, self.T
        I = self.inp
        Sall = CTX + T
        NS = Sall // 128
        with self.scope() as es:
            cb = self.sb(es, "c_cb", [128, 8], F32)
            nlam = self.sb(es, "c_nlam", [128, 1], F32)
            gd = self.sb(es, "c_gd", [128, 4], F32)
            ones_k = self.sb(es, "c_onesk", [128, 128], BF16)
            ones_d = self.sb(es, "c_onesd", [128, 128], BF16)
            S.op("dve", lambda e: e.memset(ones_k[:], 1.0), writes=[ones_k])
            S.op("dve", lambda e: e.memset(ones_d[:], 1.0 / 128), writes=[ones_d])
            S.dma("sp", gd[:], I["l0_diff_norm_g"].rearrange("(c p) -> p c", p=128), writes=[gd], allow_slow_non_contiguous=True)
            S.op("dve", lambda e: e.tensor_scalar(out=gd[:], in0=gd[:], scalar1=0.8, scalar2=None, op0=ALU.mult), reads=[gd], writes=[gd])
            with self.scope() as les:
                pa = self.ps(les, "c_pa", [8, 128], F32)
                pb = self.ps(les, "c_pb", [8, 128], F32)
                pc = self.ps(les, "c_pc", [128, 8], F32)
                pl = self.ps(les, "c_pl", [128, 1], F32)
                qm = self.sb(les, "c_qm", [8, 2], F32)
                dg = self.sb(les, "c_dg", [8, 8], F32)
                o8 = self.sb(les, "c_o8", [8, 128], F32)
                lv = self.sb(les, "c_lv", [1, 4, 64], F32)
                lr = self.sb(les, "c_lr", [1, 4], F32)
                S.op("pe", lambda e: e.transpose(out=pa[:], in_=self.q2max[:], identity=self.identf[:]), reads=[self.q2max, self.identf], writes=[pa])
                S.op("pe", lambda e: e.transpose(out=pb[:], in_=self.k2max[:], identity=self.identf[:]), reads=[self.k2max, self.identf], writes=[pb])
                S.op("dve", lambda e: e.reduce_max(out=qm[:, 0:1], in_=pa[:], axis=AX.X), reads=[pa], writes=[qm])
                S.op("dve", lambda e: e.reduce_max(out=qm[:, 1:2], in_=pb[:], axis=AX.X), reads=[pb], writes=[qm])
                S.op("dve", lambda e: e.tensor_tensor(out=qm[:, 0:1], in0=qm[:, 0:1], in1=qm[:, 1:2], op=ALU.mult), reads=[qm], writes=[qm])
                S.op("act", lambda e: e.activation(out=qm[:, 0:1], in_=qm[:, 0:1], func=AF.Ln, bias=self.epsc[0:8, 0:1]), reads=[qm, self.epsc], writes=[qm])
                S.op("act", lambda e: e.activation(out=qm[:, 0:1], in_=qm[:, 0:1], func=AF.Exp, scale=0.5), reads=[qm], writes=[qm])
                S.op("dve", lambda e: e.tensor_scalar(out=dg[:], in0=self.identf[0:8, 0:8], scalar1=qm[:, 0:1], scalar2=-0.125, op0=ALU.mult, op1=ALU.mult), reads=[self.identf, qm], writes=[dg])
                S.op("dve", lambda e: e.memset(o8[:], 1.0), writes=[o8])
                S.op("pe", lambda e: e.matmul(pc[:], lhsT=o8[:], rhs=dg[:], start=True, stop=True), reads=[o8, dg], writes=[pc])
                S.op("dve", lambda e: e.tensor_copy(out=cb[:], in_=pc[:]), reads=[pc], writes=[cb])
                for i, nm in enumerate(["l0_lambda_q1", "l0_lambda_k1", "l0_lambda_q2", "l0_lambda_k2"]):
                    S.dma("sp", lv[:, i, :], I[nm].unsqueeze(0), writes=[lv])
                S.op("dve", lambda e: e.tensor_tensor(out=lv[:, 0, :], in0=lv[:, 0, :], in1=lv[:, 1, :], op=ALU.mult), reads=[lv], writes=[lv])
                S.op("dve", lambda e: e.tensor_tensor(out=lv[:, 2, :], in0=lv[:, 2, :], in1=lv[:, 3, :], op=ALU.mult), reads=[lv], writes=[lv])
                S.op("dve", lambda e: e.reduce_sum(out=lr[:, 0:1], in_=lv[:, 0, :], axis=AX.X), reads=[lv], writes=[lr])
                S.op("dve", lambda e: e.reduce_sum(out=lr[:, 1:2], in_=lv[:, 2, :], axis=AX.X), reads=[lv], writes=[lr])
                S.op("act", lambda e: e.activation(out=lr[:, 0:2], in_=lr[:, 0:2], func=AF.Exp), reads=[lr], writes=[lr])
                S.op("dve", lambda e: e.tensor_tensor(out=lr[:, 2:3], in0=lr[:, 1:2], in1=lr[:, 0:1], op=ALU.subtract), reads=[lr], writes=[lr])
                S.op("dve", lambda e: e.tensor_scalar(out=lr[:, 2:3], in0=lr[:, 2:3], scalar1=-0.2, scalar2=None, op0=ALU.add), reads=[lr], writes=[lr])
                S.op("pe", lambda e: e.matmul(pl[:], lhsT=self.ones1[:], rhs=lr[:, 2:3], start=True, stop=True), reads=[self.ones1, lr], writes=[pl])
                S.op("dve", lambda e: e.tensor_copy(out=nlam[:], in_=pl[:]), reads=[pl], writes=[nlam])
            cbh = self.sb(es, "c_cbh", [128, 4], F32)
            cb2 = cb[:].rearrange("p (h b) -> p h b", b=2)
            S.op("dve", lambda e: e.tensor_tensor(out=cbh[:].unsqueeze(2), in0=cb2[:, :, 0:1], in1=cb2[:, :, 1:2], op=ALU.min), reads=[cb], writes=[cbh])
            Kh = [self.sb(es, "c_Kh%d" % i, [128, Sall], BF16) for i in range(2)]
            Vh = [self.sb(es, "c_Vh%d" % i, [128, NS, 128], BF16) for i in range(2)]
            Q = [self.sb(es, "c_Q%d" % i, [128, 512], BF16) for i in range(2)]
            PT = [self.sb(es, "c_PT%d" % i, [128, 2, 512], BF16) for i in range(3)]
            rr = self.sb(es, "c_rr", [128, 512], F32)
            lacc = self.sb(es, "c_lacc", [128, 2, 512], F32)
            ones_f = self.sb(es, "c_onesf", [128, 128], F32)
            S.op("dve", lambda e: e.memset(ones_f[:], 1.0), writes=[ones_f])
            O = [self.sb(es, "c_O%d" % i, [128, 512], F32) for i in range(2)]
            sqb = self.sb(es, "c_sqb", [128, 512], BF16)
            of = [self.sb(es, "c_of%d" % i, [128, 512], BF16) for i in range(2)]
            pS = []
            for i in range(2):
                t_ = es.enter_context(self.nc.psum_tensor(self._uniq("c_pS%d" % i), [128, 1024], F32))
                pS.append(Tile(t_[:].rearrange("p (a b) -> p a b", b=512), "c_pS%d" % i))
            pO = [self.ps(es, "c_pO%d" % i, [128, 512], F32) for i in range(2)]
            pL = [self.ps(es, "c_pL%d" % i, [128, 512], F32) for i in range(2)]
            kq = 0
            kk = 0
            for h in range(4):
                K_, V_ = Kh[h % 2], Vh[h % 2]
                S.dma("sp", K_[:], SC["KD"][h, :, :], reads=SC["KD_b"], writes=[K_])
                S.dma("sp", V_[:], SC["VD"][:, h * 128:(h + 1) * 128].rearrange("(n p) d -> p n d", p=128), reads=SC["VD_b"], writes=[V_])
                for qt in range(T // 512):
                    Q_ = Q[kq % 2]
                    of_ = of[kq % 2]
                    kq += 1
                    S.dma("sp", Q_[:], SC["QD"][h, :, qt * 512:(qt + 1) * 512], reads=[SC["QD_b"][1 + qt]], writes=[Q_])

                    def scores(kt, slot):
                        ps_ = pS[slot % 2]
                        for b in range(2):
                            S.op("pe", lambda e: e.matmul(ps_[:, b, :], lhsT=K_[b * 64:(b + 1) * 64, kt * 128:(kt + 1) * 128], rhs=Q_[b * 64:(b + 1) * 64, :], start=True, stop=True),
                                 reads=[K_, Q_], writes=[ps_], sig=(b == 1))

                    def expo(kt, slot):
                        ps_, pt_ = pS[slot % 2], PT[slot % 3]
                        S.op("act", lambda e: e.activation(out=pt_[:], in_=ps_[:], func=AF.Exp, scale=0.125, bias=cbh[:, h:h + 1]), reads=[ps_, cbh], writes=[pt_])

                    def consume(kt, slot):
                        pt_ = PT[slot % 3]
                        on_pe = (kt % 4 == 0)
                        for b in range(2):
                            S.op("pe", lambda e: e.matmul(pO[b][:], lhsT=V_[:, kt, :], rhs=pt_[:, b, :], start=(kt == 0), stop=(kt == NS - 1)), reads=[V_, pt_], writes=[pO[b]],
                                 sig=((not on_pe) and b == 1))
                        if on_pe:
                            for b in range(2):
                                S.op("pe", lambda e: e.matmul(pL[b][:], lhsT=ones_k[:], rhs=pt_[:, b, :], start=(kt == 0), stop=False), reads=[ones_k, pt_], writes=[pL[b]], sig=(b == 1))
                        elif kt == 1:
                            S.op("dve", lambda e: e.tensor_copy(out=lacc[:], in_=pt_[:]), reads=[pt_], writes=[lacc])
                        else:
                            S.op("dve", lambda e: e.tensor_tensor(out=lacc[:], in0=pt_[:], in1=lacc[:], op=ALU.add), reads=[pt_, lacc], writes=[lacc])

                    scores(0, kk)
                    if NS > 1:
                        scores(1, kk + 1)
                    for kt in range(NS):
                        expo(kt, kk + kt)
                        if kt + 2 < NS:
                            scores(kt + 2, kk + kt + 2)
                        consume(kt, kk + kt)
                    kk += NS
                    for b in range(2):
                        S.op("pe", lambda e: e.matmul(pL[b][:], lhsT=ones_f[:], rhs=lacc[:, b, :], start=False, stop=True), reads=[ones_f, lacc], writes=[pL[b]], sig=True)
                        S.op("dve", lambda e: e.reciprocal(out=rr[:], in_=pL[b][:]), reads=[pL[b]], writes=[rr])
                        S.op("dve", lambda e: e.tensor_tensor(out=O[b][:], in0=pO[b][:], in1=rr[:], op=ALU.mult), reads=[pO[b], rr], writes=[O[b]])
                    pM = pL[0]
                    S.op("dve", lambda e: e.scalar_tensor_tensor(out=O[0][:], in0=O[1][:], scalar=nlam[:, 0:1], in1=O[0][:], op0=ALU.mult, op1=ALU.add), reads=[O[1], nlam, O[0]], writes=[O[0]])
                    S.op("act", lambda e: e.activation(out=sqb[:], in_=O[0][:], func=AF.Square), reads=[O[0]], writes=[sqb])
                    S.op("pe", lambda e: e.matmul(pM[:], lhsT=ones_d[:], rhs=sqb[:], start=True, stop=True), reads=[ones_d, sqb], writes=[pM])
                    S.op("act", lambda e: e.activation(out=rr[:], in_=pM[:], func=AF.Ln, bias=self.epsc[:, 0:1]), reads=[pM, self.epsc], writes=[rr])
                    S.op("act", lambda e: e.activation(out=rr[:], in_=rr[:], func=AF.Exp, scale=-0.5), reads=[rr], writes=[rr])
                    S.op("dve", lambda e: e.scalar_tensor_tensor(out=of_[:], in0=O[0][:], scalar=gd[:, h:h + 1], in1=rr[:], op0=ALU.mult, op1=ALU.mult), reads=[O[0], gd, rr], writes=[of_])
                    S.dma("sp", SC["MIXT"][4 + h, :, qt * 512:(qt + 1) * 512], of_[:], reads=[of_], writes=[SC["MIXd_b"][h * (T // 512) + qt]])

    def wout_phase(self, x, x_bufs, xout, xout_bufs, bc, SC, wout, wout_b):
        S, nc, T = self.S, self.nc, self.T
        with self.scope() as es:
            wo = self.sb(es, "d_wo", [128, 8, D], BF16)
            S.dma("sp", wo[:], wout.rearrange("(c p) n -> p c n", p=128), reads=wout_b, writes=[wo])
            mT = [self.sb(es, "d_mT%d" % i, [128, 8, 128], BF16) for i in range(2)]
            xt = [self.sb(es, "d_xt%d" % i, [128, D], F32) for i in range(2)]
            ys = [self.sb(es, "d_y%d" % i, [128, D], F32) for i in range(2)]
            xos = [self.sb(es, "d_xo%d" % i, [128, D], F32) for i in range(2)]
            junks = [self.sb(es, "d_junk%d" % i, [128, D], BF16) for i in range(2)]
            sss = [self.sb(es, "d_ss%d" % i, [128, 4], F32) for i in range(2)]
            py = [self.ps(es, "d_py%d" % i, [128, 512], F32) for i in range(2)]
            ky = 0
            nq = T // 512
            for ti in range(T // 128):
                m_, x_ = mT[ti % 2], xt[ti % 2]
                y, xo, junk, ss = ys[ti % 2], xos[ti % 2], junks[ti % 2], sss[ti % 2]
                rd = [SC["MIXm_b"][ti]] + [SC["MIXd_b"][h * nq + ti // 4] for h in range(4)]
                S.dma("sp", m_[:], SC["MIXT"][:, :, ti * 128:(ti + 1) * 128].rearrange("c p t -> p c t"), reads=rd, writes=[m_])
                S.dma("sp", x_[:], x[ti * 128:(ti + 1) * 128, :], reads=[x_bufs[ti]], writes=[x_])
                for dg in range(2):
                    y_ = py[ky % 2]
                    ky += 1
                    for c in range(8):
                        S.op("pe", lambda e: e.matmul(y_[:], lhsT=m_[:, c, :], rhs=wo[:, c, dg * 512:(dg + 1) * 512], start=(c == 0), stop=(c == 7)), reads=[m_, wo], writes=[y_])
                    S.op("act", lambda e: e.copy(out=y[:, dg * 512:(dg + 1) * 512], in_=y_[:]), reads=[y_], writes=[y])
                self.post_norm_residual(y, x_, bc["ggt_m"], ss, junk, xo)
                S.dma("sp", xout[ti * 128:(ti + 1) * 128, :], xo[:], reads=[xo], writes=[xout_bufs[ti]])

    def layer0_scratch(self):
        T = self.T
        Sall = CTX + T
        NTL, NS, NG = T // 128, Sall // 128, T // 512
        SC = {}
        d = self.dscr
        SC["PQ"] = d("s_PQ", [4, 128, T + 2], F32); SC["PK"] = d("s_PK", [4, 128, T + 2], F32); SC["PKc"] = d("s_PKc", [4, 128, CTX + 2], F32)
        SC["QMT"] = d("s_QMT", [4, 128, T], BF16, dbg=True); SC["KMT"] = d("s_KMT", [4, 128, Sall], BF16, dbg=True); SC["KM"] = d("s_KM", [Sall, 512], BF16)
        SC["VM"] = d("s_VM", [Sall, 512], BF16, dbg=True); SC["OM"] = d("s_OM", [T, 512], F32, dbg=True); SC["VD"] = d("s_VD", [Sall, 512], BF16)
        SC["QD"] = d("s_QD", [4, 128, T], BF16, dbg=True); SC["KD"] = d("s_KD", [4, 128, Sall], BF16, dbg=True); SC["GT"] = d("s_GT", [2, 64, Sall], F32, dbg=True)
        SC["HF"] = d("s_HF", [T, 512], F32, dbg=True); SC["MIXT"] = d("s_MIXT", [8, 128, T], BF16, dbg=True)
        SC["pad_b"] = Buf("pad"); SC["PKc_b"] = Buf("PKc")
        SC["PQ_b"] = [Buf("PQ%d" % i) for i in range(NG)]; SC["PK_b"] = [Buf("PK%d" % i) for i in range(NG)]
        SC["QMT_b"] = [Buf("QMT%d" % i) for i in range(NG)]; SC["KMT_b"] = [Buf("KMT%d" % i) for i in range(NG + 1)]
        SC["KM_b"] = [Buf("KM%d" % i) for i in range(NS)]; SC["VM_b"] = [Buf("VM%d" % i) for i in range(NS)]; SC["VD_b"] = [Buf("VD%d" % i) for i in range(NS)]
        SC["OM_b"] = [Buf("OM%d" % i) for i in range(NTL)]; SC["HF_b"] = [Buf("HF%d" % i) for i in range(NTL)]
        SC["QD_b"] = [Buf("QD%d" % i) for i in range(NG + 1)]; SC["KD_b"] = [Buf("KD%d" % i) for i in range(NG + 1)]; SC["GT_b"] = [Buf("GT%d" % i) for i in range(NG + 1)]
        SC["MIXm_b"] = [Buf("MXm%d" % i) for i in range(NTL)]; SC["MIXd_b"] = [Buf("MXd%d" % i) for i in range(4 * NG)]
        return SC

    def build(self):
        nc, T = self.nc, self.T
        x = self.din("x", [T, D])
        self.din("c", [D])
        self.din("ctx", [CTX, D])
        self.din("c_ctx", [D])
        shapes = {
            "l0_mod_w": [D, 6 * D], "l0_mod_b": [6 * D], "l0_mix_pre_g": [D], "l0_mix_post_g": [D], "l0_w_in": [D, IN0],
            "l0_mlstm_gate_b": [32], "l0_mlstm_conv_w": [3, D], "l0_mlstm_norm_g": [512], "l0_lambda_q1": [64], "l0_lambda_k1": [64],
            "l0_lambda_q2": [64], "l0_lambda_k2": [64], "l0_diff_norm_g": [512], "l0_w_out": [D, D], "l0_ffn_pre_g": [D],
            "l0_ffn_post_g": [D], "l0_ffn_w1": [D, DFF], "l0_ffn_w3": [D, DFF], "l0_ffn_w2": [DFF, D],
            "l1_mod_w": [D, 6 * D], "l1_mod_b": [6 * D], "l1_mix_pre_g": [D], "l1_mix_post_g": [D], "l1_conv_pw1_w": [D, 2 * D],
            "l1_conv_pw1_b": [2 * D], "l1_conv_dw_w": [31, D], "l1_conv_dw_b": [D], "l1_conv_ln_g": [D], "l1_conv_ln_b": [D],
            "l1_conv_pw2_w": [D, D], "l1_conv_pw2_b": [D], "l1_ffn_pre_g": [D], "l1_ffn_post_g": [D], "l1_router_w": [D, NEXP],
            "l1_moe_w1": [NEXP * D, DFF], "l1_moe_w3": [NEXP * D, DFF], "l1_moe_w2": [NEXP * DFF, D],
        }
        for n, s in shapes.items():
            self.din(n, s)
        ident_in = self.din("k_ident", [128, 128])
        self.din("k_wfm", [D, 1024]); self.din("k_wtm", [D, 2688]); self.din("k_gate_b", [64, 2]); self.din("k_gcoef", [64, 4])
        self.din("k_sel", [64, 8, 128]); self.din("k_masks", [2, 128, 128]); self.din("k_rope", [T, 64])
        self.din("k_tri", [128, 128]); self.din("k_sbpos", [64]); self.din("k_wbase", [128, 6])
        out = nc.dram_tensor("out", [T, D], F32, kind="ExternalOutput").ap()
        I = self.inp
        NTL = T // 128
        with ExitStack() as es:
            self.S = S = Sched(nc, es)
            self.identf = self.sb(es, "identf", [128, 128], F32)
            self.identb = self.sb(es, "identb", [128, 128], BF16)
            self.ones1 = self.sb(es, "ones1", [1, 128], F32)
            self.onesb = self.sb(es, "onesb", [128, 128], BF16)
            self.epsc = self.sb(es, "epsc", [128, 1], F32)
            self.onec = self.sb(es, "onec", [128, 1], F32)
            S.dma("sp", self.identf[:], ident_in[:, :], writes=[self.identf])
            S.op("dve", lambda e: e.tensor_copy(out=self.identb[:], in_=self.identf[:]), reads=[self.identf], writes=[self.identb])
            S.op("dve", lambda e: e.memset(self.ones1[:], 1.0), writes=[self.ones1])
            S.op("dve", lambda e: e.memset(self.onesb[:], 1.0 / 1024), writes=[self.onesb])
            S.op("dve", lambda e: e.memset(self.epsc[:], EPS), writes=[self.epsc])
            S.op("dve", lambda e: e.memset(self.onec[:], 1.0), writes=[self.onec])
            W = {}
            self.q2max = self.sb(es, "q2max", [128, 8], F32)
            self.k2max = self.sb(es, "k2max", [128, 8], F32)
            if 0 in self.layers:
                W["wo"], W["wo_b"] = self.convert(I["l0_w_out"], "wb_wo", D, D)
                W["f1"], W["f1_b"] = self.convert(I["l0_ffn_w1"], "wb_f1", D, DFF)
                W["f3"], W["f3_b"] = self.convert(I["l0_ffn_w3"], "wb_f3", D, DFF)
                W["f2"], W["f2_b"] = self.convert(I["l0_ffn_w2"], "wb_f2", DFF, D)
            if 1 in self.layers:
                W["pw1"], W["pw1_b"] = self.convert(I["l1_conv_pw1_w"], "wb_pw1", D, 2 * D)
                W["pw2"], W["pw2_b"] = self.convert(I["l1_conv_pw2_w"], "wb_pw2", D, D)
                if 0 not in self.layers:
                    W.update(self.convert_moe(I["l1_moe_w1"], I["l1_moe_w3"], I["l1_moe_w2"]))
            xa = self.dscr("xa_scr", [T, D], F32, dbg=True)
            xb = self.dscr("xb_scr", [T, D], F32, dbg=True)
            xa_b = [Buf("xa%d" % i) for i in range(NTL)]
            xb_b = [Buf("xb%d" % i) for i in range(NTL)]
            x_b = [Buf("x%d" % i) for i in range(NTL)]
            out_b = [Buf("out%d" % i) for i in range(NTL)]
            cur, cur_b = x, x_b
            if 0 in self.layers:
                SC = self.layer0_scratch()
                with self.scope() as les:
                    bc = self.adaln(les, I["c"], I["l0_mod_w"], I["l0_mod_b"], None, [
                        ("gmod_m", 1, "gmod", I["l0_mix_pre_g"]), ("shift_m", 0, "shift", None), ("ggt_m", 2, "ggt", I["l0_mix_post_g"])])
                    bc.update(self.adaln(les, I["c_ctx"], I["l0_mod_w"], I["l0_mod_b"], None, [
                        ("gmod_c", 1, "gmod", I["l0_mix_pre_g"]), ("shift_c", 0, "shift", None)]))
                    self.proj_phase(x, x_b, bc, SC)
                    self.mlstm_phase(SC)
                    if 1 in self.layers:
                        W.update(self.convert_moe(I["l1_moe_w1"], I["l1_moe_w3"], I["l1_moe_w2"]))
                    self.attn_phase(SC)
                    self.wout_phase(x, x_b, xa, xa_b, bc, SC, W["wo"], W["wo_b"])
                with self.scope() as les:
                    bc = self.adaln(les, I["c"], I["l0_mod_w"], I["l0_mod_b"], None, [
                        ("gmod_f", 4, "gmod", I["l0_ffn_pre_g"]), ("shift_f", 3, "shift", None), ("ggt_f", 5, "ggt", I["l0_ffn_post_g"])])
                    dst, dst_b = (xb, xb_b) if 1 in self.layers else (out, out_b)
                    self.ffn_phase("f_", xa, xa_b, dst, dst_b, bc, W["f1"], W["f1_b"], W["f3"], W["f3_b"], W["f2"], W["f2_b"], 1)
                cur, cur_b = xb, xb_b
            if 1 in self.layers:
                with self.scope() as les:
                    bc = self.adaln(les, I["c"], I["l1_mod_w"], I["l1_mod_b"], None, [
                        ("gmod_m", 1, "gmod", I["l1_mix_pre_g"]), ("shift_m", 0, "shift", None), ("ggt_m", 2, "ggt", I["l1_mix_post_g"]),
                        ("gmod_f", 4, "gmod", I["l1_ffn_pre_g"]), ("shift_f", 3, "shift", None), ("ggt_f", 5, "ggt", I["l1_ffn_post_g"])])
                    self.conv_phase(cur, cur_b, xa, xa_b, bc, W)
                    self.moe_phase(xa, xa_b, out, out_b, bc, W)
            S.finish(out_b, "sp")
            self.stats = (dict(S.n_inst), S.n_wait, dict(S.cnt))
        return nc


_CACHE = {}


def _in_map(inputs, b):
    m = {}
    for k, v in inputs.items():
        v = np.asarray(v, dtype=np.float32)
        if k in ("x", "c", "ctx"):
            m[k] = np.ascontiguousarray(v[b])
        elif k in ("l1_moe_w1", "l1_moe_w3", "l1_moe_w2"):
            m[k] = np.ascontiguousarray(v.reshape(v.shape[0] * v.shape[1], v.shape[2]))
        else:
            m[k] = np.ascontiguousarray(v)
    m["k_ident"] = np.eye(128, dtype=np.float32)
    m.update(_consts(np.asarray(inputs["l0_w_in"], np.float32), np.asarray(inputs["l0_mlstm_gate_b"], np.float32), m["x"].shape[0]))
    return m


def _consts(w_in, gate_b, T):
    c = {}
    c["k_wfm"] = np.ascontiguousarray(w_in[:, 0:1024])
    g = np.zeros((D, 128), np.float32)
    g[:, 0:8] = w_in[:, 2048:2056]
    g[:, 32:40] = w_in[:, 2064:2072]
    g[:, 64:72] = w_in[:, 2056:2064]
    g[:, 96:104] = w_in[:, 2072:2080]
    c["k_wtm"] = np.ascontiguousarray(np.concatenate([w_in[:, 1024:1536], w_in[:, 1536:2048], w_in[:, 3104:3616], w_in[:, 2080:2592], w_in[:, 2592:3104], g], axis=1))
    gb = np.zeros((64, 2), np.float32)
    gb[0:8, 0] = gate_b[0:8]
    gb[32:40, 0] = gate_b[16:24]
    gb[0:8, 1] = gate_b[8:16]
    gb[32:40, 1] = gate_b[24:32]
    c["k_gate_b"] = gb
    co = np.zeros((64, 4), np.float32)
    co[0:32, 0] = 1.0
    co[32:64, 0] = -1.0
    co[32:64, 1] = 1.0
    c["k_gcoef"] = co
    sel = np.zeros((64, 8, 128), np.float32)
    for d in range(2):
        for j in range(4):
            sel[d * 32 + 2 * j, d * 4 + j, 0:64] = 1.0
            sel[d * 32 + 2 * j + 1, d * 4 + j, 64:128] = 1.0
    c["k_sel"] = sel
    sidx = np.arange(128)
    mk = np.zeros((2, 128, 128), np.float32)
    mk[0] = (sidx[:, None] <= sidx[None, :]) * 0.125
    mk[1] = (sidx[:, None] >= sidx[None, :]) * 0.125
    c["k_masks"] = mk
    c["k_tri"] = (sidx[:, None] < sidx[None, :]).astype(np.float32)
    c["k_sbpos"] = (np.arange(64) * 512).astype(np.float32)
    c["k_wbase"] = (np.arange(6)[None, :] * 128 + np.arange(128)[:, None]).astype(np.float32)
    t = np.arange(T)
    inv = (np.float32(10000.0) ** (-np.arange(0, 32, 2, dtype=np.float32) / np.float32(32))).astype(np.float32)
    ar = (t // 64).astype(np.float32)[:, None] * inv
    ac = (t % 64).astype(np.float32)[:, None] * inv
    c["k_rope"] = np.concatenate([np.cos(ar), np.sin(ar), np.cos(ac), np.sin(ac)], axis=1).astype(np.float32)
    return c


def kernel(**inputs):
    B, T, _ = inputs["x"].shape
    key = (T,)
    if key not in _CACHE:
        _CACHE[key] = K(T).build()
    nc = _CACHE[key]
    in_maps = [_in_map(inputs, b) for b in range(B)]
    res = run_bass_kernel_spmd(nc, in_maps, core_ids=list(range(B)))
    return np.stack([np.asarray(r["out"], dtype=np.float32) for r in res.results], axis=0)
```
